# Optimizing a Trainium2 kernel written in Bass

```python
import math
import jax, jax.numpy as jnp
from jax import lax
import numpy as np

D_MODEL = 1024
BATCH = 8
SEQ = 8192
DEPTH = 2

ATT_HEADS = D_MODEL // 128
ATT_HEAD_DIM = 64
ATT_V_DIM = 2 * ATT_HEAD_DIM
ATT_WIDTH = ATT_HEADS * ATT_V_DIM
Q_BLOCK = 128
ALIBI_MAX_BIAS = 8.0

GDN_HEADS = D_MODEL // 128
GDN_DK = 128
GDN_DV = 128
GDN_WIDTH = GDN_HEADS * GDN_DV
CONV_WIDTH = 4
CHUNK = 64

N_GROUPS = 4
EXPERTS_PER_GROUP = 8
N_EXPERTS = N_GROUPS * EXPERTS_PER_GROUP
TOP_K = 2
D_EXPERT = D_MODEL // 2
MOE_BLOCK = 128

NORM_EPS = 1e-6
NEG_INF = -1e30
ADA_INIT = 0.01

P_QA = ATT_HEADS * 2 * ATT_HEAD_DIM
P_KA = ATT_HEADS * 2 * ATT_HEAD_DIM
P_VA = ATT_WIDTH
P_QKV_B = GDN_HEADS * (2 * GDN_DK + GDN_DV)
P_ZB = GDN_WIDTH
P_BETA = GDN_HEADS
P_DECAY = GDN_HEADS
P_GATES = 2 * D_MODEL
SPLIT_SIZES = (P_QA, P_KA, P_VA, P_QKV_B, P_ZB, P_BETA, P_DECAY, P_GATES)
SPLIT_POINTS = tuple(sum(SPLIT_SIZES[:i + 1]) for i in range(len(SPLIT_SIZES) - 1))
P_TOTAL = sum(SPLIT_SIZES)

kernel_name = 'hybrid_diffattn_gdn_hier_moe'


def rms_norm(x, w):
    xf = x.astype(jnp.float32)
    y = xf * lax.rsqrt(jnp.mean(xf * xf, axis=-1, keepdims=True) + NORM_EPS)
    return (y * w.astype(jnp.float32)).astype(x.dtype)


def l2_norm(x):
    return x * lax.rsqrt(jnp.sum(x * x, axis=-1, keepdims=True) + NORM_EPS)


def modulate(h, shift, scale):
    return h * (1 + scale[:, None, :]) + shift[:, None, :]


def causal_depthwise_conv(x, w):
    k_len, ch = w.shape
    return lax.conv_general_dilated(x, w[:, None, :].astype(x.dtype), window_strides=(1,),
                                    padding=((k_len - 1, 0),), dimension_numbers=('NWC', 'WIO', 'NWC'),
                                    feature_group_count=ch)


def diff_attention(q1, q2, k1, k2, v, lam):
    b, h, s, d = q1.shape
    scale = d ** -0.5
    slopes = jnp.exp2(-ALIBI_MAX_BIAS * jnp.arange(1, h + 1, dtype=jnp.float32) / h)
    kpos = jnp.arange(s)

    def block(i):
        start = i * Q_BLOCK
        qb1 = lax.dynamic_slice_in_dim(q1, start, Q_BLOCK, axis=2)
        qb2 = lax.dynamic_slice_in_dim(q2, start, Q_BLOCK, axis=2)
        dist = (start + jnp.arange(Q_BLOCK))[:, None] - kpos[None, :]
        bias = jnp.where(dist >= 0, -slopes[:, None, None] * dist.astype(jnp.float32), NEG_INF)
        s1 = jnp.einsum('bhqd,bhkd->bhqk', qb1, k1).astype(jnp.float32) * scale + bias
        s2 = jnp.einsum('bhqd,bhkd->bhqk', qb2, k2).astype(jnp.float32) * scale + bias
        p = jax.nn.softmax(s1, axis=-1) - lam * jax.nn.softmax(s2, axis=-1)
        return jnp.einsum('bhqk,bhke->bhqe', p.astype(v.dtype), v)

    out = lax.map(block, jnp.arange(s // Q_BLOCK))
    return jnp.moveaxis(out, 0, 2).reshape(b, h, s, v.shape[-1])


def diff_attention_branch(qa, ka, va, lq1, lk1, lq2, lk2, subln_w, lam_init):
    b, s, _ = qa.shape
    q = qa.reshape(b, s, ATT_HEADS, 2, ATT_HEAD_DIM).transpose(0, 2, 3, 1, 4)
    k = ka.reshape(b, s, ATT_HEADS, 2, ATT_HEAD_DIM).transpose(0, 2, 3, 1, 4)
    v = va.reshape(b, s, ATT_HEADS, ATT_V_DIM).transpose(0, 2, 1, 3)
    lam = (jnp.exp(jnp.dot(lq1.astype(jnp.float32), lk1.astype(jnp.float32)))
           - jnp.exp(jnp.dot(lq2.astype(jnp.float32), lk2.astype(jnp.float32))) + lam_init)
    o = diff_attention(q[:, :, 0], q[:, :, 1], k[:, :, 0], k[:, :, 1], v, lam)
    o = rms_norm(o, subln_w) * (1 - lam_init)
    return o.transpose(0, 2, 1, 3).reshape(b, s, ATT_WIDTH)


def gated_delta_rule(q, k, v, g, beta):
    b, h, s, dk = q.shape
    dv = v.shape[-1]
    n = s // CHUNK
    q = q * dk ** -0.5

    def chunks(t):
        return t.reshape(b, h, n, CHUNK, *t.shape[3:])

    q, k, v, beta = chunks(q), chunks(k), chunks(v), chunks(beta)
    g = jnp.cumsum(chunks(g), axis=-1)
    kb = k * beta[..., None]
    vb = v * beta[..., None]
    incl = jnp.tril(jnp.ones((CHUNK, CHUNK), dtype=bool))
    strict = jnp.tril(jnp.ones((CHUNK, CHUNK), dtype=bool), -1)
    diff = g[..., :, None] - g[..., None, :]
    decay = jnp.where(incl, jnp.exp(jnp.where(incl, diff, 0.0)), 0.0)
    a_mat = jnp.eye(CHUNK, dtype=jnp.float32) + jnp.where(
        strict, jnp.einsum('bhncd,bhnsd->bhncs', kb, k) * decay, 0.0)
    u = lax.linalg.triangular_solve(a_mat, vb, left_side=True, lower=True, unit_diagonal=True)
    w = lax.linalg.triangular_solve(a_mat, kb * jnp.exp(g)[..., None], left_side=True, lower=True,
                                    unit_diagonal=True)
    xs = tuple(jnp.moveaxis(t, 2, 0) for t in (q, k, u, w, g, decay))

    def step(state, inp):
        qc, kc, uc, wc, gc, dc = inp
        v_new = uc - jnp.einsum('bhck,bhkv->bhcv', wc, state)
        intra = jnp.einsum('bhck,bhsk->bhcs', qc, kc) * dc
        o = (jnp.einsum('bhck,bhkv->bhcv', qc * jnp.exp(gc)[..., None], state)
             + jnp.einsum('bhcs,bhsv->bhcv', intra, v_new))
        g_last = gc[..., -1]
        state = (state * jnp.exp(g_last)[..., None, None]
                 + jnp.einsum('bhck,bhcv->bhkv', kc * jnp.exp(g_last[..., None] - gc)[..., None], v_new))
        return state, o

    state0 = jnp.zeros((b, h, dk, dv), jnp.float32)
    _, o = lax.scan(step, state0, xs)
    return jnp.moveaxis(o, 0, 2).reshape(b, h, s, dv)


def gated_deltanet_branch(qkv_raw, z, b_raw, a_raw, conv_w, a_log, dt_bias, norm_w):
    bsz, s, _ = qkv_raw.shape
    qkv = jax.nn.silu(causal_depthwise_conv(qkv_raw, conv_w))
    q, k, v = jnp.split(qkv, [GDN_HEADS * GDN_DK, 2 * GDN_HEADS * GDN_DK], axis=-1)

    def heads(t, dh):
        return t.reshape(bsz, s, GDN_HEADS, dh).transpose(0, 2, 1, 3).astype(jnp.float32)

    q = l2_norm(heads(q, GDN_DK))
    k = l2_norm(heads(k, GDN_DK))
    v = heads(v, GDN_DV)
    beta = jax.nn.sigmoid(b_raw.astype(jnp.float32)).transpose(0, 2, 1)
    g = (-jnp.exp(a_log.astype(jnp.float32))
         * jax.nn.softplus(a_raw.astype(jnp.float32) + dt_bias.astype(jnp.float32))).transpose(0, 2, 1)
    o = gated_delta_rule(q, k, v, g, beta).transpose(0, 2, 1, 3)
    zf = z.reshape(bsz, s, GDN_HEADS, GDN_DV).astype(jnp.float32)
    o = rms_norm(o, norm_w) * jax.nn.silu(zf)
    return o.reshape(bsz, s, GDN_WIDTH).astype(qkv_raw.dtype)


def dispatch_experts(xt, expert_ids, weights, w1, w3, w2):
    n_tok, d = xt.shape
    flat_e = expert_ids.reshape(-1).astype(jnp.int32)
    n_assign = flat_e.shape[0]
    order = jnp.argsort(flat_e)
    sorted_e = flat_e[order]
    counts = jnp.bincount(flat_e, length=N_EXPERTS)
    padded = (counts + MOE_BLOCK - 1) // MOE_BLOCK * MOE_BLOCK
    pad_end = jnp.cumsum(padded)
    pad_start = pad_end - padded
    start = jnp.cumsum(counts) - counts
    dest = pad_start[sorted_e] + jnp.arange(n_assign, dtype=jnp.int32) - start[sorted_e]
    n_blocks = -(-n_assign // MOE_BLOCK) + N_EXPERTS
    n_rows = n_blocks * MOE_BLOCK
    row_tok = jnp.full((n_rows,), n_tok, jnp.int32).at[dest].set((order // TOP_K).astype(jnp.int32))
    row_w = jnp.zeros((n_rows,), xt.dtype).at[dest].set(weights.reshape(-1)[order].astype(xt.dtype))
    block_e = jnp.minimum(jnp.searchsorted(pad_end, jnp.arange(n_blocks, dtype=jnp.int32) * MOE_BLOCK,
                                           side='right'), N_EXPERTS - 1)
    x_rows = jnp.concatenate([xt, jnp.zeros((1, d), xt.dtype)], axis=0)[row_tok]
    x_rows = x_rows.reshape(n_blocks, MOE_BLOCK, d)

    def expert_block(args):
        xb, e = args
        return (jax.nn.silu(xb @ w1[e]) * (xb @ w3[e])) @ w2[e]

    y_rows = lax.map(expert_block, (x_rows, block_e)).reshape(n_rows, d) * row_w[:, None]
    return jnp.zeros((n_tok + 1, d), xt.dtype).at[row_tok].add(y_rows)[:n_tok]


def hierarchical_moe(h, w_rg, b_rg, w_re, b_re, w1, w3, w2):
    bsz, s, d = h.shape
    xt = h.reshape(-1, d)
    n_tok = xt.shape[0]
    gprob = jax.nn.softmax((xt @ w_rg).astype(jnp.float32) + b_rg.astype(jnp.float32), axis=-1)
    gsel = jnp.argmax(gprob, axis=-1)
    gweight = jnp.take_along_axis(gprob, gsel[:, None], axis=-1)
    elog = ((xt @ w_re).astype(jnp.float32) + b_re.astype(jnp.float32)).reshape(
        n_tok, N_GROUPS, EXPERTS_PER_GROUP)
    elog = jnp.take_along_axis(elog, gsel[:, None, None], axis=1)[:, 0]
    top_p, top_i = lax.top_k(jax.nn.softmax(elog, axis=-1), TOP_K)
    weights = gweight * top_p / jnp.sum(top_p, axis=-1, keepdims=True)
    expert_ids = gsel[:, None] * EXPERTS_PER_GROUP + top_i
    return dispatch_experts(xt, expert_ids, weights, w1, w3, w2).reshape(bsz, s, d)


def setup_inputs(seed: int = 0) -> dict:
    key = jax.random.key(seed)
    ks = jax.random.split(key, 32)
    f32 = jnp.float32
    d, l = D_MODEL, DEPTH

    def nrm(k, shape, s):
        return jax.random.normal(k, shape, f32) * s

    return {
        'x': nrm(ks[0], (BATCH, SEQ, d), 1.0),
        'c': nrm(ks[1], (BATCH, d), 1.0),
        'ada_w': nrm(ks[2], (l, d, 6 * d), ADA_INIT),
        'ada_b': nrm(ks[3], (l, 6 * d), 0.01),
        'norm1_w': 1.0 + nrm(ks[4], (l, d), 0.01),
        'w_in': nrm(ks[5], (l, d, P_TOTAL), d ** -0.5),
        'conv_w': nrm(ks[6], (l, CONV_WIDTH, P_QKV_B), CONV_WIDTH ** -0.5),
        'lambda_q1': nrm(ks[7], (l, ATT_HEAD_DIM), 0.1),
        'lambda_k1': nrm(ks[8], (l, ATT_HEAD_DIM), 0.1),
        'lambda_q2': nrm(ks[9], (l, ATT_HEAD_DIM), 0.1),
        'lambda_k2': nrm(ks[10], (l, ATT_HEAD_DIM), 0.1),
        'subln_w': 1.0 + nrm(ks[11], (l, ATT_V_DIM), 0.01),
        'a_log': jnp.log(jax.random.uniform(ks[12], (l, GDN_HEADS), f32, 1.0, 16.0)),
        'dt_bias': nrm(ks[13], (l, GDN_HEADS), 0.1),
        'gdn_norm_w': 1.0 + nrm(ks[14], (l, GDN_DV), 0.01),
        'w_branch_a': nrm(ks[15], (l, ATT_WIDTH, d), ATT_WIDTH ** -0.5),
        'w_branch_b': nrm(ks[16], (l, GDN_WIDTH, d), GDN_WIDTH ** -0.5),
        'w_out': nrm(ks[17], (l, d, d), d ** -0.5),
        'norm2_w': 1.0 + nrm(ks[18], (l, d), 0.01),
        'router_group_w': nrm(ks[19], (l, d, N_GROUPS), d ** -0.5),
        'router_group_b': nrm(ks[20], (l, N_GROUPS), 0.01),
        'router_expert_w': nrm(ks[21], (l, d, N_EXPERTS), d ** -0.5),
        'router_expert_b': nrm(ks[22], (l, N_EXPERTS), 0.01),
        'expert_w1': nrm(ks[23], (l, N_EXPERTS, d, D_EXPERT), d ** -0.5),
        'expert_w3': nrm(ks[24], (l, N_EXPERTS, d, D_EXPERT), d ** -0.5),
        'expert_w2': nrm(ks[25], (l, N_EXPERTS, D_EXPERT, d), D_EXPERT ** -0.5),
        'final_norm_w': 1.0 + nrm(ks[26], (d,), 0.01),
    }


def reference(x, c, ada_w, ada_b, norm1_w, w_in, conv_w, lambda_q1, lambda_k1, lambda_q2, lambda_k2,
              subln_w, a_log, dt_bias, gdn_norm_w, w_branch_a, w_branch_b, w_out, norm2_w,
              router_group_w, router_group_b, router_expert_w, router_expert_b,
              expert_w1, expert_w3, expert_w2, final_norm_w):
    cond = jax.nn.silu(c)
    for l in range(DEPTH):
        lam_init = 0.8 - 0.6 * math.exp(-0.3 * l)
        mod = cond @ ada_w[l] + ada_b[l]
        shift1, scale1, gate1, shift2, scale2, gate2 = jnp.split(mod, 6, axis=-1)

        h = modulate(rms_norm(x, norm1_w[l]), shift1, scale1)
        proj = h @ w_in[l]
        qa, ka, va, qkv_b, zb, beta_b, decay_b, gates = jnp.split(proj, SPLIT_POINTS, axis=-1)
        y_a = diff_attention_branch(qa, ka, va, lambda_q1[l], lambda_k1[l], lambda_q2[l], lambda_k2[l],
                                    subln_w[l], lam_init)
        y_b = gated_deltanet_branch(qkv_b, zb, beta_b, decay_b, conv_w[l], a_log[l], dt_bias[l],
                                    gdn_norm_w[l])
        gate_a, gate_b = jnp.split(jax.nn.sigmoid(gates), 2, axis=-1)
        mixed = gate_a * (y_a @ w_branch_a[l]) + gate_b * (y_b @ w_branch_b[l])
        x = x + gate1[:, None, :] * (mixed @ w_out[l])

        h2 = modulate(rms_norm(x, norm2_w[l]), shift2, scale2)
        y_ffn = hierarchical_moe(h2, router_group_w[l], router_group_b[l], router_expert_w[l],
                                 router_expert_b[l], expert_w1[l], expert_w3[l], expert_w2[l])
        x = x + gate2[:, None, :] * y_ffn
    return rms_norm(x, final_norm_w)
```

```python
import math
from contextlib import ExitStack
import numpy as np
import ml_dtypes
import concourse.bass as bass
import concourse.mybir as mybir
from concourse.bass_utils import run_bass_kernel_spmd

F32 = mybir.dt.float32
BF16 = mybir.dt.bfloat16
I32 = mybir.dt.int32
U32 = mybir.dt.uint32
AF = mybir.ActivationFunctionType
ALU = mybir.AluOpType
AX = mybir.AxisListType

D = 1024
KC = 8
DEPTH = 2
NH = 8
NE = 32
DE = 512
PTOT = 9232
EPS = 1e-6
SEM_ROT = 1 << 30
NDSEM = 24
NSWSEM = 64
import os as _os0
SIMSAFE = bool(_os0.environ.get("BASS_SIMSAFE"))


class Sched:
    def __init__(self, nc, es):
        self.nc = nc
        self.es = es
        self.engs = {"pe": nc.tensor, "dve": nc.vector, "act": nc.scalar, "pool": nc.gpsimd, "sp": nc.sync}
        self.semobj = []
        self.cur = {}
        self.cnt = {}
        self.waited = {e: {} for e in self.engs}
        self.lastw = {}
        self.readers = {}
        self.nsem = 0
        for e in self.engs:
            self._newsem(e)
        self.dsem = []
        self.dcnt = []
        self.qsems = {}
        for q, nq in (("sp", 20), ("act", 10), ("pool", 2)):
            self.qsems[q] = []
            for i in range(nq):
                s = es.enter_context(nc.semaphore(f"dma_{q}{i}"))
                self.semobj.append(s)
                self.dsem.append(len(self.semobj) - 1)
                self.dcnt.append(0)
                self.qsems[q].append(len(self.dsem) - 1)
        self.qnext = {q: 0 for q in self.qsems}
        self.dnext = 0
        self.ninst = 0
        self.swsems = []
        for i in range(NSWSEM):
            so = es.enter_context(nc.semaphore(f"swd{i}"))
            self.semobj.append(so)
            self.swsems.append(len(self.semobj) - 1)
        self.sw_used = 0
        self.sw_next = 0
        self.swcnt = [0] * NSWSEM
        self.prog = {e: [] for e in self.engs}

    def _newsem(self, e):
        s = self.es.enter_context(self.nc.semaphore(f"s_{e}_{self.nsem}"))
        self.nsem += 1
        self.semobj.append(s)
        self.cur[e] = len(self.semobj) - 1
        self.cnt[e] = 0

    def _deps(self, e, r, w):
        deps = {}

        def add(ev):
            if ev is None:
                return
            s, v = ev
            if deps.get(s, 0) < v:
                deps[s] = v

        for k in r:
            add(self.lastw.get(k))
        for k in w:
            add(self.lastw.get(k))
            for s, v in self.readers.get(k, {}).items():
                add((s, v))
        eng = self.engs[e]
        wt = self.waited[e]
        for s, v in deps.items():
            if e == "pe" and s == self.cur["pe"]:
                continue
            if wt.get(s, 0) >= v:
                continue
            eng.wait_ge(self.semobj[s], v)
            wt[s] = v

    def _record(self, ev, r, w):
        for k in r:
            d = self.readers.setdefault(k, {})
            if d.get(ev[0], 0) < ev[1]:
                d[ev[0]] = ev[1]
        for k in w:
            self.lastw[k] = ev
            self.readers[k] = {}

    def op(self, e, fn, r=(), w=()):
        pr = [k for k in r if k in PSUM_NAMES]
        if pr:
            r = [k for k in r if k not in PSUM_NAMES]
            w = list(w) + pr
        self._deps(e, r, w)
        if self.cnt[e] >= SEM_ROT:
            self._newsem(e)
        self.cnt[e] += 1
        fn(self.engs[e]).then_inc(self.semobj[self.cur[e]], 1)
        ev = (self.cur[e], self.cnt[e])
        self._record(ev, r, w)
        self.ninst += 1
        return ev

    def dma(self, q, fn, r=(), w=()):
        self._deps(q, r, w)
        i = self.qsems[q][self.qnext[q]]
        self.qnext[q] = (self.qnext[q] + 1) % len(self.qsems[q])
        if self.dcnt[i] > 0 and self.waited[q].get(self.dsem[i], 0) < self.dcnt[i]:
            self.engs[q].wait_ge(self.semobj[self.dsem[i]], self.dcnt[i])
            self.waited[q][self.dsem[i]] = self.dcnt[i]
        self.dcnt[i] += 16
        fn(self.engs[q]).then_inc(self.semobj[self.dsem[i]], 16)
        ev = (self.dsem[i], self.dcnt[i])
        self._record(ev, r, w)
        self.ninst += 1
        return ev

    def sync_only(self, e, r=(), w=()):
        self._deps(e, r, w)

    def swdma(self, fn, r=(), w=()):
        if SIMSAFE:
            if self.sw_used == len(self.swsems):
                self.sw_recycle()
            si = self.swsems[self.sw_used]
            self.sw_used += 1
            self._deps("pool", r, w)
            fn(self.engs["pool"]).then_inc(self.semobj[si], 16)
            ev = (si, 16)
        else:
            k = self.sw_next
            self.sw_next = (self.sw_next + 1) % 16
            si = self.swsems[k]
            self._deps("pool", r, w)
            if self.swcnt[k] > 0 and self.waited["pool"].get(si, 0) < self.swcnt[k]:
                self.engs["pool"].wait_ge(self.semobj[si], self.swcnt[k])
                self.waited["pool"][si] = self.swcnt[k]
            self.swcnt[k] += 16
            fn(self.engs["pool"]).then_inc(self.semobj[si], 16)
            ev = (si, self.swcnt[k])
        self._record(ev, r, w)
        self.ninst += 1
        return ev

    def sw_recycle(self):
        self.barrier()
        self.nc.all_engine_barrier()
        for si in self.swsems[:self.sw_used]:
            self.engs["pool"].sem_clear(self.semobj[si])
        self.nc.all_engine_barrier()
        for e in self.engs:
            for si in self.swsems:
                self.waited[e].pop(si, None)
        self.sw_used = 0

    def barrier(self):
        evs = [(self.cur[e], self.cnt[e]) for e in self.engs if self.cnt[e] > 0]
        evs += [(self.dsem[i], self.dcnt[i]) for i in range(len(self.dsem)) if self.dcnt[i] > 0]
        if SIMSAFE:
            evs += [(si, 16) for si in self.swsems[:self.sw_used]]
        else:
            evs += [(self.swsems[k], self.swcnt[k]) for k in range(16) if self.swcnt[k] > 0]
        for e in self.engs:
            wt = self.waited[e]
            for s, v in evs:
                if e == "pe" and s == self.cur["pe"]:
                    continue
                if wt.get(s, 0) >= v:
                    continue
                self.engs[e].wait_ge(self.semobj[s], v)
                wt[s] = v
        self.lastw = {}
        self.readers = {}

    def final_wait(self):
        self.barrier()

    def emit(self):
        nc = self.nc
        prog = self.prog
        with nc.Block() as block:
            @block.tensor
            def _(en):
                for f in prog["pe"]:
                    f(en)

            @block.vector
            def _(en):
                for f in prog["dve"]:
                    f(en)

            @block.scalar
            def _(en):
                for f in prog["act"]:
                    f(en)

            @block.gpsimd
            def _(en):
                for f in prog["pool"]:
                    f(en)

            @block.sync
            def _(en):
                for f in prog["sp"]:
                    f(en)


def _consts():
    c = {}
    c["ident_f"] = np.eye(128, dtype=np.float32)
    c["ident_b"] = np.eye(128, dtype=np.float32).astype(ml_dtypes.bfloat16)
    k = np.arange(128)[:, None]
    q = np.arange(128)[None, :]
    c["triu_b"] = (q >= k).astype(np.float32).astype(ml_dtypes.bfloat16)
    slopes = 2.0 ** (-8.0 * np.arange(1, NH + 1) / NH)
    tab = np.zeros((128, NH * 64), np.float32)
    for h in range(NH):
        for n in range(1, 65):
            tab[:, h * 64 + n - 1] = slopes[h] * (np.arange(128) + 1 - 128 * n)
    c["alibi"] = tab
    same = (k // 64) == (q // 64)
    c["negmaskU"] = np.where(same & (q >= k), 0.0, -30000.0).astype(np.float32)
    c["strictU"] = (same & (q > k)).astype(np.float32)
    c["Lcum"] = (same & (k <= q)).astype(np.float32)
    c["Lall"] = same.astype(np.float32)
    sel = np.zeros((16, 16 * 128), np.float32)
    for j in range(16):
        sel[j, j * 128:(j + 1) * 128] = 1.0
    c["sel16"] = sel
    c["sltU"] = (k < q).astype(np.float32)
    rm = np.zeros((128, 2), np.float32)
    rm[:64, 0] = 1.0
    rm[64:, 1] = 1.0
    c["rowmask"] = rm
    return c


CONST_SHAPES = {
    "ident_f": ([128, 128], F32), "ident_b": ([128, 128], BF16), "triu_b": ([128, 128], BF16),
    "alibi": ([128, NH * 64], F32), "negmaskU": ([128, 128], F32), "strictU": ([128, 128], F32),
    "Lcum": ([128, 128], F32), "Lall": ([128, 128], F32), "sel16": ([16, 16 * 128], F32),
    "sltU": ([128, 128], F32), "rowmask": ([128, 2], F32),
}


def declare_io(nc, S, NB):
    io = {}

    def inp(name, shape, dt=F32):
        io[name] = nc.dram_tensor(name, list(shape), dt, kind="ExternalInput").ap()

    def scr(name, shape, dt):
        io[name] = nc.dram_tensor(name, list(shape), dt, kind="Internal").ap()

    inp("x", [S, D])
    inp("c_t", [128, KC])
    inp("ada_w", [DEPTH, D, 6 * D])
    inp("ada_b_t", [DEPTH, 128, 48])
    inp("n1w_t", [DEPTH, 128, KC])
    inp("n2w_t", [DEPTH, 128, KC])
    inp("fnw_bc", [128, D])
    inp("w_in", [DEPTH, D, PTOT])
    inp("conv_t", [DEPTH, 128, 24, 4])
    inp("lamv", [DEPTH, 128, 4, 64])
    inp("subln_bc", [DEPTH, 128, 128])
    inp("alog_bc", [DEPTH, 128, NH])
    inp("dtb_bc", [DEPTH, 128, NH])
    inp("gnw_t", [DEPTH, 128, 1])
    inp("w_a", [DEPTH, D, D])
    inp("w_b", [DEPTH, D, D])
    inp("w_out", [DEPTH, D, D])
    inp("wr", [DEPTH, D, 36])
    inp("rb_bc", [DEPTH, 128, 36])
    inp("ew1", [DEPTH * NE * 128, KC * DE])
    inp("ew3", [DEPTH * NE * 128, KC * DE])
    inp("ew2", [DEPTH * NE * 128, 4 * D])
    for k, (shp, dt) in CONST_SHAPES.items():
        inp(k, shp, dt)
    io["y"] = nc.dram_tensor("y", [S, D], F32, kind="ExternalOutput").ap()
    scr("xr", [S, D], F32)
    scr("PT", [64 * 128, S], BF16)
    scr("Vtok", [S, D], BF16)
    scr("BD", [S, 16], F32)
    scr("YT", [16 * 128, S], BF16)
    inp("iota_nb", [128, NB])
    inp("iota_p", [128, 1])
    scr("h2d", [S, D], F32)
    scr("xs", [NB * 128, D], F32)
    scr("ys", [NB * 128, D], F32)
    scr("e1b", [NE, D, DE], BF16)
    scr("e3b", [NE, D, DE], BF16)
    scr("e2b", [NE, DE, D], BF16)
    scr("vecs", [DEPTH, 4, D], F32)
    return io


class Ctx:
    pass


def dbg_sb(g, name, tile_, keys):
    if name not in g.dbgset:
        return
    dst = g.nc.dram_tensor("dbg_" + name, list(tile_.shape), tile_.dtype, kind="ExternalOutput").ap()
    g.sc.dma("sp", lambda e: e.dma_start(out=dst, in_=tile_), r=keys, w=["dbg_" + name])


_UID = [0]


def _uname(name):
    _UID[0] += 1
    return f"{name}_{_UID[0]}"


def T(es, nc, name, shape, dt):
    return es.enter_context(nc.sbuf_tensor(_uname(name), list(shape), dt))


PSUM_NAMES = set()


def PS(es, nc, name, shape, dt):
    PSUM_NAMES.add(name)
    return es.enter_context(nc.psum_tensor(_uname(name), list(shape), dt))


def phase_setup(g):
    nc, sc, io, es = g.nc, g.sc, g.io, g.es
    g.cst = {}
    for k, (shp, dt) in CONST_SHAPES.items():
        t = T(es, nc, "c_" + k, shp, dt)
        g.cst[k] = t
        sc.dma("sp", lambda e, t=t, k=k: e.dma_start(out=t[:], in_=io[k]), w=["c_" + k])
    g.mod = T(es, nc, "mod", [128, DEPTH, 48], F32)
    g.A1 = T(es, nc, "A1", [128, DEPTH, KC], F32)
    g.A2 = T(es, nc, "A2", [128, DEPTH, KC], F32)
    g.ones_b = T(es, nc, "ones_b", [128, 128], BF16)
    g.ones_f = T(es, nc, "ones_f", [128, 128], F32)
    sc.op("pool", lambda e: e.memset(g.ones_b[:], 1.0), w=["ones_b"])
    sc.op("pool", lambda e: e.memset(g.ones_f[:], 1.0), w=["ones_f"])
    with ExitStack() as ls:
        ct = T(ls, nc, "ct", [128, KC], F32)
        cond = T(ls, nc, "cond", [128, KC], F32)
        adab = T(ls, nc, "adab", [128, DEPTH, 48], F32)
        nw = T(ls, nc, "nw", [128, 2, DEPTH, KC], F32)
        tmp = T(ls, nc, "tmpm", [128, DEPTH, KC], F32)
        wg = [T(ls, nc, f"wg{i}", [128, KC, 1024], F32) for i in range(2)]
        psm = PS(ls, nc, "psm", [128, DEPTH * 48], F32)
        sc.dma("sp", lambda e: e.dma_start(out=ct[:], in_=io["c_t"]), w=["ct"])
        sc.dma("sp", lambda e: e.dma_start(out=adab[:], in_=io["ada_b_t"].rearrange("l p f -> p l f")), w=["adab"])
        sc.dma("sp", lambda e: e.dma_start(out=nw[:, 0], in_=io["n1w_t"].rearrange("l p f -> p l f")), w=["nw"])
        sc.dma("sp", lambda e: e.dma_start(out=nw[:, 1], in_=io["n2w_t"].rearrange("l p f -> p l f")), w=["nw"])
        sc.op("act", lambda e: e.activation(out=cond[:], in_=ct[:], func=AF.Silu), r=["ct"], w=["cond"])
        i = 0
        for l in range(DEPTH):
            for gi in range(6):
                b = i % 2
                i += 1
                src = io["ada_w"][l].rearrange("(kc p) f -> p kc f", p=128)[:, :, gi * 1024:(gi + 1) * 1024]
                sc.dma("sp" if b == 0 else "act", lambda e, b=b, src=src: e.dma_start(out=wg[b][:], in_=src), w=[f"wg{b}"])
                for f in range(8):
                    col = l * 48 + gi * 8 + f
                    for kc in range(KC):
                        sc.op("pe", lambda e, b=b, f=f, kc=kc, col=col: e.matmul(
                            psm[:, col:col + 1], lhsT=wg[b][:, kc, f * 128:(f + 1) * 128], rhs=cond[:, kc:kc + 1],
                            start=(kc == 0), stop=(kc == KC - 1)), r=[f"wg{b}", "cond"], w=["psm"])
        sc.op("dve", lambda e: e.tensor_tensor(out=g.mod[:].rearrange("p l f -> p (l f)"), in0=psm[:],
                                               in1=adab[:].rearrange("p l f -> p (l f)"), op=ALU.add),
              r=["psm", "adab"], w=["mod"])
        sc.op("dve", lambda e: e.tensor_scalar(out=tmp[:], in0=g.mod[:, :, 8:16], scalar1=1.0, scalar2=None, op0=ALU.add),
              r=["mod"], w=["tmpm"])
        sc.op("dve", lambda e: e.tensor_tensor(out=g.A1[:], in0=tmp[:], in1=nw[:, 0], op=ALU.mult), r=["tmpm", "nw"], w=["A1"])
        sc.op("dve", lambda e: e.tensor_scalar(out=tmp[:], in0=g.mod[:, :, 32:40], scalar1=1.0, scalar2=None, op0=ALU.add),
              r=["mod", "A1"], w=["tmpm"])
        sc.op("dve", lambda e: e.tensor_tensor(out=g.A2[:], in0=tmp[:], in1=nw[:, 1], op=ALU.mult), r=["tmpm", "nw"], w=["A2"])
        with nc.allow_non_contiguous_dma(reason="tiny per-feature vectors"):
            for l in range(DEPTH):
                srcs = [g.mod[:, l, 16:24], g.A2[:, l, :], g.mod[:, l, 24:32], g.mod[:, l, 40:48]]
                for j, s_ in enumerate(srcs):
                    sc.dma("sp", lambda e, l=l, j=j, s_=s_: e.dma_start(
                        out=io["vecs"][l, j].rearrange("(kc p) -> p kc", p=128), in_=s_, allow_slow_non_contiguous=True),
                        r=["mod", "A2"], w=["vecs"])
        dbg_sb(g, "mod", g.mod[:], ["mod"])
        dbg_sb(g, "A1", g.A1[:], ["A1"])
        sc.barrier()


def load_bc(g, tile_, l, j, key):
    g.sc.dma("sp", lambda e: e.dma_start(out=tile_[:], in_=g.io["vecs"][l, j].partition_broadcast(128)), r=["vecs"], w=[key])


def phase_norm(g, l, xsrc, hT, Acol, Bcol, out_dt_tag="hT"):
    nc, sc, io = g.nc, g.sc, g.io
    NT = g.S // 128
    with ExitStack() as ls:
        if g.S < 2048:
            pad_ = T(ls, nc, "npad", [128, 16384], F32)
        xt = [T(ls, nc, f"nx{i}", [128, D], F32) for i in range(2)]
        xn = [T(ls, nc, f"nxn{i}", [128, D], F32) for i in range(2)]
        junk = T(ls, nc, "njunk", [128, D], BF16)
        st = [T(ls, nc, f"nst{i}", [128, 64], F32) for i in range(2)]
        pst = [PS(ls, nc, f"npst{i}", [128, 512], F32) for i in range(4)]
        for i in range(NT):
            b = i % 2
            sc.dma("sp", lambda e, b=b, i=i: e.dma_start(out=xt[b][:], in_=xsrc[i * 128:(i + 1) * 128, :]), w=[f"nx{b}"])
            sc.op("act", lambda e, b=b: e.activation(out=junk[:], in_=xt[b][:], func=AF.Square, accum_out=st[b][:, 0:1]),
                  r=[f"nx{b}"], w=["njunk", f"nst{b}"])
            sc.op("dve", lambda e, b=b: e.tensor_scalar(out=st[b][:, 16:17], in0=st[b][:, 0:1], scalar1=1.0 / D, scalar2=EPS,
                                                         op0=ALU.mult, op1=ALU.add), r=[f"nst{b}"], w=[f"nst{b}"])
            sc.op("act", lambda e, b=b: e.activation(out=st[b][:, 32:33], in_=st[b][:, 16:17], func=AF.Sqrt), r=[f"nst{b}"], w=[f"nst{b}"])
            sc.op("dve", lambda e, b=b: e.reciprocal(out=st[b][:, 48:49], in_=st[b][:, 32:33]), r=[f"nst{b}"], w=[f"nst{b}"])
            sc.op("dve", lambda e, b=b: e.tensor_scalar(out=xn[b][:], in0=xt[b][:], scalar1=st[b][:, 48:49], scalar2=None, op0=ALU.mult),
                  r=[f"nx{b}", f"nst{b}"], w=[f"nxn{b}"])
            for kc in range(KC):
                pb = (i % 2) * 2 + kc // 4
                sc.op("pe", lambda e, b=b, kc=kc, pb=pb: e.transpose(pst[pb][:, (kc % 4) * 128:(kc % 4 + 1) * 128],
                                                                     xn[b][:, kc * 128:(kc + 1) * 128], g.cst["ident_f"][:]),
                      r=[f"nxn{b}", "c_ident_f"], w=[f"npst{pb}"])
            for kc in range(KC):
                pb = (i % 2) * 2 + kc // 4
                sc.op("act", lambda e, kc=kc, pb=pb, i=i: e.activation(
                    out=hT[:, kc, i * 128:(i + 1) * 128], in_=pst[pb][:, (kc % 4) * 128:(kc % 4 + 1) * 128],
                    func=AF.Identity, bias=Bcol[:, kc:kc + 1], scale=Acol[:, kc:kc + 1]),
                    r=[f"npst{pb}", "mod", "A1", "A2"], w=[f"{out_dt_tag}{i}"])


FM_BLOCKS = []
for _c in range(0, 1024, 512):
    FM_BLOCKS.append((_c, _c // 128, False))
for _c in range(0, 1024, 512):
    FM_BLOCKS.append((1024 + _c, 8 + _c // 128, False))
for _c in range(0, 3072, 512):
    FM_BLOCKS.append((3072 + _c, 16 + _c // 128, False))
for _c in range(0, 1024, 512):
    FM_BLOCKS.append((6144 + _c, 40 + _c // 128, False))
for _c in range(0, 2048, 512):
    FM_BLOCKS.append((7184 + _c, 48 + _c // 128, True))


def phase_proj(g, l, hT):
    nc, sc, io = g.nc, g.sc, g.io
    S = g.S
    NTC = S // 512
    NT = S // 128
    winl = io["w_in"][l].rearrange("(kc p) f -> p kc f", p=128)
    PTv = io["PT"].rearrange("(c p) t -> p c t", p=128)
    with ExitStack() as ls:
        wblk = [T(ls, nc, f"wblk{i}", [128, KC, 512], BF16) for i in range(2)]
        wst = [T(ls, nc, f"wst{i}", [128, KC, 512], F32) for i in range(2)]
        stg = [T(ls, nc, f"pstg{i}", [128, 4, 512], BF16) for i in range(2)]
        pp = [PS(ls, nc, f"pp{i}", [128, 512], F32) for i in range(8)]
        hkeys = lambda tc: [f"hT{i}" for i in range(tc * 4, tc * 4 + 4)]
        n = 0
        for bi, (c0, ch0, sig) in enumerate(FM_BLOCKS):
            wb = bi % 2
            sc.dma("sp", lambda e, wb=wb, c0=c0: e.dma_start(out=wst[wb][:], in_=winl[:, :, c0:c0 + 512]), w=[f"wst{wb}"])
            sc.op("pool", lambda e, wb=wb: e.tensor_copy(out=wblk[wb][:], in_=wst[wb][:]), r=[f"wst{wb}"], w=[f"wblk{wb}"])
            for tc in range(NTC):
                sb = n % 2
                for fc in range(4):
                    pb = (n % 2) * 4 + fc
                    for kc in range(KC):
                        sc.op("pe", lambda e, wb=wb, fc=fc, kc=kc, pb=pb, tc=tc: e.matmul(
                            pp[pb][:], lhsT=wblk[wb][:, kc, fc * 128:(fc + 1) * 128], rhs=hT[:, kc, tc * 512:(tc + 1) * 512],
                            start=(kc == 0), stop=(kc == KC - 1)), r=[f"wblk{wb}"] + hkeys(tc), w=[f"pp{pb}"])
                    if sig:
                        sc.op("act", lambda e, sb=sb, fc=fc, pb=pb: e.activation(out=stg[sb][:, fc, :], in_=pp[pb][:], func=AF.Sigmoid),
                              r=[f"pp{pb}"], w=[f"pstg{sb}"])
                    elif fc % 2 == 0:
                        sc.op("act", lambda e, sb=sb, fc=fc, pb=pb: e.activation(out=stg[sb][:, fc, :], in_=pp[pb][:], func=AF.Copy),
                              r=[f"pp{pb}"], w=[f"pstg{sb}"])
                    else:
                        sc.op("dve", lambda e, sb=sb, fc=fc, pb=pb: e.tensor_copy(out=stg[sb][:, fc, :], in_=pp[pb][:]),
                              r=[f"pp{pb}"], w=[f"pstg{sb}"])
                sc.dma("sp", lambda e, sb=sb, ch0=ch0, tc=tc: e.dma_start(
                    out=PTv[:, ch0:ch0 + 4, tc * 512:(tc + 1) * 512], in_=stg[sb][:]), r=[f"pstg{sb}"], w=["PT"])
                n += 1
    sc.barrier()
    with ExitStack() as ls:
        wv = T(ls, nc, "wv", [128, KC, 1024], BF16)
        wbd = T(ls, nc, "wbd", [128, KC, 16], BF16)
        vst = [T(ls, nc, f"vst{i}", [128, 1024], BF16) for i in range(2)]
        bst = [T(ls, nc, f"bst{i}", [128, 16], F32) for i in range(2)]
        pv = [PS(ls, nc, f"pv{i}", [128, 512], F32) for i in range(6)]
        wst2 = [T(ls, nc, f"wst2{i}", [128, KC, 512], F32) for i in range(2)]
        wbdf = T(ls, nc, "wbdf", [128, KC, 16], F32)
        for hf in range(2):
            sc.dma("sp", lambda e, hf=hf: e.dma_start(out=wst2[hf][:], in_=winl[:, :, 2048 + hf * 512:2048 + (hf + 1) * 512]), w=[f"wst2{hf}"])
            sc.op("pool", lambda e, hf=hf: e.tensor_copy(out=wv[:, :, hf * 512:(hf + 1) * 512], in_=wst2[hf][:]), r=[f"wst2{hf}"], w=["wv"])
        sc.dma("sp", lambda e: e.dma_start(out=wbdf[:], in_=winl[:, :, 7168:7184], allow_slow_non_contiguous=True), w=["wbdf"])
        sc.op("pool", lambda e: e.tensor_copy(out=wbd[:], in_=wbdf[:]), r=["wbdf"], w=["wbd"])
        for i in range(NT):
            b = i % 2
            for hf in range(2):
                pb = b * 3 + hf
                for kc in range(KC):
                    sc.op("pe", lambda e, kc=kc, hf=hf, pb=pb, i=i: e.matmul(
                        pv[pb][:], lhsT=hT[:, kc, i * 128:(i + 1) * 128], rhs=wv[:, kc, hf * 512:(hf + 1) * 512],
                        start=(kc == 0), stop=(kc == KC - 1)), r=["wv", f"hT{i}"], w=[f"pv{pb}"])
            pb2 = b * 3 + 2
            for kc in range(KC):
                sc.op("pe", lambda e, kc=kc, pb2=pb2, i=i: e.matmul(
                    pv[pb2][:, 0:16], lhsT=hT[:, kc, i * 128:(i + 1) * 128], rhs=wbd[:, kc, :],
                    start=(kc == 0), stop=(kc == KC - 1)), r=["wbd", f"hT{i}"], w=[f"pv{pb2}"])
            sc.op("act", lambda e, b=b: e.activation(out=vst[b][:, 0:512], in_=pv[b * 3][:], func=AF.Copy), r=[f"pv{b*3}"], w=[f"vst{b}"])
            sc.op("dve", lambda e, b=b: e.tensor_copy(out=vst[b][:, 512:1024], in_=pv[b * 3 + 1][:]), r=[f"pv{b*3+1}"], w=[f"vst{b}"])
            sc.op("dve", lambda e, b=b, pb2=pb2: e.tensor_copy(out=bst[b][:], in_=pv[pb2][:, 0:16]), r=[f"pv{pb2}"], w=[f"bst{b}"])
            sc.dma("sp", lambda e, b=b, i=i: e.dma_start(out=io["Vtok"][i * 128:(i + 1) * 128, :], in_=vst[b][:]), r=[f"vst{b}"], w=["Vtok"])
            sc.dma("sp", lambda e, b=b, i=i: e.dma_start(out=io["BD"][i * 128:(i + 1) * 128, :], in_=bst[b][:]), r=[f"bst{b}"], w=["BD"])


GRP = [128, 256, 512, 512, 512, 512, 512, 512]


def phase_attn(g, l):
    nc, sc, io = g.nc, g.sc, g.io
    S = g.S
    NT = S // 128
    NQC = S // 512
    lam_init = 0.8 - 0.6 * math.exp(-0.3 * l)
    Vv = io["Vtok"].rearrange("(i p) f -> p i f", p=128)
    with ExitStack() as ls:
        QT = [T(ls, nc, f"aQT{i}", [128, S], BF16) for i in range(2)]
        KT = [T(ls, nc, f"aKT{i}", [128, S], BF16) for i in range(2)]
        VT = [T(ls, nc, f"aVT{i}", [128, NT, 129], BF16) for i in range(2)]
        pT = [T(ls, nc, f"apT{i}", [128, 512], BF16) for i in range(3)]
        O1 = [T(ls, nc, f"aO1{j}", [128, 128], F32) for j in range(4)]
        Ot = [T(ls, nc, f"aO{j}", [128, 128], F32) for j in range(4)]
        yb = [T(ls, nc, f"ayb{j}", [128, 128], BF16) for j in range(4)]
        sq = T(ls, nc, "asq", [128, 128], BF16)
        stt = [T(ls, nc, f"ast{j}", [128, 128], F32) for j in range(4)]
        yst = [T(ls, nc, f"ayst{i}", [128, 512], BF16) for i in range(2)]
        lamt = T(ls, nc, "alam", [128, 4, 64], F32)
        lamp = T(ls, nc, "alamp", [128, 2, 64], F32)
        lams = T(ls, nc, "alams", [128, 128], F32)
        subw = T(ls, nc, "asubw", [128, 128], F32)
        ps_s = [PS(ls, nc, f"aps{i}", [128, 512], F32) for i in range(2)]
        ps_o = [PS(ls, nc, f"apo{j}", [128, 512], F32) for j in range(4)]
        ps_t = PS(ls, nc, "apt", [128, 1024], BF16)
        sc.dma("sp", lambda e: e.dma_start(out=lamt[:], in_=io["lamv"][l]), w=["alam"])
        sc.dma("sp", lambda e: e.dma_start(out=subw[:], in_=io["subln_bc"][l]), w=["asubw"])
        sc.op("dve", lambda e: e.tensor_tensor(out=lamp[:, 0, :], in0=lamt[:, 0, :], in1=lamt[:, 1, :], op=ALU.mult), r=["alam"], w=["alamp"])
        sc.op("dve", lambda e: e.tensor_tensor(out=lamp[:, 1, :], in0=lamt[:, 2, :], in1=lamt[:, 3, :], op=ALU.mult), r=["alam"], w=["alamp"])
        sc.op("dve", lambda e: e.tensor_reduce(out=lams[:, 0:1], in_=lamp[:, 0, :], axis=AX.X, op=ALU.add), r=["alamp"], w=["alams"])
        sc.op("dve", lambda e: e.tensor_reduce(out=lams[:, 16:17], in_=lamp[:, 1, :], axis=AX.X, op=ALU.add), r=["alamp"], w=["alams"])
        sc.op("act", lambda e: e.activation(out=lams[:, 32:33], in_=lams[:, 0:1], func=AF.Exp), r=["alams"], w=["alams"])
        sc.op("act", lambda e: e.activation(out=lams[:, 48:49], in_=lams[:, 16:17], func=AF.Exp), r=["alams"], w=["alams"])
        sc.op("dve", lambda e: e.tensor_tensor(out=lams[:, 64:65], in0=lams[:, 48:49], in1=lams[:, 32:33], op=ALU.subtract), r=["alams"], w=["alams"])
        sc.op("dve", lambda e: e.tensor_scalar(out=lams[:, 80:81], in0=lams[:, 64:65], scalar1=-lam_init, scalar2=None, op0=ALU.add),
              r=["alams"], w=["alams"])
        neglam = lams[:, 80:81]
        sc.op("dve", lambda e: e.tensor_scalar(out=subw[:], in0=subw[:], scalar1=(1.0 - lam_init), scalar2=None, op0=ALU.mult),
              r=["asubw"], w=["asubw"])
        for hb in range(2):
            sc.op("pool", lambda e, hb=hb: e.memset(VT[hb][:, :, 128:129], 1.0), w=[f"aVT{hb}"])
        n_u = 0
        n_y = 0
        for h in range(NH):
            hb = h % 2
            sc.dma("sp", lambda e, hb=hb, h=h: e.dma_start(out=QT[hb][:], in_=io["PT"][h * 128:(h + 1) * 128, :]), r=["PT"], w=[f"aQT{hb}"])
            sc.dma("sp", lambda e, hb=hb, h=h: e.dma_start(out=KT[hb][:], in_=io["PT"][(8 + h) * 128:(9 + h) * 128, :]), r=["PT"], w=[f"aKT{hb}"])
            sc.dma("sp", lambda e, hb=hb, h=h: e.dma_start(out=VT[hb][:, :, 0:128], in_=Vv[:, :, h * 128:(h + 1) * 128]), r=["Vtok"], w=[f"aVT{hb}"])
            G = GRP[h]
            for qc in range(NQC):
                for m in range(2):
                    nkb = 4 * qc + 4
                    for kb in range(nkb):
                        kl = kb - 4 * qc
                        j0 = max(0, kl)
                        sb = n_u % 2
                        pb = n_u % 3
                        n_u += 1
                        c0 = j0 * 128
                        sc.op("pe", lambda e, hb=hb, m=m, kb=kb, qc=qc, sb=sb, c0=c0: e.matmul(
                            ps_s[sb][:, c0:512], lhsT=KT[hb][m * 64:(m + 1) * 64, kb * 128:(kb + 1) * 128],
                            rhs=QT[hb][m * 64:(m + 1) * 64, qc * 512 + c0:qc * 512 + 512], start=True, stop=True),
                            r=[f"aKT{hb}", f"aQT{hb}"], w=[f"aps{sb}"])
                        cs = c0
                        while cs < 512:
                            ce = min(512, (cs // G + 1) * G)
                            nn = (512 * qc + ce - 128 * kb) // 128
                            col = h * 64 + nn - 1
                            sc.op("act", lambda e, sb=sb, pb=pb, cs=cs, ce=ce, col=col: e.activation(
                                out=pT[pb][:, cs:ce], in_=ps_s[sb][:, cs:ce], func=AF.Exp,
                                bias=g.cst["alibi"][:, col:col + 1], scale=0.125), r=[f"aps{sb}", "c_alibi"], w=[f"apT{pb}"])
                            cs = ce
                        if kl >= 0:
                            sc.op("pool", lambda e, pb=pb, kl=kl: e.tensor_tensor(
                                out=pT[pb][:, kl * 128:(kl + 1) * 128], in0=pT[pb][:, kl * 128:(kl + 1) * 128],
                                in1=g.cst["triu_b"][:], op=ALU.mult), r=[f"apT{pb}", "c_triu_b"], w=[f"apT{pb}"])
                        for j in range(j0, 4):
                            sc.op("pe", lambda e, pb=pb, j=j, hb=hb, kb=kb, qc=qc: e.matmul(
                                ps_o[j][:, 0:129], lhsT=pT[pb][:, j * 128:(j + 1) * 128], rhs=VT[hb][:, kb, :],
                                start=(kb == 0), stop=(kb == 4 * qc + j)), r=[f"apT{pb}", f"aVT{hb}"], w=[f"apo{j}"])
                    for j in range(4):
                        st_ = stt[j]
                        sc.op("dve", lambda e, j=j, st_=st_: e.reciprocal(out=st_[:, 0:1], in_=ps_o[j][:, 128:129]), r=[f"apo{j}"], w=[f"ast{j}"])
                        if m == 0:
                            sc.op("dve", lambda e, j=j, st_=st_: e.tensor_scalar(out=O1[j][:], in0=ps_o[j][:, 0:128], scalar1=st_[:, 0:1],
                                                                                scalar2=None, op0=ALU.mult), r=[f"apo{j}", f"ast{j}"], w=[f"aO1{j}"])
                        else:
                            sc.op("dve", lambda e, st_=st_: e.tensor_tensor(out=st_[:, 16:17], in0=st_[:, 0:1], in1=neglam, op=ALU.mult),
                                  r=[f"ast{j}", "alams"], w=[f"ast{j}"])
                            sc.op("dve", lambda e, j=j, st_=st_: e.scalar_tensor_tensor(
                                out=Ot[j][:], in0=ps_o[j][:, 0:128], scalar=st_[:, 16:17], in1=O1[j][:], op0=ALU.mult, op1=ALU.add),
                                r=[f"apo{j}", f"ast{j}", f"aO1{j}"], w=[f"aO{j}"])
                            sc.op("act", lambda e, j=j, st_=st_: e.activation(out=sq[:], in_=Ot[j][:], func=AF.Square, accum_out=st_[:, 32:33]),
                                  r=[f"aO{j}"], w=["asq", f"ast{j}"])
                            sc.op("dve", lambda e, st_=st_: e.tensor_scalar(out=st_[:, 48:49], in0=st_[:, 32:33], scalar1=1.0 / 128, scalar2=EPS,
                                                                         op0=ALU.mult, op1=ALU.add), r=[f"ast{j}"], w=[f"ast{j}"])
                            sc.op("act", lambda e, st_=st_: e.activation(out=st_[:, 64:65], in_=st_[:, 48:49], func=AF.Ln), r=[f"ast{j}"], w=[f"ast{j}"])
                            sc.op("act", lambda e, st_=st_: e.activation(out=st_[:, 80:81], in_=st_[:, 64:65], func=AF.Exp, scale=-0.5),
                                  r=[f"ast{j}"], w=[f"ast{j}"])
                            sc.op("dve", lambda e, j=j, st_=st_: e.scalar_tensor_tensor(
                                out=yb[j][:], in0=Ot[j][:], scalar=st_[:, 80:81], in1=subw[:], op0=ALU.mult, op1=ALU.mult),
                                r=[f"aO{j}", f"ast{j}", "asubw"], w=[f"ayb{j}"])
                            sc.op("pe", lambda e, j=j: e.transpose(ps_t[:, j * 128:(j + 1) * 128], yb[j][:], g.cst["ident_b"][:]),
                                  r=[f"ayb{j}", "c_ident_b"], w=["apt"])
                    if m == 1:
                        ysb = n_y % 2
                        n_y += 1
                        sc.op("dve", lambda e, ysb=ysb: e.tensor_copy(out=yst[ysb][:], in_=ps_t[:, 0:512]), r=["apt"], w=[f"ayst{ysb}"])
                        sc.dma("sp", lambda e, ysb=ysb, h=h, qc=qc: e.dma_start(
                            out=io["YT"][h * 128:(h + 1) * 128, qc * 512:(qc + 1) * 512], in_=yst[ysb][:]), r=[f"ayst{ysb}"], w=["YT"])


def phase_gdn(g, l):
    nc, sc, io = g.nc, g.sc, g.io
    S = g.S
    NT = S // 128
    CW = min(2048, S)
    NCW = S // CW
    cst = g.cst
    with ExitStack() as ls:
        SC = T(ls, nc, "gSC", [128, NT, 5, 8], F32)
        nega = T(ls, nc, "gnega", [128, 8], F32)
        dtb = T(ls, nc, "gdtb", [128, 8], F32)
        convw = T(ls, nc, "gconvw", [128, 24, 4], F32)
        gnw = T(ls, nc, "ggnw", [128, 1], F32)
        sc.dma("sp", lambda e: e.dma_start(out=nega[:], in_=io["alog_bc"][l]), w=["gnega"])
        sc.dma("sp", lambda e: e.dma_start(out=dtb[:], in_=io["dtb_bc"][l]), w=["gdtb"])
        sc.dma("sp", lambda e: e.dma_start(out=convw[:], in_=io["conv_t"][l]), w=["gconvw"])
        sc.dma("sp", lambda e: e.dma_start(out=gnw[:], in_=io["gnw_t"][l]), w=["ggnw"])
        sc.op("act", lambda e: e.activation(out=nega[:], in_=nega[:], func=AF.Exp), r=["gnega"], w=["gnega"])
        sc.op("dve", lambda e: e.tensor_scalar(out=nega[:], in0=nega[:], scalar1=-1.0, scalar2=None, op0=ALU.mult), r=["gnega"], w=["gnega"])
        with ExitStack() as la:
            bd = [T(la, nc, f"gbd{i}", [128, 16], F32) for i in range(2)]
            tg = [T(la, nc, f"gtg{i}", [128, 4, 8], F32) for i in range(2)]
            psa = [PS(la, nc, f"gpsa{i}", [128, 512], F32) for i in range(2)]
            for i in range(NT):
                b = i % 2
                sc.dma("sp", lambda e, b=b, i=i: e.dma_start(out=bd[b][:], in_=io["BD"][i * 128:(i + 1) * 128, :]), r=["BD"], w=[f"gbd{b}"])
                sc.op("act", lambda e, b=b, i=i: e.activation(out=SC[:, i, 2, :], in_=bd[b][:, 0:8], func=AF.Sigmoid), r=[f"gbd{b}"], w=[f"gSC{i}"])
                sc.op("dve", lambda e, b=b: e.tensor_tensor(out=tg[b][:, 0, :], in0=bd[b][:, 8:16], in1=dtb[:], op=ALU.add), r=[f"gbd{b}", "gdtb"], w=[f"gtg{b}"])
                sc.op("act", lambda e, b=b: e.activation(out=tg[b][:, 1, :], in_=tg[b][:, 0, :], func=AF.Exp), r=[f"gtg{b}"], w=[f"gtg{b}"])
                sc.op("dve", lambda e, b=b: e.tensor_scalar(out=tg[b][:, 1, :], in0=tg[b][:, 1, :], scalar1=1.0, scalar2=None, op0=ALU.add), r=[f"gtg{b}"], w=[f"gtg{b}"])
                sc.op("act", lambda e, b=b: e.activation(out=tg[b][:, 2, :], in_=tg[b][:, 1, :], func=AF.Ln), r=[f"gtg{b}"], w=[f"gtg{b}"])
                sc.op("dve", lambda e, b=b: e.tensor_tensor(out=tg[b][:, 3, :], in0=tg[b][:, 2, :], in1=nega[:], op=ALU.mult), r=[f"gtg{b}", "gnega"], w=[f"gtg{b}"])
                sc.op("pe", lambda e, b=b: e.matmul(psa[b][:, 0:8], lhsT=cst["Lcum"][:], rhs=tg[b][:, 3, :], start=True, stop=True),
                      r=[f"gtg{b}", "c_Lcum"], w=[f"gpsa{b}"])
                sc.op("pe", lambda e, b=b: e.matmul(psa[b][:, 8:16], lhsT=cst["Lall"][:], rhs=tg[b][:, 3, :], start=True, stop=True),
                      r=[f"gtg{b}", "c_Lall"], w=[f"gpsa{b}"])
                sc.op("dve", lambda e, b=b, i=i: e.tensor_copy(out=SC[:, i, 0, :], in_=psa[b][:, 0:8]), r=[f"gpsa{b}"], w=[f"gSC{i}"])
                sc.op("dve", lambda e, b=b, i=i: e.tensor_scalar(out=SC[:, i, 1, :], in0=psa[b][:, 0:8], scalar1=-1.0, scalar2=None, op0=ALU.mult),
                      r=[f"gpsa{b}"], w=[f"gSC{i}"])
                sc.op("act", lambda e, b=b, i=i: e.activation(out=SC[:, i, 3, :], in_=psa[b][:, 0:8], func=AF.Exp), r=[f"gpsa{b}"], w=[f"gSC{i}"])
                sc.op("dve", lambda e, i=i: e.tensor_tensor(out=SC[:, i, 3, :], in0=SC[:, i, 3, :], in1=SC[:, i, 2, :], op=ALU.mult), r=[f"gSC{i}"], w=[f"gSC{i}"])
                sc.op("dve", lambda e, b=b, i=i: e.tensor_tensor(out=SC[:, i, 4, :], in0=psa[b][:, 8:16], in1=SC[:, i, 0, :], op=ALU.subtract),
                      r=[f"gpsa{b}", f"gSC{i}"], w=[f"gSC{i}"])
                sc.op("act", lambda e, i=i: e.activation(out=SC[:, i, 4, :], in_=SC[:, i, 4, :], func=AF.Exp), r=[f"gSC{i}"], w=[f"gSC{i}"])
        sc.barrier()
        if getattr(g, "gdn_stop", None) == "A":
            return
        raw = T(ls, nc, "graw", [128, S + 3], BF16)
        QTn = T(ls, nc, "gQTn", [128, S], BF16)
        KTn = T(ls, nc, "gKTn", [128, S], BF16)
        Ktok = T(ls, nc, "gKtok", [128, NT, 128], BF16)
        Vtk = T(ls, nc, "gVtk", [128, NT, 128], BF16)
        zsT = T(ls, nc, "gzsT", [128, S], BF16)
        acc = T(ls, nc, "gacc", [128, CW], F32)
        yv = T(ls, nc, "gyv", [128, CW], F32)
        ybf = T(ls, nc, "gybf", [128, CW], BF16)
        sqb = T(ls, nc, "gsqb", [128, 512], BF16)
        rnt = T(ls, nc, "grnt", [128, 512], F32)
        Sf = T(ls, nc, "gSf", [128, 128], F32)
        Sb = T(ls, nc, "gSb", [128, 128], BF16)
        names = ["dg", "db", "E", "decT", "eG", "Bst", "t1", "u"]
        f32t = {n: T(ls, nc, "g_" + n, [128, 128], F32) for n in names}
        bnames = ["AT", "A", "ATn", "An", "TT", "intraT", "vb", "kbg", "kdec", "qgT", "wT", "vn", "sq2", "yo"]
        bft = {n: T(ls, nc, "g_" + n, [128, 128], BF16) for n in bnames}
        rn2 = T(ls, nc, "g_rn2", [128, 128], F32)
        t2 = T(ls, nc, "g_t2", [128, 128], F32)
        ystg = [T(ls, nc, f"gystg{i}", [128, 512], BF16) for i in range(2)]
        pG = PS(ls, nc, "gpG", [128, 512], F32)
        pK = PS(ls, nc, "gpK", [128, 512], F32)
        pP = PS(ls, nc, "gpP", [128, 512], F32)
        pTu = PS(ls, nc, "gpTu", [128, 512], F32)
        pU = PS(ls, nc, "gpU", [128, 512], F32)
        pV = PS(ls, nc, "gpV", [128, 512], F32)
        pO = PS(ls, nc, "gpO", [128, 512], F32)
        pB = PS(ls, nc, "gpB", [128, 1024], BF16)
        sc.op("pool", lambda e: e.memset(raw[:, 0:3], 0.0), w=["graw"])
        vnc = [T(ls, nc, f"g_vnc{i}", [128, 128], BF16) for i in range(2)]
        for i_ in range(2):
            sc.op("pool", lambda e, i_=i_: e.memset(vnc[i_][:], 0.0), w=[f"g_vn{i_}"])
        n_st = 0
        for h in range(NH):
            for which in range(3):
                chunk = 16 + which * 8 + h
                sc.dma("sp", lambda e, chunk=chunk: e.dma_start(out=raw[:, 3:3 + S], in_=io["PT"][chunk * 128:(chunk + 1) * 128, :]), r=["PT"], w=["graw"])
                cc = which * 8 + h
                for cw in range(NCW):
                    c0 = cw * CW
                    sc.op("dve", lambda e, c0=c0, cc=cc: e.tensor_scalar(out=acc[:], in0=raw[:, c0:c0 + CW], scalar1=convw[:, cc, 0:1], scalar2=None,
                                                                      op0=ALU.mult), r=["graw", "gconvw"], w=["gacc"])
                    for k in range(1, 4):
                        sc.op("dve", lambda e, c0=c0, cc=cc, k=k: e.scalar_tensor_tensor(
                            out=acc[:], in0=raw[:, c0 + k:c0 + k + CW], scalar=convw[:, cc, k:k + 1], in1=acc[:], op0=ALU.mult, op1=ALU.add),
                            r=["graw", "gconvw", "gacc"], w=["gacc"])
                    if which == 2:
                        sc.op("act", lambda e: e.activation(out=ybf[:], in_=acc[:], func=AF.Silu), r=["gacc"], w=["gybf"])
                        for tt in range(CW // 128):
                            ti = c0 // 128 + tt
                            sc.op("pe", lambda e, tt=tt: e.transpose(pB[:, (tt % 4) * 128:(tt % 4 + 1) * 128], ybf[:, tt * 128:(tt + 1) * 128], cst["ident_b"][:]),
                                  r=["gybf", "c_ident_b"], w=["gpB"])
                            sc.op("act", lambda e, tt=tt, ti=ti: e.activation(out=Vtk[:, ti, :], in_=pB[:, (tt % 4) * 128:(tt % 4 + 1) * 128], func=AF.Copy),
                                  r=["gpB"], w=["gVtk"])
                    else:
                        dst = QTn if which == 0 else KTn
                        dkey = "gQTn" if which == 0 else "gKTn"
                        sc.op("act", lambda e: e.activation(out=yv[:], in_=acc[:], func=AF.Silu), r=["gacc"], w=["gyv"])
                        for sbk in range(CW // 512):
                            cs = sbk * 512
                            sc.op("pool", lambda e, cs=cs: e.tensor_tensor(out=sqb[:], in0=yv[:, cs:cs + 512], in1=yv[:, cs:cs + 512], op=ALU.mult),
                                  r=["gyv"], w=["gsqb"])
                            sc.op("pe", lambda e: e.matmul(pTu[:, 0:512], lhsT=g.ones_b[:], rhs=sqb[:], start=True, stop=True), r=["gsqb", "ones_b"], w=["gpTu"])
                            sc.op("dve", lambda e: e.tensor_scalar(out=rnt[:], in0=pTu[:, 0:512], scalar1=EPS, scalar2=None, op0=ALU.add), r=["gpTu"], w=["grnt"])
                            sc.op("act", lambda e: e.activation(out=rnt[:], in_=rnt[:], func=AF.Ln), r=["grnt"], w=["grnt"])
                            sc.op("act", lambda e: e.activation(out=rnt[:], in_=rnt[:], func=AF.Exp, scale=-0.5), r=["grnt"], w=["grnt"])
                            qs = (128.0 ** -0.5) if which == 0 else 1.0
                            sc.op("dve", lambda e, cs=cs, c0=c0, dst=dst, qs=qs: e.scalar_tensor_tensor(
                                out=dst[:, c0 + cs:c0 + cs + 512], in0=yv[:, cs:cs + 512], scalar=qs, in1=rnt[:], op0=ALU.mult, op1=ALU.mult),
                                r=["gyv", "grnt"], w=[dkey])
                        if which == 1:
                            for tt in range(CW // 128):
                                ti = c0 // 128 + tt
                                sc.op("pe", lambda e, tt=tt, ti=ti: e.transpose(pB[:, (tt % 4) * 128:(tt % 4 + 1) * 128], KTn[:, ti * 128:(ti + 1) * 128], cst["ident_b"][:]),
                                      r=["gKTn", "c_ident_b"], w=["gpB"])
                                sc.op("act", lambda e, tt=tt, ti=ti: e.activation(out=Ktok[:, ti, :], in_=pB[:, (tt % 4) * 128:(tt % 4 + 1) * 128], func=AF.Copy),
                                      r=["gpB"], w=["gKtok"])
            zc = 40 + h
            sc.dma("sp", lambda e, zc=zc: e.dma_start(out=raw[:, 3:3 + S], in_=io["PT"][zc * 128:(zc + 1) * 128, :]), r=["PT"], w=["graw"])
            sc.op("act", lambda e: e.activation(out=zsT[:], in_=raw[:, 3:3 + S], func=AF.Silu), r=["graw"], w=["gzsT"])
            sc.op("pool", lambda e: e.memset(Sf[:], 0.0), w=["gSf"])
            sc.op("pool", lambda e: e.memset(Sb[:], 0.0), w=["gSb"])
            if getattr(g, "gdn_stop", None) == "B":
                return
            F = f32t
            Bt = bft
            for i in range(NT):
                tsl = slice(i * 128, (i + 1) * 128)
                sck = [f"gSC{i}"]
                gc_col = SC[:, i, 0, h:h + 1]
                ngc_col = SC[:, i, 1, h:h + 1]
                be_col = SC[:, i, 2, h:h + 1]
                bege_col = SC[:, i, 3, h:h + 1]
                kd_col = SC[:, i, 4, h:h + 1]
                sc.op("dve", lambda e: e.tensor_scalar(out=F["dg"][:], in0=cst["ident_f"][:], scalar1=gc_col, scalar2=None, op0=ALU.mult), r=sck + ["c_ident_f"], w=["g_dg"])
                sc.op("dve", lambda e: e.tensor_scalar(out=F["db"][:], in0=cst["ident_f"][:], scalar1=be_col, scalar2=None, op0=ALU.mult), r=sck + ["c_ident_f"], w=["g_db"])
                sc.op("pe", lambda e: e.matmul(pG[:, 0:128], lhsT=g.ones_f[:], rhs=F["dg"][:], start=True, stop=True), r=["g_dg", "ones_f"], w=["gpG"])
                sc.op("pe", lambda e: e.matmul(pG[:, 128:256], lhsT=g.ones_f[:], rhs=F["db"][:], start=True, stop=True), r=["g_db", "ones_f"], w=["gpG"])
                sc.op("dve", lambda e: e.tensor_tensor(out=F["E"][:], in0=pG[:, 0:128], in1=cst["negmaskU"][:], op=ALU.add), r=["gpG", "c_negmaskU"], w=["g_E"])
                sc.op("act", lambda e: e.activation(out=F["decT"][:], in_=F["E"][:], func=AF.Exp, bias=ngc_col, scale=1.0), r=["g_E"] + sck, w=["g_decT"])
                sc.op("act", lambda e: e.activation(out=F["eG"][:], in_=pG[:, 0:128], func=AF.Exp), r=["gpG"], w=["g_eG"])
                sc.op("dve", lambda e: e.tensor_tensor(out=F["Bst"][:], in0=pG[:, 128:256], in1=cst["strictU"][:], op=ALU.mult), r=["gpG", "c_strictU"], w=["g_Bst"])
                if g.gdn_stop == "C1":
                    return
                sc.op("pe", lambda e, tsl=tsl: e.matmul(pK[:, 0:128], lhsT=KTn[:, tsl], rhs=KTn[:, tsl], start=True, stop=True), r=["gKTn"], w=["gpK"])
                sc.op("pe", lambda e, tsl=tsl: e.matmul(pK[:, 128:256], lhsT=KTn[:, tsl], rhs=QTn[:, tsl], start=True, stop=True), r=["gKTn", "gQTn"], w=["gpK"])
                sc.op("dve", lambda e: e.tensor_tensor(out=F["t1"][:], in0=pK[:, 0:128], in1=F["decT"][:], op=ALU.mult), r=["gpK", "g_decT"], w=["g_t1"])
                sc.op("dve", lambda e: e.scalar_tensor_tensor(out=Bt["ATn"][:], in0=F["t1"][:], scalar=-1.0, in1=F["Bst"][:], op0=ALU.mult, op1=ALU.mult),
                      r=["g_t1", "g_Bst"], w=["g_ATn"])
                sc.op("dve", lambda e: e.tensor_tensor(out=Bt["intraT"][:], in0=pK[:, 128:256], in1=F["decT"][:], op=ALU.mult), r=["gpK", "g_decT"], w=["g_intraT"])
                sc.op("pe", lambda e: e.transpose(pB[:, 512:640], Bt["ATn"][:], cst["ident_b"][:]), r=["g_ATn", "c_ident_b"], w=["gpB"])
                sc.op("act", lambda e: e.activation(out=Bt["An"][:], in_=pB[:, 512:640], func=AF.Copy), r=["gpB"], w=["g_An"])
                sc.op("dve", lambda e: e.tensor_tensor(out=Bt["TT"][:], in0=Bt["ATn"][:], in1=cst["ident_b"][:], op=ALU.add), r=["g_ATn", "c_ident_b"], w=["g_TT"])
                if g.gdn_stop == "C2":
                    return
                Pk, PTk = "An", "ATn"
                Pn, PTn = "A", "AT"
                for k in range(1, 6):
                    sc.op("pe", lambda e, Pk=Pk, PTk=PTk: e.matmul(pP[:, 0:128], lhsT=Bt[PTk][:], rhs=Bt[Pk][:], start=True, stop=True),
                          r=["g_" + Pk, "g_" + PTk], w=["gpP"])
                    if k < 5:
                        sc.op("pe", lambda e, Pk=Pk, PTk=PTk: e.matmul(pP[:, 128:256], lhsT=Bt[Pk][:], rhs=Bt[PTk][:], start=True, stop=True),
                              r=["g_" + Pk, "g_" + PTk], w=["gpP"])
                    sc.op("act", lambda e, Pn=Pn: e.activation(out=Bt[Pn][:], in_=pP[:, 0:128], func=AF.Copy), r=["gpP"], w=["g_" + Pn])
                    if k < 5:
                        sc.op("dve", lambda e, PTn=PTn: e.tensor_copy(out=Bt[PTn][:], in_=pP[:, 128:256]), r=["gpP"], w=["g_" + PTn])
                    sc.op("pe", lambda e, Pn=Pn: e.matmul(pTu[:, 0:128], lhsT=Bt[Pn][:], rhs=Bt["TT"][:], start=True, stop=True), r=["g_" + Pn, "g_TT"], w=["gpTu"])
                    sc.op("dve", lambda e: e.tensor_tensor(out=Bt["TT"][:], in0=pTu[:, 0:128], in1=Bt["TT"][:], op=ALU.add), r=["gpTu", "g_TT"], w=["g_TT"])
                    Pk, PTk, Pn, PTn = Pn, PTn, Pk, PTk
                if g.gdn_stop == "C3":
                    return
                sc.op("dve", lambda e, i=i: e.tensor_scalar(out=Bt["vb"][:], in0=Vtk[:, i, :], scalar1=be_col, scalar2=None, op0=ALU.mult), r=["gVtk"] + sck, w=["g_vb"])
                sc.op("dve", lambda e, i=i: e.tensor_scalar(out=Bt["kbg"][:], in0=Ktok[:, i, :], scalar1=bege_col, scalar2=None, op0=ALU.mult), r=["gKtok"] + sck, w=["g_kbg"])
                sc.op("dve", lambda e, i=i: e.tensor_scalar(out=Bt["kdec"][:], in0=Ktok[:, i, :], scalar1=kd_col, scalar2=None, op0=ALU.mult), r=["gKtok"] + sck, w=["g_kdec"])
                sc.op("dve", lambda e, tsl=tsl: e.tensor_tensor(out=Bt["qgT"][:], in0=QTn[:, tsl], in1=F["eG"][:], op=ALU.mult), r=["gQTn", "g_eG"], w=["g_qgT"])
                sc.op("pe", lambda e: e.matmul(pU[:, 0:128], lhsT=Bt["TT"][:], rhs=Bt["vb"][:], start=True, stop=True), r=["g_TT", "g_vb"], w=["gpU"])
                sc.op("pe", lambda e: e.matmul(pU[:, 128:256], lhsT=Bt["kbg"][:], rhs=Bt["TT"][:], start=True, stop=True), r=["g_TT", "g_kbg"], w=["gpU"])
                sc.op("act", lambda e: e.activation(out=F["u"][:], in_=pU[:, 0:128], func=AF.Copy), r=["gpU"], w=["g_u"])
                sc.op("dve", lambda e: e.tensor_copy(out=Bt["wT"][:], in_=pU[:, 128:256]), r=["gpU"], w=["g_wT"])
                if g.gdn_stop == "C4":
                    return
                for cj in range(2):
                    r0 = cj * 64
                    rs = slice(r0, r0 + 64)
                    vn = vnc[cj]
                    vk = f"g_vn{cj}"
                    sc.op("pe", lambda e: e.matmul(pV[:, 0:128], lhsT=Bt["wT"][:], rhs=Sb[:], start=True, stop=True), r=["g_wT", "gSb"], w=["gpV"])
                    sc.op("dve", lambda e: e.scalar_tensor_tensor(out=t2[:], in0=pV[:, 0:128], scalar=-1.0, in1=F["u"][:], op0=ALU.mult, op1=ALU.add), r=["g_u", "gpV"], w=["g_t2"])
                    sc.op("dve", lambda e, cj=cj, vn=vn: e.tensor_scalar(out=vn[:], in0=t2[:], scalar1=cst["rowmask"][:, cj:cj + 1], scalar2=None, op0=ALU.mult), r=["g_t2", "c_rowmask"], w=[vk])
                    sc.op("pe", lambda e, rs=rs: e.matmul(pO[:, rs], lhsT=Sb[:], rhs=Bt["qgT"][:, rs], start=True, stop=False), r=["gSb", "g_qgT"], w=["gpO"])
                    sc.op("pe", lambda e, rs=rs, vn=vn: e.matmul(pO[:, rs], lhsT=vn[:], rhs=Bt["intraT"][:, rs], start=False, stop=True), r=[vk, "g_intraT"], w=["gpO"])
                    sc.op("pe", lambda e, vn=vn: e.matmul(pV[:, 128:256], lhsT=Bt["kdec"][:], rhs=vn[:], start=True, stop=True), r=["g_kdec", vk], w=["gpV"])
                    sc.op("dve", lambda e, r0=r0: e.scalar_tensor_tensor(out=Sf[:], in0=Sf[:], scalar=F["eG"][:, r0 + 63:r0 + 64], in1=pV[:, 128:256],
                                                                       op0=ALU.mult, op1=ALU.add), r=["gSf", "g_eG", "gpV"], w=["gSf"])
                    sc.op("act", lambda e: e.activation(out=Sb[:], in_=Sf[:], func=AF.Copy), r=["gSf"], w=["gSb"])
                if g.gdn_stop == "C5":
                    return
                sc.op("act", lambda e: e.activation(out=Bt["sq2"][:], in_=pO[:, 0:128], func=AF.Square), r=["gpO"], w=["g_sq2"])
                sc.op("pe", lambda e: e.matmul(pO[:, 128:256], lhsT=g.ones_b[:], rhs=Bt["sq2"][:], start=True, stop=True), r=["g_sq2", "ones_b"], w=["gpO"])
                sc.op("dve", lambda e: e.tensor_scalar(out=rn2[:], in0=pO[:, 128:256], scalar1=1.0 / 128, scalar2=EPS, op0=ALU.mult, op1=ALU.add), r=["gpO"], w=["g_rn2"])
                sc.op("act", lambda e: e.activation(out=rn2[:], in_=rn2[:], func=AF.Ln), r=["g_rn2"], w=["g_rn2"])
                sc.op("act", lambda e: e.activation(out=rn2[:], in_=rn2[:], func=AF.Exp, scale=-0.5), r=["g_rn2"], w=["g_rn2"])
                sc.op("dve", lambda e: e.scalar_tensor_tensor(out=t2[:], in0=pO[:, 0:128], scalar=gnw[:, 0:1], in1=rn2[:], op0=ALU.mult, op1=ALU.mult),
                      r=["gpO", "ggnw", "g_rn2"], w=["g_t2"])
                yb_ = n_st % 2
                sc.op("dve", lambda e, yb_=yb_, i=i, tsl=tsl: e.tensor_tensor(out=ystg[yb_][:, (i % 4) * 128:(i % 4 + 1) * 128], in0=t2[:], in1=zsT[:, tsl], op=ALU.mult),
                      r=["g_t2", "gzsT"], w=[f"gystg{yb_}"])
                if i % 4 == 3:
                    q0 = (i // 4) * 512
                    sc.dma("sp", lambda e, yb_=yb_, h=h, q0=q0: e.dma_start(out=io["YT"][(8 + h) * 128:(9 + h) * 128, q0:q0 + 512], in_=ystg[yb_][:]),
                           r=[f"gystg{yb_}"], w=["YT"])
                    n_st += 1


def load_w_bf16(g, dst, src_l, stg, key):
    sc = g.sc
    v = src_l.rearrange("(kc p) f -> p kc f", p=128)
    for hf in range(2):
        sc.dma("sp", lambda e, hf=hf: e.dma_start(out=stg[hf][:], in_=v[:, :, hf * 512:(hf + 1) * 512]), w=[f"mstg{hf}"])
        sc.op("pool", lambda e, hf=hf: e.tensor_copy(out=dst[:, :, hf * 512:(hf + 1) * 512], in_=stg[hf][:]), r=[f"mstg{hf}"], w=[key])


def phase_merge(g, l, xsrc):
    nc, sc, io = g.nc, g.sc, g.io
    S = g.S
    NTC = S // 512
    YTv = io["YT"].rearrange("(c p) t -> p c t", p=128)
    PTv = io["PT"].rearrange("(c p) t -> p c t", p=128)
    with ExitStack() as ls:
        wa = T(ls, nc, "mwa", [128, KC, D], BF16)
        wb = T(ls, nc, "mwb", [128, KC, D], BF16)
        wo = T(ls, nc, "mwo", [128, KC, D], BF16)
        g1bc = T(ls, nc, "mg1", [128, D], F32)
        with ExitStack() as l2:
            stg = [T(l2, nc, f"mstg{i}", [128, KC, 512], F32) for i in range(2)]
            load_w_bf16(g, wa, io["w_a"][l], stg, "mwa")
            load_w_bf16(g, wb, io["w_b"][l], stg, "mwb")
            load_w_bf16(g, wo, io["w_out"][l], stg, "mwo")
            sc.barrier()
        load_bc(g, g1bc, l, 0, "mg1")
        yaT = T(ls, nc, "myaT", [128, KC, 512], BF16)
        ybT = T(ls, nc, "mybT", [128, KC, 512], BF16)
        gaT = T(ls, nc, "mgaT", [128, KC, 512], BF16)
        gbT = T(ls, nc, "mgbT", [128, KC, 512], BF16)
        mixT = T(ls, nc, "mmixT", [128, KC, 512], BF16)
        m1 = [T(ls, nc, f"mm1{i}", [128, 512], F32) for i in range(2)]
        m2 = [T(ls, nc, f"mm2{i}", [128, 512], F32) for i in range(2)]
        xt = [T(ls, nc, f"mxt{i}", [128, D], F32) for i in range(2)]
        xo = [T(ls, nc, f"mxo{i}", [128, D], F32) for i in range(2)]
        pA = [PS(ls, nc, f"mpA{i}", [128, 512], F32) for i in range(2)]
        pBm = [PS(ls, nc, f"mpB{i}", [128, 512], F32) for i in range(2)]
        pO = [PS(ls, nc, f"mpO{i}", [128, 512], F32) for i in range(2)]
        n = 0
        for tc in range(NTC):
            cs = slice(tc * 512, (tc + 1) * 512)
            sc.dma("sp", lambda e, cs=cs: e.dma_start(out=yaT[:], in_=YTv[:, 0:8, cs]), r=["YT"], w=["myaT"])
            sc.dma("sp", lambda e, cs=cs: e.dma_start(out=ybT[:], in_=YTv[:, 8:16, cs]), r=["YT"], w=["mybT"])
            sc.dma("act", lambda e, cs=cs: e.dma_start(out=gaT[:], in_=PTv[:, 48:56, cs]), r=["PT"], w=["mgaT"])
            sc.dma("act", lambda e, cs=cs: e.dma_start(out=gbT[:], in_=PTv[:, 56:64, cs]), r=["PT"], w=["mgbT"])
            for dc in range(KC):
                b = dc % 2
                for kc in range(KC):
                    sc.op("pe", lambda e, b=b, kc=kc, dc=dc: e.matmul(pA[b][:], lhsT=wa[:, kc, dc * 128:(dc + 1) * 128], rhs=yaT[:, kc, :],
                                                                      start=(kc == 0), stop=(kc == KC - 1)), r=["mwa", "myaT"], w=[f"mpA{b}"])
                for kc in range(KC):
                    sc.op("pe", lambda e, b=b, kc=kc, dc=dc: e.matmul(pBm[b][:], lhsT=wb[:, kc, dc * 128:(dc + 1) * 128], rhs=ybT[:, kc, :],
                                                                      start=(kc == 0), stop=(kc == KC - 1)), r=["mwb", "mybT"], w=[f"mpB{b}"])
                sc.op("dve", lambda e, b=b, dc=dc: e.tensor_tensor(out=m1[b][:], in0=pA[b][:], in1=gaT[:, dc, :], op=ALU.mult), r=[f"mpA{b}", "mgaT"], w=[f"mm1{b}"])
                sc.op("dve", lambda e, b=b, dc=dc: e.tensor_tensor(out=m2[b][:], in0=pBm[b][:], in1=gbT[:, dc, :], op=ALU.mult), r=[f"mpB{b}", "mgbT"], w=[f"mm2{b}"])
                sc.op("pool", lambda e, b=b, dc=dc: e.tensor_tensor(out=mixT[:, dc, :], in0=m1[b][:], in1=m2[b][:], op=ALU.add), r=[f"mm1{b}", f"mm2{b}"], w=["mmixT"])
            for tt in range(4):
                ti = tc * 4 + tt
                xb = n % 2
                n += 1
                sc.dma("sp", lambda e, xb=xb, ti=ti: e.dma_start(out=xt[xb][:], in_=xsrc[ti * 128:(ti + 1) * 128, :]), r=[f"xr{ti}"], w=[f"mxt{xb}"])
                for hf in range(2):
                    for kc in range(KC):
                        sc.op("pe", lambda e, hf=hf, kc=kc, tt=tt: e.matmul(pO[hf][:], lhsT=mixT[:, kc, tt * 128:(tt + 1) * 128], rhs=wo[:, kc, hf * 512:(hf + 1) * 512],
                                                                            start=(kc == 0), stop=(kc == KC - 1)), r=["mmixT", "mwo"], w=[f"mpO{hf}"])
                    hs = slice(hf * 512, (hf + 1) * 512)
                    sc.op("dve", lambda e, hf=hf, hs=hs, xb=xb: e.tensor_tensor(out=xo[xb][:, hs], in0=pO[hf][:], in1=g1bc[:, hs], op=ALU.mult), r=[f"mpO{hf}", "mg1"], w=[f"mxo{xb}"])
                    sc.op("pool", lambda e, hs=hs, xb=xb: e.tensor_tensor(out=xo[xb][:, hs], in0=xo[xb][:, hs], in1=xt[xb][:, hs], op=ALU.add), r=[f"mxo{xb}", f"mxt{xb}"], w=[f"mxo{xb}"])
                sc.dma("sp", lambda e, xb=xb, ti=ti: e.dma_start(out=io["xr"][ti * 128:(ti + 1) * 128, :], in_=xo[xb][:]), r=[f"mxo{xb}"], w=[f"xr{ti}"])


def phase_moe(g, l):
    nc, sc, io = g.nc, g.sc, g.io
    S, NB = g.S, g.NB
    NT = S // 128
    cst = g.cst
    SP = mybir.EngineType.SP
    with ExitStack() as ls:
        E1 = T(ls, nc, "oE1", [128, NT, 32], F32)
        E2 = T(ls, nc, "oE2", [128, NT, 32], F32)
        POS = T(ls, nc, "oPOS", [128, NT, 32], F32)
        Wt = T(ls, nc, "oW", [128, NT, 2], F32)
        IDXf = T(ls, nc, "oIDXf", [128, NT, 2], F32)
        IDX = T(ls, nc, "oIDX", [128, NT, 2], I32)
        basebc = T(ls, nc, "obase", [128, 32], F32)
        a2bc = T(ls, nc, "oa2bc", [128, D], F32)
        b2bc = T(ls, nc, "ob2bc", [128, D], F32)
        g2bc = T(ls, nc, "og2bc", [128, D], F32)
        wr = T(ls, nc, "owr", [128, KC, 36], F32)
        rb = T(ls, nc, "orb", [128, 36], F32)
        blk = T(ls, nc, "oblk", [128, NB], F32)
        blki = T(ls, nc, "oblki", [128, NB], I32)
        load_bc(g, a2bc, l, 1, "oa2bc")
        load_bc(g, b2bc, l, 2, "ob2bc")
        load_bc(g, g2bc, l, 3, "og2bc")
        sc.dma("sp", lambda e: e.dma_start(out=wr[:], in_=io["wr"][l].rearrange("(kc p) f -> p kc f", p=128)), w=["owr"])
        sc.dma("sp", lambda e: e.dma_start(out=rb[:], in_=io["rb_bc"][l]), w=["orb"])
        sc.op("pool", lambda e: e.memset(basebc[:], 0.0), w=["obase"])
        with ExitStack() as l1:
            xt = [T(l1, nc, f"ox{i}", [128, D], F32) for i in range(2)]
            xn = [T(l1, nc, f"oxn{i}", [128, D], F32) for i in range(2)]
            h2 = [T(l1, nc, f"oh2{i}", [128, D], F32) for i in range(2)]
            h2T = [T(l1, nc, f"oh2T{i}", [128, KC, 128], F32) for i in range(2)]
            junk = T(l1, nc, "ojunk", [128, D], BF16)
            st = [T(l1, nc, f"ost{i}", [128, 16, 16], F32) for i in range(2)]
            lg = [T(l1, nc, f"olg{i}", [128, 36], F32) for i in range(2)]
            es_ = [T(l1, nc, f"oes{i}", [128, 8], F32) for i in range(2)]
            em_ = [T(l1, nc, f"oem{i}", [128, 8], F32) for i in range(2)]
            oh = [T(l1, nc, f"ooh{i}", [128, 3, 8], F32) for i in range(2)]
            esum = [T(l1, nc, f"oesum{i}", [128, 32], F32) for i in range(2)]
            pst = [PS(l1, nc, f"opst{i}", [128, 512], F32) for i in range(4)]
            plg = [PS(l1, nc, f"oplg{i}", [128, 512], F32) for i in range(2)]
            ppos = [PS(l1, nc, f"oppos{i}", [128, 512], F32) for i in range(2)]
            for i in range(NT):
                b = i % 2
                S_ = lambda j, b=b: st[b][:, j, 0:1]
                kst = f"ost{b}"
                sc.dma("sp", lambda e, b=b, i=i: e.dma_start(out=xt[b][:], in_=io["xr"][i * 128:(i + 1) * 128, :]), r=[f"xr{i}"], w=[f"ox{b}"])
                sc.op("act", lambda e, b=b: e.activation(out=junk[:], in_=xt[b][:], func=AF.Square, accum_out=st[b][:, 0, 0:1]), r=[f"ox{b}"], w=["ojunk", kst])
                sc.op("dve", lambda e, b=b: e.tensor_scalar(out=st[b][:, 1, 0:1], in0=st[b][:, 0, 0:1], scalar1=1.0 / D, scalar2=EPS, op0=ALU.mult, op1=ALU.add), r=[kst], w=[kst])
                sc.op("act", lambda e, b=b: e.activation(out=st[b][:, 2, 0:1], in_=st[b][:, 1, 0:1], func=AF.Sqrt), r=[kst], w=[kst])
                sc.op("dve", lambda e, b=b: e.reciprocal(out=st[b][:, 3, 0:1], in_=st[b][:, 2, 0:1]), r=[kst], w=[kst])
                sc.op("dve", lambda e, b=b: e.tensor_scalar(out=xn[b][:], in0=xt[b][:], scalar1=st[b][:, 3, 0:1], scalar2=None, op0=ALU.mult), r=[f"ox{b}", kst], w=[f"oxn{b}"])
                sc.op("pool", lambda e, b=b: e.tensor_tensor(out=h2[b][:], in0=xn[b][:], in1=a2bc[:], op=ALU.mult), r=[f"oxn{b}", "oa2bc"], w=[f"oh2{b}"])
                sc.op("pool", lambda e, b=b: e.tensor_tensor(out=h2[b][:], in0=h2[b][:], in1=b2bc[:], op=ALU.add), r=[f"oh2{b}", "ob2bc"], w=[f"oh2{b}"])
                sc.dma("sp", lambda e, b=b, i=i: e.dma_start(out=io["h2d"][i * 128:(i + 1) * 128, :], in_=h2[b][:]), r=[f"oh2{b}"], w=[f"h2d{i}"])
                for kc in range(KC):
                    pb = b * 2 + kc // 4
                    sc.op("pe", lambda e, b=b, kc=kc, pb=pb: e.transpose(pst[pb][:, (kc % 4) * 128:(kc % 4 + 1) * 128], xn[b][:, kc * 128:(kc + 1) * 128], cst["ident_f"][:]),
                          r=[f"oxn{b}", "c_ident_f"], w=[f"opst{pb}"])
                for kc in range(KC):
                    pb = b * 2 + kc // 4
                    sc.op("act", lambda e, b=b, kc=kc, pb=pb: e.activation(out=h2T[b][:, kc, :], in_=pst[pb][:, (kc % 4) * 128:(kc % 4 + 1) * 128], func=AF.Identity,
                                                                          bias=g.mod[:, l, 24 + kc:25 + kc], scale=g.A2[:, l, kc:kc + 1]), r=[f"opst{pb}", "mod", "A2"], w=[f"oh2T{b}"])
                for kc in range(KC):
                    sc.op("pe", lambda e, b=b, kc=kc: e.matmul(plg[b][:, 0:36], lhsT=h2T[b][:, kc, :], rhs=wr[:, kc, :], start=(kc == 0), stop=(kc == KC - 1)),
                          r=[f"oh2T{b}", "owr"], w=[f"oplg{b}"])
                L = lg[b]
                kl = f"olg{b}"
                sc.op("dve", lambda e, b=b, L=L: e.tensor_tensor(out=L[:], in0=plg[b][:, 0:36], in1=rb[:], op=ALU.add), r=[f"oplg{b}", "orb"], w=[kl])
                sc.op("dve", lambda e, b=b, L=L: e.tensor_reduce(out=st[b][:, 4, 0:1], in_=L[:, 0:4], axis=AX.X, op=ALU.max), r=[kl], w=[kst])
                sc.op("dve", lambda e, b=b: e.tensor_scalar(out=st[b][:, 5, 0:1], in0=st[b][:, 4, 0:1], scalar1=-1.0, scalar2=None, op0=ALU.mult), r=[kst], w=[kst])
                sc.op("act", lambda e, b=b, L=L: e.activation(out=es_[b][:, 0:4], in_=L[:, 0:4], func=AF.Exp, bias=st[b][:, 5, 0:1], scale=1.0, accum_out=st[b][:, 6, 0:1]),
                      r=[kl, kst], w=[f"oes{b}", kst])
                sc.op("dve", lambda e, b=b: e.reciprocal(out=st[b][:, 7, 0:1], in_=st[b][:, 6, 0:1]), r=[kst], w=[kst])
                sc.op("dve", lambda e, b=b, L=L: e.tensor_scalar(out=oh[b][:, 0, 0:4], in0=L[:, 0:4], scalar1=st[b][:, 4, 0:1], scalar2=None, op0=ALU.is_equal),
                      r=[kl, kst], w=[f"ooh{b}"])
                sc.op("dve", lambda e, b=b, L=L: e.tensor_scalar(out=es_[b][:], in0=L[:, 4:12], scalar1=oh[b][:, 0, 0:1], scalar2=None, op0=ALU.mult), r=[kl, f"ooh{b}"], w=[f"oes{b}"])
                for gq in range(1, 4):
                    sc.op("dve", lambda e, b=b, L=L, gq=gq: e.scalar_tensor_tensor(out=es_[b][:], in0=L[:, 4 + gq * 8:12 + gq * 8], scalar=oh[b][:, 0, gq:gq + 1], in1=es_[b][:],
                                                                                 op0=ALU.mult, op1=ALU.add), r=[kl, f"ooh{b}", f"oes{b}"], w=[f"oes{b}"])
                sc.op("dve", lambda e, b=b: e.tensor_reduce(out=st[b][:, 8, 0:1], in_=es_[b][:], axis=AX.X, op=ALU.max), r=[f"oes{b}"], w=[kst])
                sc.op("dve", lambda e, b=b: e.tensor_scalar(out=oh[b][:, 1, :], in0=es_[b][:], scalar1=st[b][:, 8, 0:1], scalar2=None, op0=ALU.is_equal), r=[f"oes{b}", kst], w=[f"ooh{b}"])
                sc.op("dve", lambda e, b=b: e.scalar_tensor_tensor(out=em_[b][:], in0=oh[b][:, 1, :], scalar=-1e30, in1=es_[b][:], op0=ALU.mult, op1=ALU.add),
                      r=[f"ooh{b}", f"oes{b}"], w=[f"oem{b}"])
                sc.op("dve", lambda e, b=b: e.tensor_reduce(out=st[b][:, 9, 0:1], in_=em_[b][:], axis=AX.X, op=ALU.max), r=[f"oem{b}"], w=[kst])
                sc.op("dve", lambda e, b=b: e.tensor_scalar(out=oh[b][:, 2, :], in0=em_[b][:], scalar1=st[b][:, 9, 0:1], scalar2=None, op0=ALU.is_equal), r=[f"oem{b}", kst], w=[f"ooh{b}"])
                sc.op("dve", lambda e, b=b: e.tensor_scalar(out=st[b][:, 10, 0:1], in0=st[b][:, 8, 0:1], scalar1=-1.0, scalar2=None, op0=ALU.mult), r=[kst], w=[kst])
                sc.op("act", lambda e, b=b: e.activation(out=st[b][:, 11, 0:1], in_=st[b][:, 9, 0:1], func=AF.Exp, bias=st[b][:, 10, 0:1], scale=1.0), r=[kst], w=[kst])
                sc.op("dve", lambda e, b=b: e.tensor_scalar(out=st[b][:, 12, 0:1], in0=st[b][:, 11, 0:1], scalar1=1.0, scalar2=None, op0=ALU.add), r=[kst], w=[kst])
                sc.op("dve", lambda e, b=b: e.reciprocal(out=st[b][:, 13, 0:1], in_=st[b][:, 12, 0:1]), r=[kst], w=[kst])
                sc.op("dve", lambda e, b=b, i=i: e.tensor_tensor(out=Wt[:, i, 0:1], in0=st[b][:, 13, 0:1], in1=st[b][:, 7, 0:1], op=ALU.mult), r=[kst], w=[f"oW{i}"])
                sc.op("dve", lambda e, b=b, i=i: e.tensor_tensor(out=Wt[:, i, 1:2], in0=Wt[:, i, 0:1], in1=st[b][:, 11, 0:1], op=ALU.mult), r=[kst, f"oW{i}"], w=[f"oW{i}"])
                for gq in range(4):
                    sc.op("dve", lambda e, b=b, i=i, gq=gq: e.tensor_scalar(out=E1[:, i, gq * 8:(gq + 1) * 8], in0=oh[b][:, 1, :], scalar1=oh[b][:, 0, gq:gq + 1], scalar2=None, op0=ALU.mult),
                          r=[f"ooh{b}"], w=[f"oE{i}"])
                    sc.op("dve", lambda e, b=b, i=i, gq=gq: e.tensor_scalar(out=E2[:, i, gq * 8:(gq + 1) * 8], in0=oh[b][:, 2, :], scalar1=oh[b][:, 0, gq:gq + 1], scalar2=None, op0=ALU.mult),
                          r=[f"ooh{b}"], w=[f"oE{i}"])
                sc.op("dve", lambda e, b=b, i=i: e.tensor_tensor(out=esum[b][:], in0=E1[:, i, :], in1=E2[:, i, :], op=ALU.add), r=[f"oE{i}"], w=[f"oesum{b}"])
                sc.op("pe", lambda e, b=b: e.matmul(ppos[b][:, 0:32], lhsT=cst["sltU"][:], rhs=esum[b][:], start=True, stop=True), r=[f"oesum{b}", "c_sltU"], w=[f"oppos{b}"])
                sc.op("pe", lambda e, b=b: e.matmul(ppos[b][:, 32:64], lhsT=g.ones_f[:], rhs=esum[b][:], start=True, stop=True), r=[f"oesum{b}", "ones_f"], w=[f"oppos{b}"])
                sc.op("dve", lambda e, b=b, i=i: e.tensor_tensor(out=POS[:, i, :], in0=ppos[b][:, 0:32], in1=basebc[:], op=ALU.add), r=[f"oppos{b}", "obase"], w=[f"oPOS{i}"])
                sc.op("dve", lambda e, b=b: e.tensor_tensor(out=basebc[:], in0=ppos[b][:, 32:64], in1=basebc[:], op=ALU.add), r=[f"oppos{b}", "obase"], w=["obase"])
            sc.barrier()
        if g.moe_stop == "1":
            return
        with ExitStack() as l2:
            v = T(l2, nc, "ov", [128, 8, 32], F32)
            vi = T(l2, nc, "ovi", [128, 32], I32)
            ones32 = T(l2, nc, "oones32", [128, 32], F32)
            tmp32 = T(l2, nc, "otmp32", [128, 32], F32)
            acc = T(l2, nc, "oacc", [128, NB], F32)
            cmp_ = T(l2, nc, "ocmp", [128, NB], F32)
            iot = T(l2, nc, "oiot", [128, NB], F32)
            h2r = [T(l2, nc, f"oh2r{i}", [128, D], F32) for i in range(2)]
            sc.dma("sp", lambda e: e.dma_start(out=iot[:], in_=io["iota_nb"]), w=["oiot"])
            sc.op("pool", lambda e: e.memset(ones32[:], 1.0), w=["oones32"])
            sc.op("dve", lambda e: e.tensor_scalar(out=v[:, 0, :], in0=basebc[:], scalar1=127.0, scalar2=1.0 / 128, op0=ALU.add, op1=ALU.mult), r=["obase"], w=["ov"])
            sc.op("dve", lambda e: e.tensor_copy(out=vi[:], in_=v[:, 0, :]), r=["ov"], w=["ovi"])
            sc.op("dve", lambda e: e.tensor_copy(out=v[:, 2, :], in_=vi[:]), r=["ovi"], w=["ov"])
            sc.op("dve", lambda e: e.tensor_tensor(out=v[:, 3, :], in0=v[:, 2, :], in1=v[:, 0, :], op=ALU.is_gt), r=["ov"], w=["ov"])
            sc.op("dve", lambda e: e.tensor_tensor(out=v[:, 2, :], in0=v[:, 2, :], in1=v[:, 3, :], op=ALU.subtract), r=["ov"], w=["ov"])
            sc.op("dve", lambda e: e.tensor_tensor(out=v[:, 3, :], in0=v[:, 0, :], in1=v[:, 2, :], op=ALU.subtract), r=["ov"], w=["ov"])
            sc.op("dve", lambda e: e.tensor_scalar(out=v[:, 3, :], in0=v[:, 3, :], scalar1=1.0, scalar2=None, op0=ALU.is_ge), r=["ov"], w=["ov"])
            sc.op("dve", lambda e: e.tensor_tensor(out=v[:, 2, :], in0=v[:, 2, :], in1=v[:, 3, :], op=ALU.add), r=["ov"], w=["ov"])
            sc.op("dve", lambda e: e.tensor_tensor_scan(out=v[:, 4, :], data0=ones32[:], data1=v[:, 2, :], initial=0.0, op0=ALU.mult, op1=ALU.add), r=["ov", "oones32"], w=["ov"])
            sc.op("dve", lambda e: e.tensor_tensor(out=v[:, 5, :], in0=v[:, 4, :], in1=v[:, 2, :], op=ALU.subtract), r=["ov"], w=["ov"])
            sc.op("dve", lambda e: e.tensor_scalar(out=v[:, 5, :], in0=v[:, 5, :], scalar1=128.0, scalar2=None, op0=ALU.mult), r=["ov"], w=["ov"])
            sc.op("pool", lambda e: e.memset(acc[:], 0.0), w=["oacc"])
            for e_ in range(32):
                sc.op("dve", lambda e, e_=e_: e.tensor_scalar(out=cmp_[:], in0=iot[:], scalar1=v[:, 4, e_:e_ + 1], scalar2=None, op0=ALU.is_ge), r=["oiot", "ov"], w=["ocmp"])
                sc.op("dve", lambda e: e.tensor_tensor(out=acc[:], in0=acc[:], in1=cmp_[:], op=ALU.add), r=["ocmp", "oacc"], w=["oacc"])
            sc.op("dve", lambda e: e.tensor_scalar(out=blk[:], in0=acc[:], scalar1=31.0, scalar2=None, op0=ALU.min), r=["oacc"], w=["oblk"])
            sc.op("dve", lambda e: e.tensor_copy(out=blki[:], in_=blk[:]), r=["oblk"], w=["oblki"])
            zt = T(l2, nc, "ozt", [128, D], F32)
            sc.op("pool", lambda e: e.memset(zt[:], 0.0), w=["ozt"])
            for bk in range(NB):
                sc.dma("act" if bk % 2 else "sp", lambda e, bk=bk: e.dma_start(out=io["xs"][bk * 128:(bk + 1) * 128, :], in_=zt[:]), r=["ozt"], w=["xs"])
            for i in range(NT):
                b = i % 2
                for k, Ek in enumerate((E1, E2)):
                    sc.op("dve", lambda e, i=i: e.tensor_tensor(out=tmp32[:], in0=POS[:, i, :], in1=v[:, 5, :], op=ALU.add), r=[f"oPOS{i}", "ov"], w=["otmp32"])
                    sc.op("dve", lambda e, i=i, Ek=Ek: e.tensor_tensor(out=tmp32[:], in0=tmp32[:], in1=Ek[:, i, :], op=ALU.mult), r=["otmp32", f"oE{i}"], w=["otmp32"])
                    sc.op("dve", lambda e, i=i, k=k: e.tensor_reduce(out=IDXf[:, i, k:k + 1], in_=tmp32[:], axis=AX.X, op=ALU.add), r=["otmp32"], w=[f"oIDX{i}"])
                sc.op("dve", lambda e, i=i: e.tensor_copy(out=IDX[:, i, :], in_=IDXf[:, i, :]), r=[f"oIDX{i}"], w=[f"oIDX{i}"])
                sc.dma("sp", lambda e, b=b, i=i: e.dma_start(out=h2r[b][:], in_=io["h2d"][i * 128:(i + 1) * 128, :]), r=[f"h2d{i}"], w=[f"oh2r{b}"])
                for k in range(2):
                    sc.swdma(lambda e, b=b, i=i, k=k: e.indirect_dma_start(
                        out=io["xs"], out_offset=bass.IndirectOffsetOnAxis(ap=IDX[:, i, k:k + 1], axis=0), in_=h2r[b][:], in_offset=None),
                        r=[f"oh2r{b}", f"oIDX{i}"], w=["xs"])
            sc.barrier()
        if g.moe_stop == "2":
            return
        with ExitStack() as l3:
            w1 = [T(l3, nc, f"ow1{i}", [128, KC, DE], F32) for i in range(2)]
            w3 = [T(l3, nc, f"ow3{i}", [128, KC, DE], F32) for i in range(2)]
            w2 = [T(l3, nc, f"ow2{i}", [128, 4, D], F32) for i in range(2)]
            xb_ = [T(l3, nc, f"oxb{i}", [128, D], F32) for i in range(2)]
            xT = [T(l3, nc, f"oxT{i}", [128, KC, 128], F32) for i in range(2)]
            sl = T(l3, nc, "osl", [128, 128], F32)
            gT = T(l3, nc, "ogT", [128, 4, 128], F32)
            yb_ = [T(l3, nc, f"oyb{i}", [128, D], F32) for i in range(2)]
            ptr = [PS(l3, nc, f"optr{i}", [128, 512], F32) for i in range(2)]
            ph = [PS(l3, nc, f"oph{i}", [128, 512], F32) for i in range(2)]
            py = [PS(l3, nc, f"opy{i}", [128, 512], F32) for i in range(2)]
            widf = T(l3, nc, "owidf", [128, 16], F32)
            widx = [T(l3, nc, f"owidx{i}", [128, 16], I32) for i in range(2)]
            iop = T(l3, nc, "oiop", [128, 1], F32)
            sc.dma("sp", lambda e: e.dma_start(out=iop[:], in_=io["iota_p"]), w=["oiop"])
            for bk in range(NB):
                b = bk % 2
                sc.op("dve", lambda e, bk=bk: e.scalar_tensor_tensor(out=widf[:, 0:1], in0=blk[:, bk:bk + 1], scalar=128.0, in1=iop[:], op0=ALU.mult, op1=ALU.add),
                      r=["oblk", "oiop"], w=["owidf"])
                sc.op("dve", lambda e: e.tensor_scalar(out=widf[:, 0:1], in0=widf[:, 0:1], scalar1=float(l * NE * 128), scalar2=None, op0=ALU.add), r=["owidf"], w=["owidf"])
                sc.op("dve", lambda e, b=b: e.tensor_copy(out=widx[b][:, 0:1], in_=widf[:, 0:1]), r=["owidf"], w=[f"owidx{b}"])
                sc.swdma(lambda e, b=b: e.indirect_dma_start(out=w1[b][:].rearrange("p k f -> p (k f)"), out_offset=None, in_=io["ew1"],
                                                             in_offset=bass.IndirectOffsetOnAxis(ap=widx[b][:, 0:1], axis=0)), r=[f"owidx{b}"], w=[f"ow1{b}"])
                sc.swdma(lambda e, b=b: e.indirect_dma_start(out=w3[b][:].rearrange("p k f -> p (k f)"), out_offset=None, in_=io["ew3"],
                                                             in_offset=bass.IndirectOffsetOnAxis(ap=widx[b][:, 0:1], axis=0)), r=[f"owidx{b}"], w=[f"ow3{b}"])
                sc.swdma(lambda e, b=b: e.indirect_dma_start(out=w2[b][:].rearrange("p k f -> p (k f)"), out_offset=None, in_=io["ew2"],
                                                             in_offset=bass.IndirectOffsetOnAxis(ap=widx[b][:, 0:1], axis=0)), r=[f"owidx{b}"], w=[f"ow2{b}"])
                sc.dma("act", lambda e, b=b, bk=bk: e.dma_start(out=xb_[b][:], in_=io["xs"][bk * 128:(bk + 1) * 128, :]), r=["xs"], w=[f"oxb{b}"])
                for kc in range(KC):
                    pb = kc // 4
                    sc.op("pe", lambda e, b=b, kc=kc, pb=pb: e.transpose(ptr[pb][:, (kc % 4) * 128:(kc % 4 + 1) * 128], xb_[b][:, kc * 128:(kc + 1) * 128], cst["ident_f"][:]),
                          r=[f"oxb{b}", "c_ident_f"], w=[f"optr{pb}"])
                for kc in range(KC):
                    pb = kc // 4
                    if pb == 0:
                        sc.op("act", lambda e, b=b, kc=kc, pb=pb: e.activation(out=xT[b][:, kc, :], in_=ptr[pb][:, (kc % 4) * 128:(kc % 4 + 1) * 128], func=AF.Copy),
                              r=[f"optr{pb}"], w=[f"oxT{b}"])
                    else:
                        sc.op("dve", lambda e, b=b, kc=kc, pb=pb: e.tensor_copy(out=xT[b][:, kc, :], in_=ptr[pb][:, (kc % 4) * 128:(kc % 4 + 1) * 128]),
                              r=[f"optr{pb}"], w=[f"oxT{b}"])
                for fc in range(4):
                    pb = fc % 2
                    for kc in range(KC):
                        sc.op("pe", lambda e, b=b, fc=fc, kc=kc, pb=pb: e.matmul(ph[pb][:, 0:128], lhsT=w1[b][:, kc, fc * 128:(fc + 1) * 128], rhs=xT[b][:, kc, :],
                                                                              start=(kc == 0), stop=(kc == KC - 1)), r=[f"ow1{b}", f"oxT{b}"], w=[f"oph{pb}"])
                    for kc in range(KC):
                        sc.op("pe", lambda e, b=b, fc=fc, kc=kc, pb=pb: e.matmul(py[pb][:, 0:128], lhsT=w3[b][:, kc, fc * 128:(fc + 1) * 128], rhs=xT[b][:, kc, :],
                                                                              start=(kc == 0), stop=(kc == KC - 1)), r=[f"ow3{b}", f"oxT{b}"], w=[f"opy{pb}"])
                    sc.op("act", lambda e, pb=pb: e.activation(out=sl[:], in_=ph[pb][:, 0:128], func=AF.Silu), r=[f"oph{pb}"], w=["osl"])
                    sc.op("dve", lambda e, pb=pb, fc=fc: e.tensor_tensor(out=gT[:, fc, :], in0=py[pb][:, 0:128], in1=sl[:], op=ALU.mult), r=[f"opy{pb}", "osl"], w=["ogT"])
                for hf in range(2):
                    for fc in range(4):
                        sc.op("pe", lambda e, b=b, hf=hf, fc=fc: e.matmul(ptr[hf][:], lhsT=gT[:, fc, :], rhs=w2[b][:, fc, hf * 512:(hf + 1) * 512],
                                                                          start=(fc == 0), stop=(fc == 3)), r=["ogT", f"ow2{b}"], w=[f"optr{hf}"])
                    if hf == 0:
                        sc.op("act", lambda e, b=b: e.activation(out=yb_[b][:, 0:512], in_=ptr[0][:], func=AF.Copy), r=["optr0"], w=[f"oyb{b}"])
                    else:
                        sc.op("dve", lambda e, b=b: e.tensor_copy(out=yb_[b][:, 512:1024], in_=ptr[1][:]), r=["optr1"], w=[f"oyb{b}"])
                sc.dma("act", lambda e, b=b, bk=bk: e.dma_start(out=io["ys"][bk * 128:(bk + 1) * 128, :], in_=yb_[b][:]), r=[f"oyb{b}"], w=["ys"])
            sc.barrier()
        if g.moe_stop == "3":
            return
        with ExitStack() as l4:
            y0 = [T(l4, nc, f"oy0{i}", [128, D], F32) for i in range(2)]
            y1 = [T(l4, nc, f"oy1{i}", [128, D], F32) for i in range(2)]
            xt = [T(l4, nc, f"oxc{i}", [128, D], F32) for i in range(2)]
            for i in range(NT):
                b = i % 2
                sc.swdma(lambda e, b=b, i=i: e.indirect_dma_start(out=y0[b][:], out_offset=None, in_=io["ys"],
                                                                  in_offset=bass.IndirectOffsetOnAxis(ap=IDX[:, i, 0:1], axis=0)), r=["ys", f"oIDX{i}"], w=[f"oy0{b}"])
                sc.swdma(lambda e, b=b, i=i: e.indirect_dma_start(out=y1[b][:], out_offset=None, in_=io["ys"],
                                                                  in_offset=bass.IndirectOffsetOnAxis(ap=IDX[:, i, 1:2], axis=0)), r=["ys", f"oIDX{i}"], w=[f"oy1{b}"])
                if g.moe_stop == "4a":
                    continue
                sc.dma("sp", lambda e, b=b, i=i: e.dma_start(out=xt[b][:], in_=io["xr"][i * 128:(i + 1) * 128, :]), r=[f"xr{i}"], w=[f"oxc{b}"])
                sc.op("dve", lambda e, b=b, i=i: e.tensor_scalar(out=y0[b][:], in0=y0[b][:], scalar1=Wt[:, i, 0:1], scalar2=None, op0=ALU.mult), r=[f"oy0{b}", f"oW{i}"], w=[f"oy0{b}"])
                sc.op("dve", lambda e, b=b, i=i: e.scalar_tensor_tensor(out=y0[b][:], in0=y1[b][:], scalar=Wt[:, i, 1:2], in1=y0[b][:], op0=ALU.mult, op1=ALU.add),
                      r=[f"oy0{b}", f"oy1{b}", f"oW{i}"], w=[f"oy0{b}"])
                sc.op("pool", lambda e, b=b: e.tensor_tensor(out=y0[b][:], in0=y0[b][:], in1=g2bc[:], op=ALU.mult), r=[f"oy0{b}", "og2bc"], w=[f"oy0{b}"])
                sc.op("pool", lambda e, b=b: e.tensor_tensor(out=xt[b][:], in0=xt[b][:], in1=y0[b][:], op=ALU.add), r=[f"oy0{b}", f"oxc{b}"], w=[f"oxc{b}"])
                sc.dma("sp", lambda e, b=b, i=i: e.dma_start(out=io["xr"][i * 128:(i + 1) * 128, :], in_=xt[b][:]), r=[f"oxc{b}"], w=[f"xr{i}"])


def phase_final(g, xsrc):
    nc, sc, io = g.nc, g.sc, g.io
    NT = g.S // 128
    with ExitStack() as ls:
        fw = T(ls, nc, "fw", [128, D], F32)
        xt = [T(ls, nc, f"fx{i}", [128, D], F32) for i in range(2)]
        yt = [T(ls, nc, f"fy{i}", [128, D], F32) for i in range(2)]
        junk = T(ls, nc, "fjunk", [128, D], BF16)
        st = [T(ls, nc, f"fst{i}", [128, 64], F32) for i in range(2)]
        sc.dma("sp", lambda e: e.dma_start(out=fw[:], in_=io["fnw_bc"]), w=["fw"])
        for i in range(NT):
            b = i % 2
            sc.dma("sp", lambda e, b=b, i=i: e.dma_start(out=xt[b][:], in_=xsrc[i * 128:(i + 1) * 128, :]), w=[f"fx{b}"])
            sc.op("act", lambda e, b=b: e.activation(out=junk[:], in_=xt[b][:], func=AF.Square, accum_out=st[b][:, 0:1]),
                  r=[f"fx{b}"], w=["fjunk", f"fst{b}"])
            sc.op("dve", lambda e, b=b: e.tensor_scalar(out=st[b][:, 16:17], in0=st[b][:, 0:1], scalar1=1.0 / D, scalar2=EPS,
                                                         op0=ALU.mult, op1=ALU.add), r=[f"fst{b}"], w=[f"fst{b}"])
            sc.op("act", lambda e, b=b: e.activation(out=st[b][:, 32:33], in_=st[b][:, 16:17], func=AF.Sqrt), r=[f"fst{b}"], w=[f"fst{b}"])
            sc.op("dve", lambda e, b=b: e.reciprocal(out=st[b][:, 48:49], in_=st[b][:, 32:33]), r=[f"fst{b}"], w=[f"fst{b}"])
            sc.op("dve", lambda e, b=b: e.scalar_tensor_tensor(out=yt[b][:], in0=xt[b][:], scalar=st[b][:, 48:49], in1=fw[:],
                                                                op0=ALU.mult, op1=ALU.mult), r=[f"fx{b}", f"fst{b}", "fw"], w=[f"fy{b}"])
            sc.dma("sp", lambda e, b=b, i=i: e.dma_start(out=io["y"][i * 128:(i + 1) * 128, :], in_=yt[b][:]), r=[f"fy{b}"], w=["y"])


def build(S, dbg=(), stop_after=None, layers=DEPTH):
    NB = (2 * S) // 128 + NE
    nc = bass.Bass("TRN2", target_bir_lowering=False)
    g = Ctx()
    g.nc, g.S, g.NB = nc, S, NB
    g.dbgset = set(dbg)
    import os as _os
    g.gdn_stop = _os.environ.get("GDN_STOP")
    g.moe_stop = _os.environ.get("MOE_STOP")
    dbg = [d for d in dbg if d in ("PT", "Vtok", "BD", "YT", "xr", "xs", "ys")]
    g.io = declare_io(nc, S, NB)
    io = g.io
    dbg_out = {}
    for name in dbg:
        src = io[name]
        dbg_out[name] = nc.dram_tensor("dbg_" + name, list(src.shape), src.dtype, kind="ExternalOutput").ap()
    with ExitStack() as es:
        g.es = es
        g.sc = Sched(nc, es)
        sc = g.sc
        phase_setup(g)
        done = False
        for l in range(layers):
            xsrc = io["x"] if l == 0 else io["xr"]
            with ExitStack() as ls:
                hT = T(ls, nc, "hT", [128, KC, S], BF16)
                phase_norm(g, l, xsrc, hT, g.A1[:, l, :], g.mod[:, l, 0:8])
                sc.barrier()
                dbg_sb(g, "hT", hT[:], [f"hT{i}" for i in range(S // 128)])
                phase_proj(g, l, hT)
                sc.barrier()
            if stop_after == "proj":
                break
            phase_attn(g, l)
            sc.barrier()
            if stop_after == "attn":
                break
            phase_gdn(g, l)
            sc.barrier()
            if stop_after == "gdn":
                break
            phase_merge(g, l, xsrc)
            sc.barrier()
            if stop_after == "merge":
                break
            phase_moe(g, l)
            sc.barrier()
            if stop_after == "moe":
                break
        sc.barrier()
        phase_final(g, io["xr"] if stop_after is None else io["x"])
        for name in dbg:
            src = io[name]
            dst = dbg_out[name]
            sc.dma("sp", lambda e, src=src, dst=dst: e.dma_start(out=dst, in_=src), r=[name], w=["dbg_" + name])
        sc.final_wait()
        print("instructions:", sc.ninst, "sems:", len(sc.semobj))
    return nc


def host_shared(inp):
    f = lambda a: np.ascontiguousarray(np.asarray(a, dtype=np.float32))
    sh = {}
    colT = lambda v: f(np.asarray(v).reshape(-1, 128).T)
    sh["ada_w"] = f(inp["ada_w"])
    sh["ada_b_t"] = f(np.stack([colT(inp["ada_b"][l]) for l in range(DEPTH)]))
    sh["n1w_t"] = f(np.stack([colT(inp["norm1_w"][l]) for l in range(DEPTH)]))
    sh["n2w_t"] = f(np.stack([colT(inp["norm2_w"][l]) for l in range(DEPTH)]))
    sh["fnw_bc"] = f(np.broadcast_to(np.asarray(inp["final_norm_w"])[None, :], (128, D)))
    sh["w_in"] = f(inp["w_in"])
    cw = np.asarray(inp["conv_w"])
    sh["conv_t"] = f(cw.reshape(DEPTH, 4, 24, 128).transpose(0, 3, 2, 1))
    lam = np.stack([np.asarray(inp[k]) for k in ("lambda_q1", "lambda_k1", "lambda_q2", "lambda_k2")], axis=1)
    sh["lamv"] = f(np.broadcast_to(lam[:, None], (DEPTH, 128, 4, 64)))
    sh["subln_bc"] = f(np.broadcast_to(np.asarray(inp["subln_w"])[:, None, :], (DEPTH, 128, 128)))
    sh["alog_bc"] = f(np.broadcast_to(np.asarray(inp["a_log"])[:, None, :], (DEPTH, 128, NH)))
    sh["dtb_bc"] = f(np.broadcast_to(np.asarray(inp["dt_bias"])[:, None, :], (DEPTH, 128, NH)))
    sh["gnw_t"] = f(np.asarray(inp["gdn_norm_w"])[:, :, None])
    sh["w_a"] = f(inp["w_branch_a"])
    sh["w_b"] = f(inp["w_branch_b"])
    sh["w_out"] = f(inp["w_out"])
    sh["wr"] = f(np.concatenate([np.asarray(inp["router_group_w"]), np.asarray(inp["router_expert_w"])], axis=2))
    rb = np.concatenate([np.asarray(inp["router_group_b"]), np.asarray(inp["router_expert_b"])], axis=1)
    sh["rb_bc"] = f(np.broadcast_to(rb[:, None, :], (DEPTH, 128, 36)))
    sh["ew1"] = f(np.asarray(inp["expert_w1"]).reshape(DEPTH, NE, KC, 128, DE).transpose(0, 1, 3, 2, 4).reshape(DEPTH * NE * 128, KC * DE))
    sh["ew3"] = f(np.asarray(inp["expert_w3"]).reshape(DEPTH, NE, KC, 128, DE).transpose(0, 1, 3, 2, 4).reshape(DEPTH * NE * 128, KC * DE))
    sh["ew2"] = f(np.asarray(inp["expert_w2"]).reshape(DEPTH, NE, 4, 128, D).transpose(0, 1, 3, 2, 4).reshape(DEPTH * NE * 128, 4 * D))
    sh.update(_consts())
    return sh


def host_core(inp, b):
    S_ = int(np.asarray(inp["x"]).shape[1])
    NB_ = (2 * S_) // 128 + NE
    return {"iota_nb": np.ascontiguousarray(np.broadcast_to(np.arange(NB_, dtype=np.float32)[None, :], (128, NB_))),
            "iota_p": np.arange(128, dtype=np.float32)[:, None].copy(),
            "x": np.ascontiguousarray(np.asarray(inp["x"][b], dtype=np.float32)),
            "c_t": np.ascontiguousarray(np.asarray(inp["c"][b], dtype=np.float32).reshape(KC, 128).T)}


def kernel(**inputs):
    S = int(np.asarray(inputs["x"]).shape[1])
    B = int(np.asarray(inputs["x"]).shape[0])
    sh = host_shared(inputs)
    nc = build(S)
    in_maps = [{**sh, **host_core(inputs, b)} for b in range(B)]
    res = run_bass_kernel_spmd(nc, in_maps, core_ids=list(range(B)))
    return np.stack([np.asarray(r["y"], dtype=np.float32) for r in res.results], axis=0)
```

```python
import math
from contextlib import ExitStack
import numpy as np
import ml_dtypes
import concourse.bass as bass
import concourse.mybir as mybir
from concourse.bass_utils import run_bass_kernel_spmd

F32 = mybir.dt.float32
BF16 = mybir.dt.bfloat16
I32 = mybir.dt.int32
U32 = mybir.dt.uint32
AF = mybir.ActivationFunctionType
ALU = mybir.AluOpType
AX = mybir.AxisListType

D = 1024
KC = 8
DEPTH = 2
NH = 8
NE = 32
DE = 512
PTOT = 9232
EPS = 1e-6
SEM_ROT = 1 << 30
NDSEM = 24
NSWSEM = 64
import os as _os0
SIMSAFE = bool(_os0.environ.get("BASS_SIMSAFE"))


class Sched:
    def __init__(self, nc, es):
        self.nc = nc
        self.es = es
        self.engs = {"pe": nc.tensor, "dve": nc.vector, "act": nc.scalar, "pool": nc.gpsimd, "sp": nc.sync}
        self.semobj = []
        self.cur = {}
        self.cnt = {}
        self.waited = {e: {} for e in self.engs}
        self.lastw = {}
        self.readers = {}
        self.nsem = 0
        for e in self.engs:
            self._newsem(e)
        self.dsem = []
        self.dcnt = []
        self.qsems = {}
        for q, nq in (("sp", 20), ("act", 10), ("pool", 2)):
            self.qsems[q] = []
            for i in range(nq):
                s = es.enter_context(nc.semaphore(f"dma_{q}{i}"))
                self.semobj.append(s)
                self.dsem.append(len(self.semobj) - 1)
                self.dcnt.append(0)
                self.qsems[q].append(len(self.dsem) - 1)
        self.qnext = {q: 0 for q in self.qsems}
        self.dnext = 0
        self.ninst = 0
        self.swsems = []
        for i in range(NSWSEM):
            so = es.enter_context(nc.semaphore(f"swd{i}"))
            self.semobj.append(so)
            self.swsems.append(len(self.semobj) - 1)
        self.sw_used = 0
        self.sw_next = 0
        self.swcnt = [0] * NSWSEM
        self.prog = {e: [] for e in self.engs}

    def _newsem(self, e):
        s = self.es.enter_context(self.nc.semaphore(f"s_{e}_{self.nsem}"))
        self.nsem += 1
        self.semobj.append(s)
        self.cur[e] = len(self.semobj) - 1
        self.cnt[e] = 0

    def _deps(self, e, r, w):
        deps = {}

        def add(ev):
            if ev is None:
                return
            s, v = ev
            if deps.get(s, 0) < v:
                deps[s] = v

        for k in r:
            add(self.lastw.get(k))
        for k in w:
            add(self.lastw.get(k))
            for s, v in self.readers.get(k, {}).items():
                add((s, v))
        eng = self.engs[e]
        wt = self.waited[e]
        for s, v in deps.items():
            if e == "pe" and s == self.cur["pe"]:
                continue
            if wt.get(s, 0) >= v:
                continue
            eng.wait_ge(self.semobj[s], v)
            wt[s] = v

    def _record(self, ev, r, w):
        for k in r:
            d = self.readers.setdefault(k, {})
            if d.get(ev[0], 0) < ev[1]:
                d[ev[0]] = ev[1]
        for k in w:
            self.lastw[k] = ev
            self.readers[k] = {}

    def op(self, e, fn, r=(), w=()):
        pr = [k for k in r if k in PSUM_NAMES]
        if pr:
            r = [k for k in r if k not in PSUM_NAMES]
            w = list(w) + pr
        self._deps(e, r, w)
        if self.cnt[e] >= SEM_ROT:
            self._newsem(e)
        self.cnt[e] += 1
        fn(self.engs[e]).then_inc(self.semobj[self.cur[e]], 1)
        ev = (self.cur[e], self.cnt[e])
        self._record(ev, r, w)
        self.ninst += 1
        return ev

    def dma(self, q, fn, r=(), w=()):
        self._deps(q, r, w)
        i = self.qsems[q][self.qnext[q]]
        self.qnext[q] = (self.qnext[q] + 1) % len(self.qsems[q])
        if self.dcnt[i] > 0 and self.waited[q].get(self.dsem[i], 0) < self.dcnt[i]:
            self.engs[q].wait_ge(self.semobj[self.dsem[i]], self.dcnt[i])
            self.waited[q][self.dsem[i]] = self.dcnt[i]
        self.dcnt[i] += 16
        fn(self.engs[q]).then_inc(self.semobj[self.dsem[i]], 16)
        ev = (self.dsem[i], self.dcnt[i])
        self._record(ev, r, w)
        self.ninst += 1
        return ev

    def sync_only(self, e, r=(), w=()):
        self._deps(e, r, w)

    def swdma(self, fn, r=(), w=()):
        if SIMSAFE:
            if self.sw_used == len(self.swsems):
                self.sw_recycle()
            si = self.swsems[self.sw_used]
            self.sw_used += 1
            self._deps("pool", r, w)
            fn(self.engs["pool"]).then_inc(self.semobj[si], 16)
            ev = (si, 16)
        else:
            k = self.sw_next
            self.sw_next = (self.sw_next + 1) % 16
            si = self.swsems[k]
            self._deps("pool", r, w)
            if self.swcnt[k] > 0 and self.waited["pool"].get(si, 0) < self.swcnt[k]:
                self.engs["pool"].wait_ge(self.semobj[si], self.swcnt[k])
                self.waited["pool"][si] = self.swcnt[k]
            self.swcnt[k] += 16
            fn(self.engs["pool"]).then_inc(self.semobj[si], 16)
            ev = (si, self.swcnt[k])
        self._record(ev, r, w)
        self.ninst += 1
        return ev

    def sw_recycle(self):
        self.barrier()
        self.nc.all_engine_barrier()
        for si in self.swsems[:self.sw_used]:
            self.engs["pool"].sem_clear(self.semobj[si])
        self.nc.all_engine_barrier()
        for e in self.engs:
            for si in self.swsems:
                self.waited[e].pop(si, None)
        self.sw_used = 0

    def barrier(self):
        evs = [(self.cur[e], self.cnt[e]) for e in self.engs if self.cnt[e] > 0]
        evs += [(self.dsem[i], self.dcnt[i]) for i in range(len(self.dsem)) if self.dcnt[i] > 0]
        if SIMSAFE:
            evs += [(si, 16) for si in self.swsems[:self.sw_used]]
        else:
            evs += [(self.swsems[k], self.swcnt[k]) for k in range(16) if self.swcnt[k] > 0]
        for e in self.engs:
            wt = self.waited[e]
            for s, v in evs:
                if e == "pe" and s == self.cur["pe"]:
                    continue
                if wt.get(s, 0) >= v:
                    continue
                self.engs[e].wait_ge(self.semobj[s], v)
                wt[s] = v
        self.lastw = {}
        self.readers = {}

    def final_wait(self):
        self.barrier()

    def emit(self):
        nc = self.nc
        prog = self.prog
        with nc.Block() as block:
            @block.tensor
            def _(en):
                for f in prog["pe"]:
                    f(en)

            @block.vector
            def _(en):
                for f in prog["dve"]:
                    f(en)

            @block.scalar
            def _(en):
                for f in prog["act"]:
                    f(en)

            @block.gpsimd
            def _(en):
                for f in prog["pool"]:
                    f(en)

            @block.sync
            def _(en):
                for f in prog["sp"]:
                    f(en)


def _consts():
    c = {}
    c["ident_f"] = np.eye(128, dtype=np.float32)
    c["ident_b"] = np.eye(128, dtype=np.float32).astype(ml_dtypes.bfloat16)
    k = np.arange(128)[:, None]
    q = np.arange(128)[None, :]
    c["triu_b"] = (q >= k).astype(np.float32).astype(ml_dtypes.bfloat16)
    slopes = 2.0 ** (-8.0 * np.arange(1, NH + 1) / NH)
    tab = np.zeros((128, NH * 64), np.float32)
    for h in range(NH):
        for n in range(1, 65):
            tab[:, h * 64 + n - 1] = slopes[h] * (np.arange(128) + 1 - 128 * n)
    c["alibi"] = tab
    same = (k // 64) == (q // 64)
    c["negmaskU"] = np.where(same & (q >= k), 0.0, -30000.0).astype(np.float32)
    c["strictU"] = (same & (q > k)).astype(np.float32)
    c["Lcum"] = (same & (k <= q)).astype(np.float32)
    c["Lall"] = same.astype(np.float32)
    sel = np.zeros((16, 16 * 128), np.float32)
    for j in range(16):
        sel[j, j * 128:(j + 1) * 128] = 1.0
    c["sel16"] = sel
    c["sltU"] = (k < q).astype(np.float32)
    rm = np.zeros((128, 2), np.float32)
    rm[:64, 0] = 1.0
    rm[64:, 1] = 1.0
    c["rowmask"] = rm
    return c


CONST_SHAPES = {
    "ident_f": ([128, 128], F32), "ident_b": ([128, 128], BF16), "triu_b": ([128, 128], BF16),
    "alibi": ([128, NH * 64], F32), "negmaskU": ([128, 128], F32), "strictU": ([128, 128], F32),
    "Lcum": ([128, 128], F32), "Lall": ([128, 128], F32), "sel16": ([16, 16 * 128], F32),
    "sltU": ([128, 128], F32), "rowmask": ([128, 2], F32),
}


def declare_io(nc, S, NB):
    io = {}

    def inp(name, shape, dt=F32):
        io[name] = nc.dram_tensor(name, list(shape), dt, kind="ExternalInput").ap()

    def scr(name, shape, dt):
        io[name] = nc.dram_tensor(name, list(shape), dt, kind="Internal").ap()

    inp("x", [S, D])
    inp("c_t", [128, KC])
    inp("ada_w", [DEPTH, D, 6 * D])
    inp("ada_b_t", [DEPTH, 128, 48])
    inp("n1w_t", [DEPTH, 128, KC])
    inp("n2w_t", [DEPTH, 128, KC])
    inp("fnw_bc", [128, D])
    inp("w_in", [DEPTH, D, PTOT])
    inp("conv_t", [DEPTH, 128, 24, 4])
    inp("lamv", [DEPTH, 128, 4, 64])
    inp("subln_bc", [DEPTH, 128, 128])
    inp("alog_bc", [DEPTH, 128, NH])
    inp("dtb_bc", [DEPTH, 128, NH])
    inp("gnw_t", [DEPTH, 128, 1])
    inp("w_a", [DEPTH, D, D])
    inp("w_b", [DEPTH, D, D])
    inp("w_out", [DEPTH, D, D])
    inp("wr", [DEPTH, D, 36])
    inp("rb_bc", [DEPTH, 128, 36])
    inp("ew1", [DEPTH * NE * 128, KC * DE])
    inp("ew3", [DEPTH * NE * 128, KC * DE])
    inp("ew2", [DEPTH * NE * 128, 4 * D])
    for k, (shp, dt) in CONST_SHAPES.items():
        inp(k, shp, dt)
    io["y"] = nc.dram_tensor("y", [S, D], F32, kind="ExternalOutput").ap()
    scr("xr", [S, D], F32)
    scr("PT", [64 * 128, S], BF16)
    scr("Vtok", [S, D], BF16)
    scr("BD", [S, 16], F32)
    scr("YT", [16 * 128, S], BF16)
    inp("iota_nb", [128, NB])
    inp("iota_p", [128, 1])
    scr("h2d", [S, D], F32)
    scr("xs", [NB * 128, D], F32)
    scr("ys", [NB * 128, D], F32)
    scr("e1b", [NE, D, DE], BF16)
    scr("e3b", [NE, D, DE], BF16)
    scr("e2b", [NE, DE, D], BF16)
    scr("vecs", [DEPTH, 4, D], F32)
    return io


class Ctx:
    pass


def dbg_sb(g, name, tile_, keys):
    if name not in g.dbgset:
        return
    dst = g.nc.dram_tensor("dbg_" + name, list(tile_.shape), tile_.dtype, kind="ExternalOutput").ap()
    g.sc.dma("sp", lambda e: e.dma_start(out=dst, in_=tile_), r=keys, w=["dbg_" + name])


_UID = [0]


def _uname(name):
    _UID[0] += 1
    return f"{name}_{_UID[0]}"


def T(es, nc, name, shape, dt):
    return es.enter_context(nc.sbuf_tensor(_uname(name), list(shape), dt))


PSUM_NAMES = set()


def PS(es, nc, name, shape, dt):
    PSUM_NAMES.add(name)
    return es.enter_context(nc.psum_tensor(_uname(name), list(shape), dt))


def phase_setup(g):
    nc, sc, io, es = g.nc, g.sc, g.io, g.es
    g.cst = {}
    for k, (shp, dt) in CONST_SHAPES.items():
        t = T(es, nc, "c_" + k, shp, dt)
        g.cst[k] = t
        sc.dma("sp", lambda e, t=t, k=k: e.dma_start(out=t[:], in_=io[k]), w=["c_" + k])
    g.mod = T(es, nc, "mod", [128, DEPTH, 48], F32)
    g.A1 = T(es, nc, "A1", [128, DEPTH, KC], F32)
    g.A2 = T(es, nc, "A2", [128, DEPTH, KC], F32)
    g.ones_b = T(es, nc, "ones_b", [128, 128], BF16)
    g.ones_f = T(es, nc, "ones_f", [128, 128], F32)
    sc.op("pool", lambda e: e.memset(g.ones_b[:], 1.0), w=["ones_b"])
    sc.op("pool", lambda e: e.memset(g.ones_f[:], 1.0), w=["ones_f"])
    with ExitStack() as ls:
        ct = T(ls, nc, "ct", [128, KC], F32)
        cond = T(ls, nc, "cond", [128, KC], F32)
        adab = T(ls, nc, "adab", [128, DEPTH, 48], F32)
        nw = T(ls, nc, "nw", [128, 2, DEPTH, KC], F32)
        tmp = T(ls, nc, "tmpm", [128, DEPTH, KC], F32)
        wg = [T(ls, nc, f"wg{i}", [128, KC, 1024], F32) for i in range(2)]
        psm = PS(ls, nc, "psm", [128, DEPTH * 48], F32)
        sc.dma("sp", lambda e: e.dma_start(out=ct[:], in_=io["c_t"]), w=["ct"])
        sc.dma("sp", lambda e: e.dma_start(out=adab[:], in_=io["ada_b_t"].rearrange("l p f -> p l f")), w=["adab"])
        sc.dma("sp", lambda e: e.dma_start(out=nw[:, 0], in_=io["n1w_t"].rearrange("l p f -> p l f")), w=["nw"])
        sc.dma("sp", lambda e: e.dma_start(out=nw[:, 1], in_=io["n2w_t"].rearrange("l p f -> p l f")), w=["nw"])
        sc.op("act", lambda e: e.activation(out=cond[:], in_=ct[:], func=AF.Silu), r=["ct"], w=["cond"])
        i = 0
        for l in range(DEPTH):
            for gi in range(6):
                b = i % 2
                i += 1
                src = io["ada_w"][l].rearrange("(kc p) f -> p kc f", p=128)[:, :, gi * 1024:(gi + 1) * 1024]
                sc.dma("sp" if b == 0 else "act", lambda e, b=b, src=src: e.dma_start(out=wg[b][:], in_=src), w=[f"wg{b}"])
                for f in range(8):
                    col = l * 48 + gi * 8 + f
                    for kc in range(KC):
                        sc.op("pe", lambda e, b=b, f=f, kc=kc, col=col: e.matmul(
                            psm[:, col:col + 1], lhsT=wg[b][:, kc, f * 128:(f + 1) * 128], rhs=cond[:, kc:kc + 1],
                            start=(kc == 0), stop=(kc == KC - 1)), r=[f"wg{b}", "cond"], w=["psm"])
        sc.op("dve", lambda e: e.tensor_tensor(out=g.mod[:].rearrange("p l f -> p (l f)"), in0=psm[:],
                                               in1=adab[:].rearrange("p l f -> p (l f)"), op=ALU.add),
              r=["psm", "adab"], w=["mod"])
        sc.op("dve", lambda e: e.tensor_scalar(out=tmp[:], in0=g.mod[:, :, 8:16], scalar1=1.0, scalar2=None, op0=ALU.add),
              r=["mod"], w=["tmpm"])
        sc.op("dve", lambda e: e.tensor_tensor(out=g.A1[:], in0=tmp[:], in1=nw[:, 0], op=ALU.mult), r=["tmpm", "nw"], w=["A1"])
        sc.op("dve", lambda e: e.tensor_scalar(out=tmp[:], in0=g.mod[:, :, 32:40], scalar1=1.0, scalar2=None, op0=ALU.add),
              r=["mod", "A1"], w=["tmpm"])
        sc.op("dve", lambda e: e.tensor_tensor(out=g.A2[:], in0=tmp[:], in1=nw[:, 1], op=ALU.mult), r=["tmpm", "nw"], w=["A2"])
        with nc.allow_non_contiguous_dma(reason="tiny per-feature vectors"):
            for l in range(DEPTH):
                srcs = [g.mod[:, l, 16:24], g.A2[:, l, :], g.mod[:, l, 24:32], g.mod[:, l, 40:48]]
                for j, s_ in enumerate(srcs):
                    sc.dma("sp", lambda e, l=l, j=j, s_=s_: e.dma_start(
                        out=io["vecs"][l, j].rearrange("(kc p) -> p kc", p=128), in_=s_, allow_slow_non_contiguous=True),
                        r=["mod", "A2"], w=["vecs"])
        dbg_sb(g, "mod", g.mod[:], ["mod"])
        dbg_sb(g, "A1", g.A1[:], ["A1"])
        sc.barrier()


def load_bc(g, tile_, l, j, key):
    g.sc.dma("sp", lambda e: e.dma_start(out=tile_[:], in_=g.io["vecs"][l, j].partition_broadcast(128)), r=["vecs"], w=[key])


def phase_norm(g, l, xsrc, hT, Acol, Bcol, out_dt_tag="hT"):
    nc, sc, io = g.nc, g.sc, g.io
    NT = g.S // 128
    with ExitStack() as ls:
        if g.S < 2048:
            pad_ = T(ls, nc, "npad", [128, 16384], F32)
        xt = [T(ls, nc, f"nx{i}", [128, D], F32) for i in range(2)]
        xn = [T(ls, nc, f"nxn{i}", [128, D], F32) for i in range(2)]
        junk = T(ls, nc, "njunk", [128, D], BF16)
        st = [T(ls, nc, f"nst{i}", [128, 64], F32) for i in range(2)]
        pst = [PS(ls, nc, f"npst{i}", [128, 512], F32) for i in range(4)]
        for i in range(NT):
            b = i % 2
            sc.dma("sp", lambda e, b=b, i=i: e.dma_start(out=xt[b][:], in_=xsrc[i * 128:(i + 1) * 128, :]), w=[f"nx{b}"])
            sc.op("act", lambda e, b=b: e.activation(out=junk[:], in_=xt[b][:], func=AF.Square, accum_out=st[b][:, 0:1]),
                  r=[f"nx{b}"], w=["njunk", f"nst{b}"])
            sc.op("dve", lambda e, b=b: e.tensor_scalar(out=st[b][:, 16:17], in0=st[b][:, 0:1], scalar1=1.0 / D, scalar2=EPS,
                                                         op0=ALU.mult, op1=ALU.add), r=[f"nst{b}"], w=[f"nst{b}"])
            sc.op("act", lambda e, b=b: e.activation(out=st[b][:, 32:33], in_=st[b][:, 16:17], func=AF.Sqrt), r=[f"nst{b}"], w=[f"nst{b}"])
            sc.op("dve", lambda e, b=b: e.reciprocal(out=st[b][:, 48:49], in_=st[b][:, 32:33]), r=[f"nst{b}"], w=[f"nst{b}"])
            sc.op("dve", lambda e, b=b: e.tensor_scalar(out=xn[b][:], in0=xt[b][:], scalar1=st[b][:, 48:49], scalar2=None, op0=ALU.mult),
                  r=[f"nx{b}", f"nst{b}"], w=[f"nxn{b}"])
            for kc in range(KC):
                pb = (i % 2) * 2 + kc // 4
                sc.op("pe", lambda e, b=b, kc=kc, pb=pb: e.transpose(pst[pb][:, (kc % 4) * 128:(kc % 4 + 1) * 128],
                                                                     xn[b][:, kc * 128:(kc + 1) * 128], g.cst["ident_f"][:]),
                      r=[f"nxn{b}", "c_ident_f"], w=[f"npst{pb}"])
            for kc in range(KC):
                pb = (i % 2) * 2 + kc // 4
                sc.op("act", lambda e, kc=kc, pb=pb, i=i: e.activation(
                    out=hT[:, kc, i * 128:(i + 1) * 128], in_=pst[pb][:, (kc % 4) * 128:(kc % 4 + 1) * 128],
                    func=AF.Identity, bias=Bcol[:, kc:kc + 1], scale=Acol[:, kc:kc + 1]),
                    r=[f"npst{pb}", "mod", "A1", "A2"], w=[f"{out_dt_tag}{i}"])


FM_BLOCKS = []
for _c in range(0, 1024, 512):
    FM_BLOCKS.append((_c, _c // 128, False))
for _c in range(0, 1024, 512):
    FM_BLOCKS.append((1024 + _c, 8 + _c // 128, False))
for _c in range(0, 3072, 512):
    FM_BLOCKS.append((3072 + _c, 16 + _c // 128, False))
for _c in range(0, 1024, 512):
    FM_BLOCKS.append((6144 + _c, 40 + _c // 128, False))
for _c in range(0, 2048, 512):
    FM_BLOCKS.append((7184 + _c, 48 + _c // 128, True))


def phase_proj(g, l, hT):
    nc, sc, io = g.nc, g.sc, g.io
    S = g.S
    NTC = S // 512
    NT = S // 128
    winl = io["w_in"][l].rearrange("(kc p) f -> p kc f", p=128)
    PTv = io["PT"].rearrange("(c p) t -> p c t", p=128)
    with ExitStack() as ls:
        wblk = [T(ls, nc, f"wblk{i}", [128, KC, 512], BF16) for i in range(2)]
        wst = [T(ls, nc, f"wst{i}", [128, KC, 512], F32) for i in range(2)]
        stg = [T(ls, nc, f"pstg{i}", [128, 4, 512], BF16) for i in range(2)]
        pp = [PS(ls, nc, f"pp{i}", [128, 512], F32) for i in range(8)]
        hkeys = lambda tc: [f"hT{i}" for i in range(tc * 4, tc * 4 + 4)]
        n = 0
        for bi, (c0, ch0, sig) in enumerate(FM_BLOCKS):
            wb = bi % 2
            sc.dma("sp", lambda e, wb=wb, c0=c0: e.dma_start(out=wst[wb][:], in_=winl[:, :, c0:c0 + 512]), w=[f"wst{wb}"])
            sc.op("pool", lambda e, wb=wb: e.tensor_copy(out=wblk[wb][:], in_=wst[wb][:]), r=[f"wst{wb}"], w=[f"wblk{wb}"])
            for tc in range(NTC):
                sb = n % 2
                for fc in range(4):
                    pb = (n % 2) * 4 + fc
                    for kc in range(KC):
                        sc.op("pe", lambda e, wb=wb, fc=fc, kc=kc, pb=pb, tc=tc: e.matmul(
                            pp[pb][:], lhsT=wblk[wb][:, kc, fc * 128:(fc + 1) * 128], rhs=hT[:, kc, tc * 512:(tc + 1) * 512],
                            start=(kc == 0), stop=(kc == KC - 1)), r=[f"wblk{wb}"] + hkeys(tc), w=[f"pp{pb}"])
                    if sig:
                        sc.op("act", lambda e, sb=sb, fc=fc, pb=pb: e.activation(out=stg[sb][:, fc, :], in_=pp[pb][:], func=AF.Sigmoid),
                              r=[f"pp{pb}"], w=[f"pstg{sb}"])
                    elif fc % 2 == 0:
                        sc.op("act", lambda e, sb=sb, fc=fc, pb=pb: e.activation(out=stg[sb][:, fc, :], in_=pp[pb][:], func=AF.Copy),
                              r=[f"pp{pb}"], w=[f"pstg{sb}"])
                    else:
                        sc.op("dve", lambda e, sb=sb, fc=fc, pb=pb: e.tensor_copy(out=stg[sb][:, fc, :], in_=pp[pb][:]),
                              r=[f"pp{pb}"], w=[f"pstg{sb}"])
                sc.dma("sp", lambda e, sb=sb, ch0=ch0, tc=tc: e.dma_start(
                    out=PTv[:, ch0:ch0 + 4, tc * 512:(tc + 1) * 512], in_=stg[sb][:]), r=[f"pstg{sb}"], w=["PT"])
                n += 1
    sc.barrier()
    with ExitStack() as ls:
        wv = T(ls, nc, "wv", [128, KC, 1024], BF16)
        wbd = T(ls, nc, "wbd", [128, KC, 16], BF16)
        vst = [T(ls, nc, f"vst{i}", [128, 1024], BF16) for i in range(2)]
        bst = [T(ls, nc, f"bst{i}", [128, 16], F32) for i in range(2)]
        pv = [PS(ls, nc, f"pv{i}", [128, 512], F32) for i in range(6)]
        wst2 = [T(ls, nc, f"wst2{i}", [128, KC, 512], F32) for i in range(2)]
        wbdf = T(ls, nc, "wbdf", [128, KC, 16], F32)
        for hf in range(2):
            sc.dma("sp", lambda e, hf=hf: e.dma_start(out=wst2[hf][:], in_=winl[:, :, 2048 + hf * 512:2048 + (hf + 1) * 512]), w=[f"wst2{hf}"])
            sc.op("pool", lambda e, hf=hf: e.tensor_copy(out=wv[:, :, hf * 512:(hf + 1) * 512], in_=wst2[hf][:]), r=[f"wst2{hf}"], w=["wv"])
        sc.dma("sp", lambda e: e.dma_start(out=wbdf[:], in_=winl[:, :, 7168:7184], allow_slow_non_contiguous=True), w=["wbdf"])
        sc.op("pool", lambda e: e.tensor_copy(out=wbd[:], in_=wbdf[:]), r=["wbdf"], w=["wbd"])
        for i in range(NT):
            b = i % 2
            for hf in range(2):
                pb = b * 3 + hf
                for kc in range(KC):
                    sc.op("pe", lambda e, kc=kc, hf=hf, pb=pb, i=i: e.matmul(
                        pv[pb][:], lhsT=hT[:, kc, i * 128:(i + 1) * 128], rhs=wv[:, kc, hf * 512:(hf + 1) * 512],
                        start=(kc == 0), stop=(kc == KC - 1)), r=["wv", f"hT{i}"], w=[f"pv{pb}"])
            pb2 = b * 3 + 2
            for kc in range(KC):
                sc.op("pe", lambda e, kc=kc, pb2=pb2, i=i: e.matmul(
                    pv[pb2][:, 0:16], lhsT=hT[:, kc, i * 128:(i + 1) * 128], rhs=wbd[:, kc, :],
                    start=(kc == 0), stop=(kc == KC - 1)), r=["wbd", f"hT{i}"], w=[f"pv{pb2}"])
            sc.op("act", lambda e, b=b: e.activation(out=vst[b][:, 0:512], in_=pv[b * 3][:], func=AF.Copy), r=[f"pv{b*3}"], w=[f"vst{b}"])
            sc.op("dve", lambda e, b=b: e.tensor_copy(out=vst[b][:, 512:1024], in_=pv[b * 3 + 1][:]), r=[f"pv{b*3+1}"], w=[f"vst{b}"])
            sc.op("dve", lambda e, b=b, pb2=pb2: e.tensor_copy(out=bst[b][:], in_=pv[pb2][:, 0:16]), r=[f"pv{pb2}"], w=[f"bst{b}"])
            sc.dma("sp", lambda e, b=b, i=i: e.dma_start(out=io["Vtok"][i * 128:(i + 1) * 128, :], in_=vst[b][:]), r=[f"vst{b}"], w=["Vtok"])
            sc.dma("sp", lambda e, b=b, i=i: e.dma_start(out=io["BD"][i * 128:(i + 1) * 128, :], in_=bst[b][:]), r=[f"bst{b}"], w=["BD"])


GRP = [128, 256, 512, 512, 512, 512, 512, 512]


def phase_attn(g, l):
    nc, sc, io = g.nc, g.sc, g.io
    S = g.S
    NT = S // 128
    NQC = S // 512
    lam_init = 0.8 - 0.6 * math.exp(-0.3 * l)
    Vv = io["Vtok"].rearrange("(i p) f -> p i f", p=128)
    with ExitStack() as ls:
        QT = [T(ls, nc, f"aQT{i}", [128, S], BF16) for i in range(2)]
        KT = [T(ls, nc, f"aKT{i}", [128, S], BF16) for i in range(2)]
        VT = [T(ls, nc, f"aVT{i}", [128, NT, 129], BF16) for i in range(2)]
        pT = [T(ls, nc, f"apT{i}", [128, 512], BF16) for i in range(3)]
        O1 = [T(ls, nc, f"aO1{j}", [128, 128], F32) for j in range(4)]
        Ot = [T(ls, nc, f"aO{j}", [128, 128], F32) for j in range(4)]
        yb = [T(ls, nc, f"ayb{j}", [128, 128], BF16) for j in range(4)]
        sq = T(ls, nc, "asq", [128, 128], BF16)
        stt = [T(ls, nc, f"ast{j}", [128, 128], F32) for j in range(4)]
        yst = [T(ls, nc, f"ayst{i}", [128, 512], BF16) for i in range(2)]
        lamt = T(ls, nc, "alam", [128, 4, 64], F32)
        lamp = T(ls, nc, "alamp", [128, 2, 64], F32)
        lams = T(ls, nc, "alams", [128, 128], F32)
        subw = T(ls, nc, "asubw", [128, 128], F32)
        ps_s = [PS(ls, nc, f"aps{i}", [128, 512], F32) for i in range(2)]
        ps_o = [PS(ls, nc, f"apo{j}", [128, 512], F32) for j in range(4)]
        ps_t = PS(ls, nc, "apt", [128, 1024], BF16)
        sc.dma("sp", lambda e: e.dma_start(out=lamt[:], in_=io["lamv"][l]), w=["alam"])
        sc.dma("sp", lambda e: e.dma_start(out=subw[:], in_=io["subln_bc"][l]), w=["asubw"])
        sc.op("dve", lambda e: e.tensor_tensor(out=lamp[:, 0, :], in0=lamt[:, 0, :], in1=lamt[:, 1, :], op=ALU.mult), r=["alam"], w=["alamp"])
        sc.op("dve", lambda e: e.tensor_tensor(out=lamp[:, 1, :], in0=lamt[:, 2, :], in1=lamt[:, 3, :], op=ALU.mult), r=["alam"], w=["alamp"])
        sc.op("dve", lambda e: e.tensor_reduce(out=lams[:, 0:1], in_=lamp[:, 0, :], axis=AX.X, op=ALU.add), r=["alamp"], w=["alams"])
        sc.op("dve", lambda e: e.tensor_reduce(out=lams[:, 16:17], in_=lamp[:, 1, :], axis=AX.X, op=ALU.add), r=["alamp"], w=["alams"])
        sc.op("act", lambda e: e.activation(out=lams[:, 32:33], in_=lams[:, 0:1], func=AF.Exp), r=["alams"], w=["alams"])
        sc.op("act", lambda e: e.activation(out=lams[:, 48:49], in_=lams[:, 16:17], func=AF.Exp), r=["alams"], w=["alams"])
        sc.op("dve", lambda e: e.tensor_tensor(out=lams[:, 64:65], in0=lams[:, 48:49], in1=lams[:, 32:33], op=ALU.subtract), r=["alams"], w=["alams"])
        sc.op("dve", lambda e: e.tensor_scalar(out=lams[:, 80:81], in0=lams[:, 64:65], scalar1=-lam_init, scalar2=None, op0=ALU.add),
              r=["alams"], w=["alams"])
        neglam = lams[:, 80:81]
        sc.op("dve", lambda e: e.tensor_scalar(out=subw[:], in0=subw[:], scalar1=(1.0 - lam_init), scalar2=None, op0=ALU.mult),
              r=["asubw"], w=["asubw"])
        for hb in range(2):
            sc.op("pool", lambda e, hb=hb: e.memset(VT[hb][:, :, 128:129], 1.0), w=[f"aVT{hb}"])
        n_u = 0
        n_y = 0
        for h in range(NH):
            hb = h % 2
            sc.dma("sp", lambda e, hb=hb, h=h: e.dma_start(out=QT[hb][:], in_=io["PT"][h * 128:(h + 1) * 128, :]), r=["PT"], w=[f"aQT{hb}"])
            sc.dma("sp", lambda e, hb=hb, h=h: e.dma_start(out=KT[hb][:], in_=io["PT"][(8 + h) * 128:(9 + h) * 128, :]), r=["PT"], w=[f"aKT{hb}"])
            sc.dma("sp", lambda e, hb=hb, h=h: e.dma_start(out=VT[hb][:, :, 0:128], in_=Vv[:, :, h * 128:(h + 1) * 128]), r=["Vtok"], w=[f"aVT{hb}"])
            G = GRP[h]
            for qc in range(NQC):
                for m in range(2):
                    nkb = 4 * qc + 4
                    for kb in range(nkb):
                        kl = kb - 4 * qc
                        j0 = max(0, kl)
                        sb = n_u % 2
                        pb = n_u % 3
                        n_u += 1
                        c0 = j0 * 128
                        sc.op("pe", lambda e, hb=hb, m=m, kb=kb, qc=qc, sb=sb, c0=c0: e.matmul(
                            ps_s[sb][:, c0:512], lhsT=KT[hb][m * 64:(m + 1) * 64, kb * 128:(kb + 1) * 128],
                            rhs=QT[hb][m * 64:(m + 1) * 64, qc * 512 + c0:qc * 512 + 512], start=True, stop=True),
                            r=[f"aKT{hb}", f"aQT{hb}"], w=[f"aps{sb}"])
                        cs = c0
                        while cs < 512:
                            ce = min(512, (cs // G + 1) * G)
                            nn = (512 * qc + ce - 128 * kb) // 128
                            col = h * 64 + nn - 1
                            sc.op("act", lambda e, sb=sb, pb=pb, cs=cs, ce=ce, col=col: e.activation(
                                out=pT[pb][:, cs:ce], in_=ps_s[sb][:, cs:ce], func=AF.Exp,
                                bias=g.cst["alibi"][:, col:col + 1], scale=0.125), r=[f"aps{sb}", "c_alibi"], w=[f"apT{pb}"])
                            cs = ce
                        if kl >= 0:
                            sc.op("pool", lambda e, pb=pb, kl=kl: e.tensor_tensor(
                                out=pT[pb][:, kl * 128:(kl + 1) * 128], in0=pT[pb][:, kl * 128:(kl + 1) * 128],
                                in1=g.cst["triu_b"][:], op=ALU.mult), r=[f"apT{pb}", "c_triu_b"], w=[f"apT{pb}"])
                        for j in range(j0, 4):
                            sc.op("pe", lambda e, pb=pb, j=j, hb=hb, kb=kb, qc=qc: e.matmul(
                                ps_o[j][:, 0:129], lhsT=pT[pb][:, j * 128:(j + 1) * 128], rhs=VT[hb][:, kb, :],
                                start=(kb == 0), stop=(kb == 4 * qc + j)), r=[f"apT{pb}", f"aVT{hb}"], w=[f"apo{j}"])
                    for j in range(4):
                        st_ = stt[j]
                        sc.op("dve", lambda e, j=j, st_=st_: e.reciprocal(out=st_[:, 0:1], in_=ps_o[j][:, 128:129]), r=[f"apo{j}"], w=[f"ast{j}"])
                        if m == 0:
                            sc.op("dve", lambda e, j=j, st_=st_: e.tensor_scalar(out=O1[j][:], in0=ps_o[j][:, 0:128], scalar1=st_[:, 0:1],
                                                                                scalar2=None, op0=ALU.mult), r=[f"apo{j}", f"ast{j}"], w=[f"aO1{j}"])
                        else:
                            sc.op("dve", lambda e, st_=st_: e.tensor_tensor(out=st_[:, 16:17], in0=st_[:, 0:1], in1=neglam, op=ALU.mult),
                                  r=[f"ast{j}", "alams"], w=[f"ast{j}"])
                            sc.op("dve", lambda e, j=j, st_=st_: e.scalar_tensor_tensor(
                                out=Ot[j][:], in0=ps_o[j][:, 0:128], scalar=st_[:, 16:17], in1=O1[j][:], op0=ALU.mult, op1=ALU.add),
                                r=[f"apo{j}", f"ast{j}", f"aO1{j}"], w=[f"aO{j}"])
                            sc.op("act", lambda e, j=j, st_=st_: e.activation(out=sq[:], in_=Ot[j][:], func=AF.Square, accum_out=st_[:, 32:33]),
                                  r=[f"aO{j}"], w=["asq", f"ast{j}"])
                            sc.op("dve", lambda e, st_=st_: e.tensor_scalar(out=st_[:, 48:49], in0=st_[:, 32:33], scalar1=1.0 / 128, scalar2=EPS,
                                                                         op0=ALU.mult, op1=ALU.add), r=[f"ast{j}"], w=[f"ast{j}"])
                            sc.op("act", lambda e, st_=st_: e.activation(out=st_[:, 64:65], in_=st_[:, 48:49], func=AF.Ln), r=[f"ast{j}"], w=[f"ast{j}"])
                            sc.op("act", lambda e, st_=st_: e.activation(out=st_[:, 80:81], in_=st_[:, 64:65], func=AF.Exp, scale=-0.5),
                                  r=[f"ast{j}"], w=[f"ast{j}"])
                            sc.op("dve", lambda e, j=j, st_=st_: e.scalar_tensor_tensor(
                                out=yb[j][:], in0=Ot[j][:], scalar=st_[:, 80:81], in1=subw[:], op0=ALU.mult, op1=ALU.mult),
                                r=[f"aO{j}", f"ast{j}", "asubw"], w=[f"ayb{j}"])
                            sc.op("pe", lambda e, j=j: e.transpose(ps_t[:, j * 128:(j + 1) * 128], yb[j][:], g.cst["ident_b"][:]),
                                  r=[f"ayb{j}", "c_ident_b"], w=["apt"])
                    if m == 1:
                        ysb = n_y % 2
                        n_y += 1
                        sc.op("dve", lambda e, ysb=ysb: e.tensor_copy(out=yst[ysb][:], in_=ps_t[:, 0:512]), r=["apt"], w=[f"ayst{ysb}"])
                        sc.dma("sp", lambda e, ysb=ysb, h=h, qc=qc: e.dma_start(
                            out=io["YT"][h * 128:(h + 1) * 128, qc * 512:(qc + 1) * 512], in_=yst[ysb][:]), r=[f"ayst{ysb}"], w=["YT"])


def phase_gdn(g, l):
    nc, sc, io = g.nc, g.sc, g.io
    S = g.S
    NT = S // 128
    CW = min(2048, S)
    NCW = S // CW
    cst = g.cst
    with ExitStack() as ls:
        SC = T(ls, nc, "gSC", [128, NT, 5, 8], F32)
        nega = T(ls, nc, "gnega", [128, 8], F32)
        dtb = T(ls, nc, "gdtb", [128, 8], F32)
        convw = T(ls, nc, "gconvw", [128, 24, 4], F32)
        gnw = T(ls, nc, "ggnw", [128, 1], F32)
        sc.dma("sp", lambda e: e.dma_start(out=nega[:], in_=io["alog_bc"][l]), w=["gnega"])
        sc.dma("sp", lambda e: e.dma_start(out=dtb[:], in_=io["dtb_bc"][l]), w=["gdtb"])
        sc.dma("sp", lambda e: e.dma_start(out=convw[:], in_=io["conv_t"][l]), w=["gconvw"])
        sc.dma("sp", lambda e: e.dma_start(out=gnw[:], in_=io["gnw_t"][l]), w=["ggnw"])
        sc.op("act", lambda e: e.activation(out=nega[:], in_=nega[:], func=AF.Exp), r=["gnega"], w=["gnega"])
        sc.op("dve", lambda e: e.tensor_scalar(out=nega[:], in0=nega[:], scalar1=-1.0, scalar2=None, op0=ALU.mult), r=["gnega"], w=["gnega"])
        with ExitStack() as la:
            bd = [T(la, nc, f"gbd{i}", [128, 16], F32) for i in range(2)]
            tg = [T(la, nc, f"gtg{i}", [128, 4, 8], F32) for i in range(2)]
            psa = [PS(la, nc, f"gpsa{i}", [128, 512], F32) for i in range(2)]
            for i in range(NT):
                b = i % 2
                sc.dma("sp", lambda e, b=b, i=i: e.dma_start(out=bd[b][:], in_=io["BD"][i * 128:(i + 1) * 128, :]), r=["BD"], w=[f"gbd{b}"])
                sc.op("act", lambda e, b=b, i=i: e.activation(out=SC[:, i, 2, :], in_=bd[b][:, 0:8], func=AF.Sigmoid), r=[f"gbd{b}"], w=[f"gSC{i}"])
                sc.op("dve", lambda e, b=b: e.tensor_tensor(out=tg[b][:, 0, :], in0=bd[b][:, 8:16], in1=dtb[:], op=ALU.add), r=[f"gbd{b}", "gdtb"], w=[f"gtg{b}"])
                sc.op("act", lambda e, b=b: e.activation(out=tg[b][:, 1, :], in_=tg[b][:, 0, :], func=AF.Exp), r=[f"gtg{b}"], w=[f"gtg{b}"])
                sc.op("dve", lambda e, b=b: e.tensor_scalar(out=tg[b][:, 1, :], in0=tg[b][:, 1, :], scalar1=1.0, scalar2=None, op0=ALU.add), r=[f"gtg{b}"], w=[f"gtg{b}"])
                sc.op("act", lambda e, b=b: e.activation(out=tg[b][:, 2, :], in_=tg[b][:, 1, :], func=AF.Ln), r=[f"gtg{b}"], w=[f"gtg{b}"])
                sc.op("dve", lambda e, b=b: e.tensor_tensor(out=tg[b][:, 3, :], in0=tg[b][:, 2, :], in1=nega[:], op=ALU.mult), r=[f"gtg{b}", "gnega"], w=[f"gtg{b}"])
                sc.op("pe", lambda e, b=b: e.matmul(psa[b][:, 0:8], lhsT=cst["Lcum"][:], rhs=tg[b][:, 3, :], start=True, stop=True),
                      r=[f"gtg{b}", "c_Lcum"], w=[f"gpsa{b}"])
                sc.op("pe", lambda e, b=b: e.matmul(psa[b][:, 8:16], lhsT=cst["Lall"][:], rhs=tg[b][:, 3, :], start=True, stop=True),
                      r=[f"gtg{b}", "c_Lall"], w=[f"gpsa{b}"])
                sc.op("dve", lambda e, b=b, i=i: e.tensor_copy(out=SC[:, i, 0, :], in_=psa[b][:, 0:8]), r=[f"gpsa{b}"], w=[f"gSC{i}"])
                sc.op("dve", lambda e, b=b, i=i: e.tensor_scalar(out=SC[:, i, 1, :], in0=psa[b][:, 0:8], scalar1=-1.0, scalar2=None, op0=ALU.mult),
                      r=[f"gpsa{b}"], w=[f"gSC{i}"])
                sc.op("act", lambda e, b=b, i=i: e.activation(out=SC[:, i, 3, :], in_=psa[b][:, 0:8], func=AF.Exp), r=[f"gpsa{b}"], w=[f"gSC{i}"])
                sc.op("dve", lambda e, i=i: e.tensor_tensor(out=SC[:, i, 3, :], in0=SC[:, i, 3, :], in1=SC[:, i, 2, :], op=ALU.mult), r=[f"gSC{i}"], w=[f"gSC{i}"])
                sc.op("dve", lambda e, b=b, i=i: e.tensor_tensor(out=SC[:, i, 4, :], in0=psa[b][:, 8:16], in1=SC[:, i, 0, :], op=ALU.subtract),
                      r=[f"gpsa{b}", f"gSC{i}"], w=[f"gSC{i}"])
                sc.op("act", lambda e, i=i: e.activation(out=SC[:, i, 4, :], in_=SC[:, i, 4, :], func=AF.Exp), r=[f"gSC{i}"], w=[f"gSC{i}"])
        sc.barrier()
        if getattr(g, "gdn_stop", None) == "A":
            return
        raw = T(ls, nc, "graw", [128, S + 3], BF16)
        QTn = T(ls, nc, "gQTn", [128, S], BF16)
        KTn = T(ls, nc, "gKTn", [128, S], BF16)
        Ktok = T(ls, nc, "gKtok", [128, NT, 128], BF16)
        Vtk = T(ls, nc, "gVtk", [128, NT, 128], BF16)
        zsT = T(ls, nc, "gzsT", [128, S], BF16)
        acc = T(ls, nc, "gacc", [128, CW], F32)
        yv = T(ls, nc, "gyv", [128, CW], F32)
        ybf = T(ls, nc, "gybf", [128, CW], BF16)
        sqb = T(ls, nc, "gsqb", [128, 512], BF16)
        rnt = T(ls, nc, "grnt", [128, 512], F32)
        Sf = T(ls, nc, "gSf", [128, 128], F32)
        Sb = T(ls, nc, "gSb", [128, 128], BF16)
        names = ["dg", "db", "E", "decT", "eG", "Bst", "t1", "u"]
        f32t = {n: T(ls, nc, "g_" + n, [128, 128], F32) for n in names}
        bnames = ["AT", "A", "ATn", "An", "TT", "intraT", "vb", "kbg", "kdec", "qgT", "wT", "vn", "sq2", "yo"]
        bft = {n: T(ls, nc, "g_" + n, [128, 128], BF16) for n in bnames}
        Fu = [f32t["u"], T(ls, nc, "g_u1", [128, 128], F32)]
        FeG = [f32t["eG"], T(ls, nc, "g_eG1", [128, 128], F32)]
        BwT = [bft["wT"], T(ls, nc, "g_wT1", [128, 128], BF16)]
        Bkd = [bft["kdec"], T(ls, nc, "g_kdec1", [128, 128], BF16)]
        Bqg = [bft["qgT"], T(ls, nc, "g_qgT1", [128, 128], BF16)]
        Bin = [bft["intraT"], T(ls, nc, "g_intraT1", [128, 128], BF16)]
        rn2 = T(ls, nc, "g_rn2", [128, 128], F32)
        t2 = T(ls, nc, "g_t2", [128, 128], F32)
        ystg = [T(ls, nc, f"gystg{i}", [128, 512], BF16) for i in range(2)]
        pG = PS(ls, nc, "gpG", [128, 512], F32)
        pK = PS(ls, nc, "gpK", [128, 512], F32)
        pP = PS(ls, nc, "gpP", [128, 512], F32)
        pTu = PS(ls, nc, "gpTu", [128, 512], F32)
        pU = PS(ls, nc, "gpU", [128, 512], F32)
        pV = PS(ls, nc, "gpV", [128, 512], F32)
        pO = PS(ls, nc, "gpO", [128, 512], F32)
        pB = PS(ls, nc, "gpB", [128, 1024], BF16)
        sc.op("pool", lambda e: e.memset(raw[:, 0:3], 0.0), w=["graw"])
        vnc = [T(ls, nc, f"g_vnc{i}", [128, 128], BF16) for i in range(2)]
        for i_ in range(2):
            sc.op("pool", lambda e, i_=i_: e.memset(vnc[i_][:], 0.0), w=[f"g_vn{i_}"])
        n_st = 0
        for h in range(NH):
            for which in range(3):
                chunk = 16 + which * 8 + h
                sc.dma("sp", lambda e, chunk=chunk: e.dma_start(out=raw[:, 3:3 + S], in_=io["PT"][chunk * 128:(chunk + 1) * 128, :]), r=["PT"], w=["graw"])
                cc = which * 8 + h
                for cw in range(NCW):
                    c0 = cw * CW
                    sc.op("dve", lambda e, c0=c0, cc=cc: e.tensor_scalar(out=acc[:], in0=raw[:, c0:c0 + CW], scalar1=convw[:, cc, 0:1], scalar2=None,
                                                                      op0=ALU.mult), r=["graw", "gconvw"], w=["gacc"])
                    for k in range(1, 4):
                        sc.op("dve", lambda e, c0=c0, cc=cc, k=k: e.scalar_tensor_tensor(
                            out=acc[:], in0=raw[:, c0 + k:c0 + k + CW], scalar=convw[:, cc, k:k + 1], in1=acc[:], op0=ALU.mult, op1=ALU.add),
                            r=["graw", "gconvw", "gacc"], w=["gacc"])
                    if which == 2:
                        sc.op("act", lambda e: e.activation(out=ybf[:], in_=acc[:], func=AF.Silu), r=["gacc"], w=["gybf"])
                        for tt in range(CW // 128):
                            ti = c0 // 128 + tt
                            sc.op("pe", lambda e, tt=tt: e.transpose(pB[:, (tt % 4) * 128:(tt % 4 + 1) * 128], ybf[:, tt * 128:(tt + 1) * 128], cst["ident_b"][:]),
                                  r=["gybf", "c_ident_b"], w=["gpB"])
                            sc.op("act", lambda e, tt=tt, ti=ti: e.activation(out=Vtk[:, ti, :], in_=pB[:, (tt % 4) * 128:(tt % 4 + 1) * 128], func=AF.Copy),
                                  r=["gpB"], w=["gVtk"])
                    else:
                        dst = QTn if which == 0 else KTn
                        dkey = "gQTn" if which == 0 else "gKTn"
                        sc.op("act", lambda e: e.activation(out=yv[:], in_=acc[:], func=AF.Silu), r=["gacc"], w=["gyv"])
                        for sbk in range(CW // 512):
                            cs = sbk * 512
                            sc.op("pool", lambda e, cs=cs: e.tensor_tensor(out=sqb[:], in0=yv[:, cs:cs + 512], in1=yv[:, cs:cs + 512], op=ALU.mult),
                                  r=["gyv"], w=["gsqb"])
                            sc.op("pe", lambda e: e.matmul(pTu[:, 0:512], lhsT=g.ones_b[:], rhs=sqb[:], start=True, stop=True), r=["gsqb", "ones_b"], w=["gpTu"])
                            sc.op("dve", lambda e: e.tensor_scalar(out=rnt[:], in0=pTu[:, 0:512], scalar1=EPS, scalar2=None, op0=ALU.add), r=["gpTu"], w=["grnt"])
                            sc.op("act", lambda e: e.activation(out=rnt[:], in_=rnt[:], func=AF.Ln), r=["grnt"], w=["grnt"])
                            sc.op("act", lambda e: e.activation(out=rnt[:], in_=rnt[:], func=AF.Exp, scale=-0.5), r=["grnt"], w=["grnt"])
                            qs = (128.0 ** -0.5) if which == 0 else 1.0
                            sc.op("dve", lambda e, cs=cs, c0=c0, dst=dst, qs=qs: e.scalar_tensor_tensor(
                                out=dst[:, c0 + cs:c0 + cs + 512], in0=yv[:, cs:cs + 512], scalar=qs, in1=rnt[:], op0=ALU.mult, op1=ALU.mult),
                                r=["gyv", "grnt"], w=[dkey])
                        if which == 1:
                            for tt in range(CW // 128):
                                ti = c0 // 128 + tt
                                sc.op("pe", lambda e, tt=tt, ti=ti: e.transpose(pB[:, (tt % 4) * 128:(tt % 4 + 1) * 128], KTn[:, ti * 128:(ti + 1) * 128], cst["ident_b"][:]),
                                      r=["gKTn", "c_ident_b"], w=["gpB"])
                                sc.op("act", lambda e, tt=tt, ti=ti: e.activation(out=Ktok[:, ti, :], in_=pB[:, (tt % 4) * 128:(tt % 4 + 1) * 128], func=AF.Copy),
                                      r=["gpB"], w=["gKtok"])
            zc = 40 + h
            sc.dma("sp", lambda e, zc=zc: e.dma_start(out=raw[:, 3:3 + S], in_=io["PT"][zc * 128:(zc + 1) * 128, :]), r=["PT"], w=["graw"])
            sc.op("act", lambda e: e.activation(out=zsT[:], in_=raw[:, 3:3 + S], func=AF.Silu), r=["graw"], w=["gzsT"])
            sc.op("pool", lambda e: e.memset(Sf[:], 0.0), w=["gSf"])
            sc.op("pool", lambda e: e.memset(Sb[:], 0.0), w=["gSb"])
            if getattr(g, "gdn_stop", None) == "B":
                return
            F = f32t
            Bt = bft
            def tpart(i):
                par = i % 2
                tsl = slice(i * 128, (i + 1) * 128)
                sck = [f"gSC{i}"]
                gc_col = SC[:, i, 0, h:h + 1]
                ngc_col = SC[:, i, 1, h:h + 1]
                be_col = SC[:, i, 2, h:h + 1]
                bege_col = SC[:, i, 3, h:h + 1]
                kd_col = SC[:, i, 4, h:h + 1]
                sc.op("dve", lambda e: e.tensor_scalar(out=F["dg"][:], in0=cst["ident_f"][:], scalar1=gc_col, scalar2=None, op0=ALU.mult), r=sck + ["c_ident_f"], w=["g_dg"])
                yield
                sc.op("dve", lambda e: e.tensor_scalar(out=F["db"][:], in0=cst["ident_f"][:], scalar1=be_col, scalar2=None, op0=ALU.mult), r=sck + ["c_ident_f"], w=["g_db"])
                yield
                sc.op("pe", lambda e: e.matmul(pG[:, 0:128], lhsT=g.ones_f[:], rhs=F["dg"][:], start=True, stop=True), r=["g_dg", "ones_f"], w=["gpG"])
                yield
                sc.op("pe", lambda e: e.matmul(pG[:, 128:256], lhsT=g.ones_f[:], rhs=F["db"][:], start=True, stop=True), r=["g_db", "ones_f"], w=["gpG"])
                yield
                sc.op("dve", lambda e: e.tensor_tensor(out=F["E"][:], in0=pG[:, 0:128], in1=cst["negmaskU"][:], op=ALU.add), r=["gpG", "c_negmaskU"], w=["g_E"])
                yield
                sc.op("act", lambda e: e.activation(out=F["decT"][:], in_=F["E"][:], func=AF.Exp, bias=ngc_col, scale=1.0), r=["g_E"] + sck, w=["g_decT"])
                yield
                sc.op("act", lambda e: e.activation(out=FeG[par][:], in_=pG[:, 0:128], func=AF.Exp), r=["gpG"], w=[f"g_eG{par}"])
                yield
                sc.op("dve", lambda e: e.tensor_tensor(out=F["Bst"][:], in0=pG[:, 128:256], in1=cst["strictU"][:], op=ALU.mult), r=["gpG", "c_strictU"], w=["g_Bst"])
                yield
                if g.gdn_stop == "C1":
                    return
                sc.op("pe", lambda e, tsl=tsl: e.matmul(pK[:, 0:128], lhsT=KTn[:, tsl], rhs=KTn[:, tsl], start=True, stop=True), r=["gKTn"], w=["gpK"])
                yield
                sc.op("pe", lambda e, tsl=tsl: e.matmul(pK[:, 128:256], lhsT=KTn[:, tsl], rhs=QTn[:, tsl], start=True, stop=True), r=["gKTn", "gQTn"], w=["gpK"])
                yield
                sc.op("dve", lambda e: e.tensor_tensor(out=F["t1"][:], in0=pK[:, 0:128], in1=F["decT"][:], op=ALU.mult), r=["gpK", "g_decT"], w=["g_t1"])
                yield
                sc.op("dve", lambda e: e.scalar_tensor_tensor(out=Bt["ATn"][:], in0=F["t1"][:], scalar=-1.0, in1=F["Bst"][:], op0=ALU.mult, op1=ALU.mult),
                      r=["g_t1", "g_Bst"], w=["g_ATn"])
                yield
                sc.op("dve", lambda e: e.tensor_tensor(out=Bin[par][:], in0=pK[:, 128:256], in1=F["decT"][:], op=ALU.mult), r=["gpK", "g_decT"], w=[f"g_intraT{par}"])
                yield
                sc.op("pe", lambda e: e.transpose(pB[:, 512:640], Bt["ATn"][:], cst["ident_b"][:]), r=["g_ATn", "c_ident_b"], w=["gpB"])
                yield
                sc.op("act", lambda e: e.activation(out=Bt["An"][:], in_=pB[:, 512:640], func=AF.Copy), r=["gpB"], w=["g_An"])
                yield
                sc.op("dve", lambda e: e.tensor_tensor(out=Bt["TT"][:], in0=Bt["ATn"][:], in1=cst["ident_b"][:], op=ALU.add), r=["g_ATn", "c_ident_b"], w=["g_TT"])
                yield
                if g.gdn_stop == "C2":
                    return
                Pk, PTk = "An", "ATn"
                Pn, PTn = "A", "AT"
                for k in range(1, 6):
                    sc.op("pe", lambda e, Pk=Pk, PTk=PTk: e.matmul(pP[:, 0:128], lhsT=Bt[PTk][:], rhs=Bt[Pk][:], start=True, stop=True),
                          r=["g_" + Pk, "g_" + PTk], w=["gpP"])
                    yield
                    if k < 5:
                        sc.op("pe", lambda e, Pk=Pk, PTk=PTk: e.matmul(pP[:, 128:256], lhsT=Bt[Pk][:], rhs=Bt[PTk][:], start=True, stop=True),
                              r=["g_" + Pk, "g_" + PTk], w=["gpP"])
                        yield
                    sc.op("act", lambda e, Pn=Pn: e.activation(out=Bt[Pn][:], in_=pP[:, 0:128], func=AF.Copy), r=["gpP"], w=["g_" + Pn])
                    yield
                    if k < 5:
                        sc.op("dve", lambda e, PTn=PTn: e.tensor_copy(out=Bt[PTn][:], in_=pP[:, 128:256]), r=["gpP"], w=["g_" + PTn])
                        yield
                    sc.op("pe", lambda e, Pn=Pn: e.matmul(pTu[:, 0:128], lhsT=Bt[Pn][:], rhs=Bt["TT"][:], start=True, stop=True), r=["g_" + Pn, "g_TT"], w=["gpTu"])
                    yield
                    sc.op("dve", lambda e: e.tensor_tensor(out=Bt["TT"][:], in0=pTu[:, 0:128], in1=Bt["TT"][:], op=ALU.add), r=["gpTu", "g_TT"], w=["g_TT"])
                    yield
                    Pk, PTk, Pn, PTn = Pn, PTn, Pk, PTk
                if g.gdn_stop == "C3":
                    return
                sc.op("dve", lambda e, i=i: e.tensor_scalar(out=Bt["vb"][:], in0=Vtk[:, i, :], scalar1=be_col, scalar2=None, op0=ALU.mult), r=["gVtk"] + sck, w=["g_vb"])
                yield
                sc.op("dve", lambda e, i=i: e.tensor_scalar(out=Bt["kbg"][:], in0=Ktok[:, i, :], scalar1=bege_col, scalar2=None, op0=ALU.mult), r=["gKtok"] + sck, w=["g_kbg"])
                yield
                sc.op("dve", lambda e, i=i: e.tensor_scalar(out=Bkd[par][:], in0=Ktok[:, i, :], scalar1=kd_col, scalar2=None, op0=ALU.mult), r=["gKtok"] + sck, w=[f"g_kdec{par}"])
                yield
                sc.op("dve", lambda e, tsl=tsl: e.tensor_tensor(out=Bqg[par][:], in0=QTn[:, tsl], in1=FeG[par][:], op=ALU.mult), r=["gQTn", f"g_eG{par}"], w=[f"g_qgT{par}"])
                yield
                sc.op("pe", lambda e: e.matmul(pU[:, 0:128], lhsT=Bt["TT"][:], rhs=Bt["vb"][:], start=True, stop=True), r=["g_TT", "g_vb"], w=["gpU"])
                yield
                sc.op("pe", lambda e: e.matmul(pU[:, 128:256], lhsT=Bt["kbg"][:], rhs=Bt["TT"][:], start=True, stop=True), r=["g_TT", "g_kbg"], w=["gpU"])
                yield
                sc.op("act", lambda e: e.activation(out=Fu[par][:], in_=pU[:, 0:128], func=AF.Copy), r=["gpU"], w=[f"g_u{par}"])
                yield
                sc.op("dve", lambda e: e.tensor_copy(out=BwT[par][:], in_=pU[:, 128:256]), r=["gpU"], w=[f"g_wT{par}"])
                yield
            def rpart(i):
                nonlocal n_st
                par = i % 2
                tsl = slice(i * 128, (i + 1) * 128)
                if g.gdn_stop == "C4":
                    return
                for cj in range(2):
                    r0 = cj * 64
                    rs = slice(r0, r0 + 64)
                    vn = vnc[cj]
                    vk = f"g_vn{cj}"
                    sc.op("pe", lambda e: e.matmul(pV[:, 0:128], lhsT=BwT[par][:], rhs=Sb[:], start=True, stop=True), r=[f"g_wT{par}", "gSb"], w=["gpV"])
                    yield
                    sc.op("dve", lambda e: e.scalar_tensor_tensor(out=t2[:], in0=pV[:, 0:128], scalar=-1.0, in1=Fu[par][:], op0=ALU.mult, op1=ALU.add), r=[f"g_u{par}", "gpV"], w=["g_t2"])
                    yield
                    sc.op("dve", lambda e, cj=cj, vn=vn: e.tensor_scalar(out=vn[:], in0=t2[:], scalar1=cst["rowmask"][:, cj:cj + 1], scalar2=None, op0=ALU.mult), r=["g_t2", "c_rowmask"], w=[vk])
                    yield
                    sc.op("pe", lambda e, rs=rs: e.matmul(pO[:, rs], lhsT=Sb[:], rhs=Bqg[par][:, rs], start=True, stop=False), r=["gSb", f"g_qgT{par}"], w=["gpO"])
                    yield
                    sc.op("pe", lambda e, rs=rs, vn=vn: e.matmul(pO[:, rs], lhsT=vn[:], rhs=Bin[par][:, rs], start=False, stop=True), r=[vk, f"g_intraT{par}"], w=["gpO"])
                    yield
                    sc.op("pe", lambda e, vn=vn: e.matmul(pV[:, 128:256], lhsT=Bkd[par][:], rhs=vn[:], start=True, stop=True), r=[f"g_kdec{par}", vk], w=["gpV"])
                    yield
                    sc.op("dve", lambda e, r0=r0: e.scalar_tensor_tensor(out=Sf[:], in0=Sf[:], scalar=FeG[par][:, r0 + 63:r0 + 64], in1=pV[:, 128:256],
                                                                       op0=ALU.mult, op1=ALU.add), r=["gSf", f"g_eG{par}", "gpV"], w=["gSf"])
                    yield
                    sc.op("act", lambda e: e.activation(out=Sb[:], in_=Sf[:], func=AF.Copy), r=["gSf"], w=["gSb"])
                    yield
                if g.gdn_stop == "C5":
                    return
                sc.op("act", lambda e: e.activation(out=Bt["sq2"][:], in_=pO[:, 0:128], func=AF.Square), r=["gpO"], w=["g_sq2"])
                yield
                sc.op("pe", lambda e: e.matmul(pO[:, 128:256], lhsT=g.ones_b[:], rhs=Bt["sq2"][:], start=True, stop=True), r=["g_sq2", "ones_b"], w=["gpO"])
                yield
                sc.op("dve", lambda e: e.tensor_scalar(out=rn2[:], in0=pO[:, 128:256], scalar1=1.0 / 128, scalar2=EPS, op0=ALU.mult, op1=ALU.add), r=["gpO"], w=["g_rn2"])
                yield
                sc.op("act", lambda e: e.activation(out=rn2[:], in_=rn2[:], func=AF.Ln), r=["g_rn2"], w=["g_rn2"])
                yield
                sc.op("act", lambda e: e.activation(out=rn2[:], in_=rn2[:], func=AF.Exp, scale=-0.5), r=["g_rn2"], w=["g_rn2"])
                yield
                sc.op("dve", lambda e: e.scalar_tensor_tensor(out=t2[:], in0=pO[:, 0:128], scalar=gnw[:, 0:1], in1=rn2[:], op0=ALU.mult, op1=ALU.mult),
                      r=["gpO", "ggnw", "g_rn2"], w=["g_t2"])
                yield
                yb_ = n_st % 2
                sc.op("dve", lambda e, yb_=yb_, i=i, tsl=tsl: e.tensor_tensor(out=ystg[yb_][:, (i % 4) * 128:(i % 4 + 1) * 128], in0=t2[:], in1=zsT[:, tsl], op=ALU.mult),
                      r=["g_t2", "gzsT"], w=[f"gystg{yb_}"])
                yield
                if i % 4 == 3:
                    q0 = (i // 4) * 512
                    sc.dma("sp", lambda e, yb_=yb_, h=h, q0=q0: e.dma_start(out=io["YT"][(8 + h) * 128:(9 + h) * 128, q0:q0 + 512], in_=ystg[yb_][:]),
                           r=[f"gystg{yb_}"], w=["YT"])
                    n_st += 1
            for _ in tpart(0):
                pass
            for i in range(NT):
                ga = tpart(i + 1) if i + 1 < NT else None
                gb = rpart(i)
                while ga is not None or gb is not None:
                    if ga is not None:
                        try:
                            next(ga)
                        except StopIteration:
                            ga = None
                    if gb is not None:
                        try:
                            next(gb)
                        except StopIteration:
                            gb = None


def load_w_bf16(g, dst, src_l, stg, key):
    sc = g.sc
    v = src_l.rearrange("(kc p) f -> p kc f", p=128)
    for hf in range(2):
        sc.dma("sp", lambda e, hf=hf: e.dma_start(out=stg[hf][:], in_=v[:, :, hf * 512:(hf + 1) * 512]), w=[f"mstg{hf}"])
        sc.op("pool", lambda e, hf=hf: e.tensor_copy(out=dst[:, :, hf * 512:(hf + 1) * 512], in_=stg[hf][:]), r=[f"mstg{hf}"], w=[key])


def phase_merge(g, l, xsrc):
    nc, sc, io = g.nc, g.sc, g.io
    S = g.S
    NTC = S // 512
    YTv = io["YT"].rearrange("(c p) t -> p c t", p=128)
    PTv = io["PT"].rearrange("(c p) t -> p c t", p=128)
    with ExitStack() as ls:
        wa = T(ls, nc, "mwa", [128, KC, D], BF16)
        wb = T(ls, nc, "mwb", [128, KC, D], BF16)
        wo = T(ls, nc, "mwo", [128, KC, D], BF16)
        g1bc = T(ls, nc, "mg1", [128, D], F32)
        with ExitStack() as l2:
            stg = [T(l2, nc, f"mstg{i}", [128, KC, 512], F32) for i in range(2)]
            load_w_bf16(g, wa, io["w_a"][l], stg, "mwa")
            load_w_bf16(g, wb, io["w_b"][l], stg, "mwb")
            load_w_bf16(g, wo, io["w_out"][l], stg, "mwo")
            sc.barrier()
        load_bc(g, g1bc, l, 0, "mg1")
        yaT = T(ls, nc, "myaT", [128, KC, 512], BF16)
        ybT = T(ls, nc, "mybT", [128, KC, 512], BF16)
        gaT = T(ls, nc, "mgaT", [128, KC, 512], BF16)
        gbT = T(ls, nc, "mgbT", [128, KC, 512], BF16)
        mixT = T(ls, nc, "mmixT", [128, KC, 512], BF16)
        m1 = [T(ls, nc, f"mm1{i}", [128, 512], F32) for i in range(2)]
        m2 = [T(ls, nc, f"mm2{i}", [128, 512], F32) for i in range(2)]
        xt = [T(ls, nc, f"mxt{i}", [128, D], F32) for i in range(2)]
        xo = [T(ls, nc, f"mxo{i}", [128, D], F32) for i in range(2)]
        pA = [PS(ls, nc, f"mpA{i}", [128, 512], F32) for i in range(2)]
        pBm = [PS(ls, nc, f"mpB{i}", [128, 512], F32) for i in range(2)]
        pO = [PS(ls, nc, f"mpO{i}", [128, 512], F32) for i in range(2)]
        n = 0
        for tc in range(NTC):
            cs = slice(tc * 512, (tc + 1) * 512)
            sc.dma("sp", lambda e, cs=cs: e.dma_start(out=yaT[:], in_=YTv[:, 0:8, cs]), r=["YT"], w=["myaT"])
            sc.dma("sp", lambda e, cs=cs: e.dma_start(out=ybT[:], in_=YTv[:, 8:16, cs]), r=["YT"], w=["mybT"])
            sc.dma("act", lambda e, cs=cs: e.dma_start(out=gaT[:], in_=PTv[:, 48:56, cs]), r=["PT"], w=["mgaT"])
            sc.dma("act", lambda e, cs=cs: e.dma_start(out=gbT[:], in_=PTv[:, 56:64, cs]), r=["PT"], w=["mgbT"])
            for dc in range(KC):
                b = dc % 2
                for kc in range(KC):
                    sc.op("pe", lambda e, b=b, kc=kc, dc=dc: e.matmul(pA[b][:], lhsT=wa[:, kc, dc * 128:(dc + 1) * 128], rhs=yaT[:, kc, :],
                                                                      start=(kc == 0), stop=(kc == KC - 1)), r=["mwa", "myaT"], w=[f"mpA{b}"])
                for kc in range(KC):
                    sc.op("pe", lambda e, b=b, kc=kc, dc=dc: e.matmul(pBm[b][:], lhsT=wb[:, kc, dc * 128:(dc + 1) * 128], rhs=ybT[:, kc, :],
                                                                      start=(kc == 0), stop=(kc == KC - 1)), r=["mwb", "mybT"], w=[f"mpB{b}"])
                sc.op("dve", lambda e, b=b, dc=dc: e.tensor_tensor(out=m1[b][:], in0=pA[b][:], in1=gaT[:, dc, :], op=ALU.mult), r=[f"mpA{b}", "mgaT"], w=[f"mm1{b}"])
                sc.op("dve", lambda e, b=b, dc=dc: e.tensor_tensor(out=m2[b][:], in0=pBm[b][:], in1=gbT[:, dc, :], op=ALU.mult), r=[f"mpB{b}", "mgbT"], w=[f"mm2{b}"])
                sc.op("pool", lambda e, b=b, dc=dc: e.tensor_tensor(out=mixT[:, dc, :], in0=m1[b][:], in1=m2[b][:], op=ALU.add), r=[f"mm1{b}", f"mm2{b}"], w=["mmixT"])
            for tt in range(4):
                ti = tc * 4 + tt
                xb = n % 2
                n += 1
                sc.dma("sp", lambda e, xb=xb, ti=ti: e.dma_start(out=xt[xb][:], in_=xsrc[ti * 128:(ti + 1) * 128, :]), r=[f"xr{ti}"], w=[f"mxt{xb}"])
                for hf in range(2):
                    for kc in range(KC):
                        sc.op("pe", lambda e, hf=hf, kc=kc, tt=tt: e.matmul(pO[hf][:], lhsT=mixT[:, kc, tt * 128:(tt + 1) * 128], rhs=wo[:, kc, hf * 512:(hf + 1) * 512],
                                                                            start=(kc == 0), stop=(kc == KC - 1)), r=["mmixT", "mwo"], w=[f"mpO{hf}"])
                    hs = slice(hf * 512, (hf + 1) * 512)
                    sc.op("dve", lambda e, hf=hf, hs=hs, xb=xb: e.tensor_tensor(out=xo[xb][:, hs], in0=pO[hf][:], in1=g1bc[:, hs], op=ALU.mult), r=[f"mpO{hf}", "mg1"], w=[f"mxo{xb}"])
                    sc.op("pool", lambda e, hs=hs, xb=xb: e.tensor_tensor(out=xo[xb][:, hs], in0=xo[xb][:, hs], in1=xt[xb][:, hs], op=ALU.add), r=[f"mxo{xb}", f"mxt{xb}"], w=[f"mxo{xb}"])
                sc.dma("sp", lambda e, xb=xb, ti=ti: e.dma_start(out=io["xr"][ti * 128:(ti + 1) * 128, :], in_=xo[xb][:]), r=[f"mxo{xb}"], w=[f"xr{ti}"])


def phase_moe(g, l):
    nc, sc, io = g.nc, g.sc, g.io
    S, NB = g.S, g.NB
    NT = S // 128
    cst = g.cst
    SP = mybir.EngineType.SP
    with ExitStack() as ls:
        E1 = T(ls, nc, "oE1", [128, NT, 32], F32)
        E2 = T(ls, nc, "oE2", [128, NT, 32], F32)
        POS = T(ls, nc, "oPOS", [128, NT, 32], F32)
        Wt = T(ls, nc, "oW", [128, NT, 2], F32)
        IDXf = T(ls, nc, "oIDXf", [128, NT, 2], F32)
        IDX = T(ls, nc, "oIDX", [128, NT, 2], I32)
        basebc = T(ls, nc, "obase", [128, 32], F32)
        a2bc = T(ls, nc, "oa2bc", [128, D], F32)
        b2bc = T(ls, nc, "ob2bc", [128, D], F32)
        g2bc = T(ls, nc, "og2bc", [128, D], F32)
        wr = T(ls, nc, "owr", [128, KC, 36], F32)
        rb = T(ls, nc, "orb", [128, 36], F32)
        blk = T(ls, nc, "oblk", [128, NB], F32)
        blki = T(ls, nc, "oblki", [128, NB], I32)
        load_bc(g, a2bc, l, 1, "oa2bc")
        load_bc(g, b2bc, l, 2, "ob2bc")
        load_bc(g, g2bc, l, 3, "og2bc")
        sc.dma("sp", lambda e: e.dma_start(out=wr[:], in_=io["wr"][l].rearrange("(kc p) f -> p kc f", p=128)), w=["owr"])
        sc.dma("sp", lambda e: e.dma_start(out=rb[:], in_=io["rb_bc"][l]), w=["orb"])
        sc.op("pool", lambda e: e.memset(basebc[:], 0.0), w=["obase"])
        with ExitStack() as l1:
            xt = [T(l1, nc, f"ox{i}", [128, D], F32) for i in range(2)]
            xn = [T(l1, nc, f"oxn{i}", [128, D], F32) for i in range(2)]
            h2 = [T(l1, nc, f"oh2{i}", [128, D], F32) for i in range(2)]
            h2T = [T(l1, nc, f"oh2T{i}", [128, KC, 128], F32) for i in range(2)]
            junk = T(l1, nc, "ojunk", [128, D], BF16)
            st = [T(l1, nc, f"ost{i}", [128, 16, 16], F32) for i in range(2)]
            lg = [T(l1, nc, f"olg{i}", [128, 36], F32) for i in range(2)]
            es_ = [T(l1, nc, f"oes{i}", [128, 8], F32) for i in range(2)]
            em_ = [T(l1, nc, f"oem{i}", [128, 8], F32) for i in range(2)]
            oh = [T(l1, nc, f"ooh{i}", [128, 3, 8], F32) for i in range(2)]
            esum = [T(l1, nc, f"oesum{i}", [128, 32], F32) for i in range(2)]
            pst = [PS(l1, nc, f"opst{i}", [128, 512], F32) for i in range(4)]
            plg = [PS(l1, nc, f"oplg{i}", [128, 512], F32) for i in range(2)]
            ppos = [PS(l1, nc, f"oppos{i}", [128, 512], F32) for i in range(2)]
            for i in range(NT):
                b = i % 2
                S_ = lambda j, b=b: st[b][:, j, 0:1]
                kst = f"ost{b}"
                sc.dma("sp", lambda e, b=b, i=i: e.dma_start(out=xt[b][:], in_=io["xr"][i * 128:(i + 1) * 128, :]), r=[f"xr{i}"], w=[f"ox{b}"])
                sc.op("act", lambda e, b=b: e.activation(out=junk[:], in_=xt[b][:], func=AF.Square, accum_out=st[b][:, 0, 0:1]), r=[f"ox{b}"], w=["ojunk", kst])
                sc.op("dve", lambda e, b=b: e.tensor_scalar(out=st[b][:, 1, 0:1], in0=st[b][:, 0, 0:1], scalar1=1.0 / D, scalar2=EPS, op0=ALU.mult, op1=ALU.add), r=[kst], w=[kst])
                sc.op("act", lambda e, b=b: e.activation(out=st[b][:, 2, 0:1], in_=st[b][:, 1, 0:1], func=AF.Sqrt), r=[kst], w=[kst])
                sc.op("dve", lambda e, b=b: e.reciprocal(out=st[b][:, 3, 0:1], in_=st[b][:, 2, 0:1]), r=[kst], w=[kst])
                sc.op("dve", lambda e, b=b: e.tensor_scalar(out=xn[b][:], in0=xt[b][:], scalar1=st[b][:, 3, 0:1], scalar2=None, op0=ALU.mult), r=[f"ox{b}", kst], w=[f"oxn{b}"])
                sc.op("pool", lambda e, b=b: e.tensor_tensor(out=h2[b][:], in0=xn[b][:], in1=a2bc[:], op=ALU.mult), r=[f"oxn{b}", "oa2bc"], w=[f"oh2{b}"])
                sc.op("pool", lambda e, b=b: e.tensor_tensor(out=h2[b][:], in0=h2[b][:], in1=b2bc[:], op=ALU.add), r=[f"oh2{b}", "ob2bc"], w=[f"oh2{b}"])
                sc.dma("sp", lambda e, b=b, i=i: e.dma_start(out=io["h2d"][i * 128:(i + 1) * 128, :], in_=h2[b][:]), r=[f"oh2{b}"], w=[f"h2d{i}"])
                for kc in range(KC):
                    pb = b * 2 + kc // 4
                    sc.op("pe", lambda e, b=b, kc=kc, pb=pb: e.transpose(pst[pb][:, (kc % 4) * 128:(kc % 4 + 1) * 128], xn[b][:, kc * 128:(kc + 1) * 128], cst["ident_f"][:]),
                          r=[f"oxn{b}", "c_ident_f"], w=[f"opst{pb}"])
                for kc in range(KC):
                    pb = b * 2 + kc // 4
                    sc.op("act", lambda e, b=b, kc=kc, pb=pb: e.activation(out=h2T[b][:, kc, :], in_=pst[pb][:, (kc % 4) * 128:(kc % 4 + 1) * 128], func=AF.Identity,
                                                                          bias=g.mod[:, l, 24 + kc:25 + kc], scale=g.A2[:, l, kc:kc + 1]), r=[f"opst{pb}", "mod", "A2"], w=[f"oh2T{b}"])
                for kc in range(KC):
                    sc.op("pe", lambda e, b=b, kc=kc: e.matmul(plg[b][:, 0:36], lhsT=h2T[b][:, kc, :], rhs=wr[:, kc, :], start=(kc == 0), stop=(kc == KC - 1)),
                          r=[f"oh2T{b}", "owr"], w=[f"oplg{b}"])
                L = lg[b]
                kl = f"olg{b}"
                sc.op("dve", lambda e, b=b, L=L: e.tensor_tensor(out=L[:], in0=plg[b][:, 0:36], in1=rb[:], op=ALU.add), r=[f"oplg{b}", "orb"], w=[kl])
                sc.op("dve", lambda e, b=b, L=L: e.tensor_reduce(out=st[b][:, 4, 0:1], in_=L[:, 0:4], axis=AX.X, op=ALU.max), r=[kl], w=[kst])
                sc.op("dve", lambda e, b=b: e.tensor_scalar(out=st[b][:, 5, 0:1], in0=st[b][:, 4, 0:1], scalar1=-1.0, scalar2=None, op0=ALU.mult), r=[kst], w=[kst])
                sc.op("act", lambda e, b=b, L=L: e.activation(out=es_[b][:, 0:4], in_=L[:, 0:4], func=AF.Exp, bias=st[b][:, 5, 0:1], scale=1.0, accum_out=st[b][:, 6, 0:1]),
                      r=[kl, kst], w=[f"oes{b}", kst])
                sc.op("dve", lambda e, b=b: e.reciprocal(out=st[b][:, 7, 0:1], in_=st[b][:, 6, 0:1]), r=[kst], w=[kst])
                sc.op("dve", lambda e, b=b, L=L: e.tensor_scalar(out=oh[b][:, 0, 0:4], in0=L[:, 0:4], scalar1=st[b][:, 4, 0:1], scalar2=None, op0=ALU.is_equal),
                      r=[kl, kst], w=[f"ooh{b}"])
                sc.op("dve", lambda e, b=b, L=L: e.tensor_scalar(out=es_[b][:], in0=L[:, 4:12], scalar1=oh[b][:, 0, 0:1], scalar2=None, op0=ALU.mult), r=[kl, f"ooh{b}"], w=[f"oes{b}"])
                for gq in range(1, 4):
                    sc.op("dve", lambda e, b=b, L=L, gq=gq: e.scalar_tensor_tensor(out=es_[b][:], in0=L[:, 4 + gq * 8:12 + gq * 8], scalar=oh[b][:, 0, gq:gq + 1], in1=es_[b][:],
                                                                                 op0=ALU.mult, op1=ALU.add), r=[kl, f"ooh{b}", f"oes{b}"], w=[f"oes{b}"])
                sc.op("dve", lambda e, b=b: e.tensor_reduce(out=st[b][:, 8, 0:1], in_=es_[b][:], axis=AX.X, op=ALU.max), r=[f"oes{b}"], w=[kst])
                sc.op("dve", lambda e, b=b: e.tensor_scalar(out=oh[b][:, 1, :], in0=es_[b][:], scalar1=st[b][:, 8, 0:1], scalar2=None, op0=ALU.is_equal), r=[f"oes{b}", kst], w=[f"ooh{b}"])
                sc.op("dve", lambda e, b=b: e.scalar_tensor_tensor(out=em_[b][:], in0=oh[b][:, 1, :], scalar=-1e30, in1=es_[b][:], op0=ALU.mult, op1=ALU.add),
                      r=[f"ooh{b}", f"oes{b}"], w=[f"oem{b}"])
                sc.op("dve", lambda e, b=b: e.tensor_reduce(out=st[b][:, 9, 0:1], in_=em_[b][:], axis=AX.X, op=ALU.max), r=[f"oem{b}"], w=[kst])
                sc.op("dve", lambda e, b=b: e.tensor_scalar(out=oh[b][:, 2, :], in0=em_[b][:], scalar1=st[b][:, 9, 0:1], scalar2=None, op0=ALU.is_equal), r=[f"oem{b}", kst], w=[f"ooh{b}"])
                sc.op("dve", lambda e, b=b: e.tensor_scalar(out=st[b][:, 10, 0:1], in0=st[b][:, 8, 0:1], scalar1=-1.0, scalar2=None, op0=ALU.mult), r=[kst], w=[kst])
                sc.op("act", lambda e, b=b: e.activation(out=st[b][:, 11, 0:1], in_=st[b][:, 9, 0:1], func=AF.Exp, bias=st[b][:, 10, 0:1], scale=1.0), r=[kst], w=[kst])
                sc.op("dve", lambda e, b=b: e.tensor_scalar(out=st[b][:, 12, 0:1], in0=st[b][:, 11, 0:1], scalar1=1.0, scalar2=None, op0=ALU.add), r=[kst], w=[kst])
                sc.op("dve", lambda e, b=b: e.reciprocal(out=st[b][:, 13, 0:1], in_=st[b][:, 12, 0:1]), r=[kst], w=[kst])
                sc.op("dve", lambda e, b=b, i=i: e.tensor_tensor(out=Wt[:, i, 0:1], in0=st[b][:, 13, 0:1], in1=st[b][:, 7, 0:1], op=ALU.mult), r=[kst], w=[f"oW{i}"])
                sc.op("dve", lambda e, b=b, i=i: e.tensor_tensor(out=Wt[:, i, 1:2], in0=Wt[:, i, 0:1], in1=st[b][:, 11, 0:1], op=ALU.mult), r=[kst, f"oW{i}"], w=[f"oW{i}"])
                for gq in range(4):
                    sc.op("dve", lambda e, b=b, i=i, gq=gq: e.tensor_scalar(out=E1[:, i, gq * 8:(gq + 1) * 8], in0=oh[b][:, 1, :], scalar1=oh[b][:, 0, gq:gq + 1], scalar2=None, op0=ALU.mult),
                          r=[f"ooh{b}"], w=[f"oE{i}"])
                    sc.op("dve", lambda e, b=b, i=i, gq=gq: e.tensor_scalar(out=E2[:, i, gq * 8:(gq + 1) * 8], in0=oh[b][:, 2, :], scalar1=oh[b][:, 0, gq:gq + 1], scalar2=None, op0=ALU.mult),
                          r=[f"ooh{b}"], w=[f"oE{i}"])
                sc.op("dve", lambda e, b=b, i=i: e.tensor_tensor(out=esum[b][:], in0=E1[:, i, :], in1=E2[:, i, :], op=ALU.add), r=[f"oE{i}"], w=[f"oesum{b}"])
                sc.op("pe", lambda e, b=b: e.matmul(ppos[b][:, 0:32], lhsT=cst["sltU"][:], rhs=esum[b][:], start=True, stop=True), r=[f"oesum{b}", "c_sltU"], w=[f"oppos{b}"])
                sc.op("pe", lambda e, b=b: e.matmul(ppos[b][:, 32:64], lhsT=g.ones_f[:], rhs=esum[b][:], start=True, stop=True), r=[f"oesum{b}", "ones_f"], w=[f"oppos{b}"])
                sc.op("dve", lambda e, b=b, i=i: e.tensor_tensor(out=POS[:, i, :], in0=ppos[b][:, 0:32], in1=basebc[:], op=ALU.add), r=[f"oppos{b}", "obase"], w=[f"oPOS{i}"])
                sc.op("dve", lambda e, b=b: e.tensor_tensor(out=basebc[:], in0=ppos[b][:, 32:64], in1=basebc[:], op=ALU.add), r=[f"oppos{b}", "obase"], w=["obase"])
            sc.barrier()
        if g.moe_stop == "1":
            return
        with ExitStack() as l2:
            v = T(l2, nc, "ov", [128, 8, 32], F32)
            vi = T(l2, nc, "ovi", [128, 32], I32)
            ones32 = T(l2, nc, "oones32", [128, 32], F32)
            tmp32 = T(l2, nc, "otmp32", [128, 32], F32)
            acc = T(l2, nc, "oacc", [128, NB], F32)
            cmp_ = T(l2, nc, "ocmp", [128, NB], F32)
            iot = T(l2, nc, "oiot", [128, NB], F32)
            h2r = [T(l2, nc, f"oh2r{i}", [128, D], F32) for i in range(2)]
            sc.dma("sp", lambda e: e.dma_start(out=iot[:], in_=io["iota_nb"]), w=["oiot"])
            sc.op("pool", lambda e: e.memset(ones32[:], 1.0), w=["oones32"])
            sc.op("dve", lambda e: e.tensor_scalar(out=v[:, 0, :], in0=basebc[:], scalar1=127.0, scalar2=1.0 / 128, op0=ALU.add, op1=ALU.mult), r=["obase"], w=["ov"])
            sc.op("dve", lambda e: e.tensor_copy(out=vi[:], in_=v[:, 0, :]), r=["ov"], w=["ovi"])
            sc.op("dve", lambda e: e.tensor_copy(out=v[:, 2, :], in_=vi[:]), r=["ovi"], w=["ov"])
            sc.op("dve", lambda e: e.tensor_tensor(out=v[:, 3, :], in0=v[:, 2, :], in1=v[:, 0, :], op=ALU.is_gt), r=["ov"], w=["ov"])
            sc.op("dve", lambda e: e.tensor_tensor(out=v[:, 2, :], in0=v[:, 2, :], in1=v[:, 3, :], op=ALU.subtract), r=["ov"], w=["ov"])
            sc.op("dve", lambda e: e.tensor_tensor(out=v[:, 3, :], in0=v[:, 0, :], in1=v[:, 2, :], op=ALU.subtract), r=["ov"], w=["ov"])
            sc.op("dve", lambda e: e.tensor_scalar(out=v[:, 3, :], in0=v[:, 3, :], scalar1=1.0, scalar2=None, op0=ALU.is_ge), r=["ov"], w=["ov"])
            sc.op("dve", lambda e: e.tensor_tensor(out=v[:, 2, :], in0=v[:, 2, :], in1=v[:, 3, :], op=ALU.add), r=["ov"], w=["ov"])
            sc.op("dve", lambda e: e.tensor_tensor_scan(out=v[:, 4, :], data0=ones32[:], data1=v[:, 2, :], initial=0.0, op0=ALU.mult, op1=ALU.add), r=["ov", "oones32"], w=["ov"])
            sc.op("dve", lambda e: e.tensor_tensor(out=v[:, 5, :], in0=v[:, 4, :], in1=v[:, 2, :], op=ALU.subtract), r=["ov"], w=["ov"])
            sc.op("dve", lambda e: e.tensor_scalar(out=v[:, 5, :], in0=v[:, 5, :], scalar1=128.0, scalar2=None, op0=ALU.mult), r=["ov"], w=["ov"])
            sc.op("pool", lambda e: e.memset(acc[:], 0.0), w=["oacc"])
            for e_ in range(32):
                sc.op("dve", lambda e, e_=e_: e.tensor_scalar(out=cmp_[:], in0=iot[:], scalar1=v[:, 4, e_:e_ + 1], scalar2=None, op0=ALU.is_ge), r=["oiot", "ov"], w=["ocmp"])
                sc.op("dve", lambda e: e.tensor_tensor(out=acc[:], in0=acc[:], in1=cmp_[:], op=ALU.add), r=["ocmp", "oacc"], w=["oacc"])
            sc.op("dve", lambda e: e.tensor_scalar(out=blk[:], in0=acc[:], scalar1=31.0, scalar2=None, op0=ALU.min), r=["oacc"], w=["oblk"])
            sc.op("dve", lambda e: e.tensor_copy(out=blki[:], in_=blk[:]), r=["oblk"], w=["oblki"])
            zt = T(l2, nc, "ozt", [128, D], F32)
            sc.op("pool", lambda e: e.memset(zt[:], 0.0), w=["ozt"])
            for bk in range(NB):
                sc.dma("act" if bk % 2 else "sp", lambda e, bk=bk: e.dma_start(out=io["xs"][bk * 128:(bk + 1) * 128, :], in_=zt[:]), r=["ozt"], w=["xs"])
            for i in range(NT):
                b = i % 2
                for k, Ek in enumerate((E1, E2)):
                    sc.op("dve", lambda e, i=i: e.tensor_tensor(out=tmp32[:], in0=POS[:, i, :], in1=v[:, 5, :], op=ALU.add), r=[f"oPOS{i}", "ov"], w=["otmp32"])
                    sc.op("dve", lambda e, i=i, Ek=Ek: e.tensor_tensor(out=tmp32[:], in0=tmp32[:], in1=Ek[:, i, :], op=ALU.mult), r=["otmp32", f"oE{i}"], w=["otmp32"])
                    sc.op("dve", lambda e, i=i, k=k: e.tensor_reduce(out=IDXf[:, i, k:k + 1], in_=tmp32[:], axis=AX.X, op=ALU.add), r=["otmp32"], w=[f"oIDX{i}"])
                sc.op("dve", lambda e, i=i: e.tensor_copy(out=IDX[:, i, :], in_=IDXf[:, i, :]), r=[f"oIDX{i}"], w=[f"oIDX{i}"])
                sc.dma("sp", lambda e, b=b, i=i: e.dma_start(out=h2r[b][:], in_=io["h2d"][i * 128:(i + 1) * 128, :]), r=[f"h2d{i}"], w=[f"oh2r{b}"])
                for k in range(2):
                    sc.swdma(lambda e, b=b, i=i, k=k: e.indirect_dma_start(
                        out=io["xs"], out_offset=bass.IndirectOffsetOnAxis(ap=IDX[:, i, k:k + 1], axis=0), in_=h2r[b][:], in_offset=None),
                        r=[f"oh2r{b}", f"oIDX{i}"], w=["xs"])
            sc.barrier()
        if g.moe_stop == "2":
            return
        with ExitStack() as l3:
            w1 = [T(l3, nc, f"ow1{i}", [128, KC, DE], F32) for i in range(2)]
            w3 = [T(l3, nc, f"ow3{i}", [128, KC, DE], F32) for i in range(2)]
            w2 = [T(l3, nc, f"ow2{i}", [128, 4, D], F32) for i in range(2)]
            xb_ = [T(l3, nc, f"oxb{i}", [128, D], F32) for i in range(2)]
            xT = [T(l3, nc, f"oxT{i}", [128, KC, 128], F32) for i in range(2)]
            sl = T(l3, nc, "osl", [128, 128], F32)
            gT = T(l3, nc, "ogT", [128, 4, 128], F32)
            yb_ = [T(l3, nc, f"oyb{i}", [128, D], F32) for i in range(2)]
            ptr = [PS(l3, nc, f"optr{i}", [128, 512], F32) for i in range(2)]
            ph = [PS(l3, nc, f"oph{i}", [128, 512], F32) for i in range(2)]
            py = [PS(l3, nc, f"opy{i}", [128, 512], F32) for i in range(2)]
            widf = T(l3, nc, "owidf", [128, 16], F32)
            widx = [T(l3, nc, f"owidx{i}", [128, 16], I32) for i in range(2)]
            iop = T(l3, nc, "oiop", [128, 1], F32)
            sc.dma("sp", lambda e: e.dma_start(out=iop[:], in_=io["iota_p"]), w=["oiop"])
            for bk in range(NB):
                b = bk % 2
                sc.op("dve", lambda e, bk=bk: e.scalar_tensor_tensor(out=widf[:, 0:1], in0=blk[:, bk:bk + 1], scalar=128.0, in1=iop[:], op0=ALU.mult, op1=ALU.add),
                      r=["oblk", "oiop"], w=["owidf"])
                sc.op("dve", lambda e: e.tensor_scalar(out=widf[:, 0:1], in0=widf[:, 0:1], scalar1=float(l * NE * 128), scalar2=None, op0=ALU.add), r=["owidf"], w=["owidf"])
                sc.op("dve", lambda e, b=b: e.tensor_copy(out=widx[b][:, 0:1], in_=widf[:, 0:1]), r=["owidf"], w=[f"owidx{b}"])
                sc.swdma(lambda e, b=b: e.indirect_dma_start(out=w1[b][:].rearrange("p k f -> p (k f)"), out_offset=None, in_=io["ew1"],
                                                             in_offset=bass.IndirectOffsetOnAxis(ap=widx[b][:, 0:1], axis=0)), r=[f"owidx{b}"], w=[f"ow1{b}"])
                sc.swdma(lambda e, b=b: e.indirect_dma_start(out=w3[b][:].rearrange("p k f -> p (k f)"), out_offset=None, in_=io["ew3"],
                                                             in_offset=bass.IndirectOffsetOnAxis(ap=widx[b][:, 0:1], axis=0)), r=[f"owidx{b}"], w=[f"ow3{b}"])
                sc.swdma(lambda e, b=b: e.indirect_dma_start(out=w2[b][:].rearrange("p k f -> p (k f)"), out_offset=None, in_=io["ew2"],
                                                             in_offset=bass.IndirectOffsetOnAxis(ap=widx[b][:, 0:1], axis=0)), r=[f"owidx{b}"], w=[f"ow2{b}"])
                sc.dma("act", lambda e, b=b, bk=bk: e.dma_start(out=xb_[b][:], in_=io["xs"][bk * 128:(bk + 1) * 128, :]), r=["xs"], w=[f"oxb{b}"])
                for kc in range(KC):
                    pb = kc // 4
                    sc.op("pe", lambda e, b=b, kc=kc, pb=pb: e.transpose(ptr[pb][:, (kc % 4) * 128:(kc % 4 + 1) * 128], xb_[b][:, kc * 128:(kc + 1) * 128], cst["ident_f"][:]),
                          r=[f"oxb{b}", "c_ident_f"], w=[f"optr{pb}"])
                for kc in range(KC):
                    pb = kc // 4
                    if pb == 0:
                        sc.op("act", lambda e, b=b, kc=kc, pb=pb: e.activation(out=xT[b][:, kc, :], in_=ptr[pb][:, (kc % 4) * 128:(kc % 4 + 1) * 128], func=AF.Copy),
                              r=[f"optr{pb}"], w=[f"oxT{b}"])
                    else:
                        sc.op("dve", lambda e, b=b, kc=kc, pb=pb: e.tensor_copy(out=xT[b][:, kc, :], in_=ptr[pb][:, (kc % 4) * 128:(kc % 4 + 1) * 128]),
                              r=[f"optr{pb}"], w=[f"oxT{b}"])
                for fc in range(4):
                    pb = fc % 2
                    for kc in range(KC):
                        sc.op("pe", lambda e, b=b, fc=fc, kc=kc, pb=pb: e.matmul(ph[pb][:, 0:128], lhsT=w1[b][:, kc, fc * 128:(fc + 1) * 128], rhs=xT[b][:, kc, :],
                                                                              start=(kc == 0), stop=(kc == KC - 1)), r=[f"ow1{b}", f"oxT{b}"], w=[f"oph{pb}"])
                    for kc in range(KC):
                        sc.op("pe", lambda e, b=b, fc=fc, kc=kc, pb=pb: e.matmul(py[pb][:, 0:128], lhsT=w3[b][:, kc, fc * 128:(fc + 1) * 128], rhs=xT[b][:, kc, :],
                                                                              start=(kc == 0), stop=(kc == KC - 1)), r=[f"ow3{b}", f"oxT{b}"], w=[f"opy{pb}"])
                    sc.op("act", lambda e, pb=pb: e.activation(out=sl[:], in_=ph[pb][:, 0:128], func=AF.Silu), r=[f"oph{pb}"], w=["osl"])
                    sc.op("dve", lambda e, pb=pb, fc=fc: e.tensor_tensor(out=gT[:, fc, :], in0=py[pb][:, 0:128], in1=sl[:], op=ALU.mult), r=[f"opy{pb}", "osl"], w=["ogT"])
                for hf in range(2):
                    for fc in range(4):
                        sc.op("pe", lambda e, b=b, hf=hf, fc=fc: e.matmul(ptr[hf][:], lhsT=gT[:, fc, :], rhs=w2[b][:, fc, hf * 512:(hf + 1) * 512],
                                                                          start=(fc == 0), stop=(fc == 3)), r=["ogT", f"ow2{b}"], w=[f"optr{hf}"])
                    if hf == 0:
                        sc.op("act", lambda e, b=b: e.activation(out=yb_[b][:, 0:512], in_=ptr[0][:], func=AF.Copy), r=["optr0"], w=[f"oyb{b}"])
                    else:
                        sc.op("dve", lambda e, b=b: e.tensor_copy(out=yb_[b][:, 512:1024], in_=ptr[1][:]), r=["optr1"], w=[f"oyb{b}"])
                sc.dma("act", lambda e, b=b, bk=bk: e.dma_start(out=io["ys"][bk * 128:(bk + 1) * 128, :], in_=yb_[b][:]), r=[f"oyb{b}"], w=["ys"])
            sc.barrier()
        if g.moe_stop == "3":
            return
        with ExitStack() as l4:
            y0 = [T(l4, nc, f"oy0{i}", [128, D], F32) for i in range(2)]
            y1 = [T(l4, nc, f"oy1{i}", [128, D], F32) for i in range(2)]
            xt = [T(l4, nc, f"oxc{i}", [128, D], F32) for i in range(2)]
            for i in range(NT):
                b = i % 2
                sc.swdma(lambda e, b=b, i=i: e.indirect_dma_start(out=y0[b][:], out_offset=None, in_=io["ys"],
                                                                  in_offset=bass.IndirectOffsetOnAxis(ap=IDX[:, i, 0:1], axis=0)), r=["ys", f"oIDX{i}"], w=[f"oy0{b}"])
                sc.swdma(lambda e, b=b, i=i: e.indirect_dma_start(out=y1[b][:], out_offset=None, in_=io["ys"],
                                                                  in_offset=bass.IndirectOffsetOnAxis(ap=IDX[:, i, 1:2], axis=0)), r=["ys", f"oIDX{i}"], w=[f"oy1{b}"])
                if g.moe_stop == "4a":
                    continue
                sc.dma("sp", lambda e, b=b, i=i: e.dma_start(out=xt[b][:], in_=io["xr"][i * 128:(i + 1) * 128, :]), r=[f"xr{i}"], w=[f"oxc{b}"])
                sc.op("dve", lambda e, b=b, i=i: e.tensor_scalar(out=y0[b][:], in0=y0[b][:], scalar1=Wt[:, i, 0:1], scalar2=None, op0=ALU.mult), r=[f"oy0{b}", f"oW{i}"], w=[f"oy0{b}"])
                sc.op("dve", lambda e, b=b, i=i: e.scalar_tensor_tensor(out=y0[b][:], in0=y1[b][:], scalar=Wt[:, i, 1:2], in1=y0[b][:], op0=ALU.mult, op1=ALU.add),
                      r=[f"oy0{b}", f"oy1{b}", f"oW{i}"], w=[f"oy0{b}"])
                sc.op("pool", lambda e, b=b: e.tensor_tensor(out=y0[b][:], in0=y0[b][:], in1=g2bc[:], op=ALU.mult), r=[f"oy0{b}", "og2bc"], w=[f"oy0{b}"])
                sc.op("pool", lambda e, b=b: e.tensor_tensor(out=xt[b][:], in0=xt[b][:], in1=y0[b][:], op=ALU.add), r=[f"oy0{b}", f"oxc{b}"], w=[f"oxc{b}"])
                sc.dma("sp", lambda e, b=b, i=i: e.dma_start(out=io["xr"][i * 128:(i + 1) * 128, :], in_=xt[b][:]), r=[f"oxc{b}"], w=[f"xr{i}"])


def phase_final(g, xsrc):
    nc, sc, io = g.nc, g.sc, g.io
    NT = g.S // 128
    with ExitStack() as ls:
        fw = T(ls, nc, "fw", [128, D], F32)
        xt = [T(ls, nc, f"fx{i}", [128, D], F32) for i in range(2)]
        yt = [T(ls, nc, f"fy{i}", [128, D], F32) for i in range(2)]
        junk = T(ls, nc, "fjunk", [128, D], BF16)
        st = [T(ls, nc, f"fst{i}", [128, 64], F32) for i in range(2)]
        sc.dma("sp", lambda e: e.dma_start(out=fw[:], in_=io["fnw_bc"]), w=["fw"])
        for i in range(NT):
            b = i % 2
            sc.dma("sp", lambda e, b=b, i=i: e.dma_start(out=xt[b][:], in_=xsrc[i * 128:(i + 1) * 128, :]), w=[f"fx{b}"])
            sc.op("act", lambda e, b=b: e.activation(out=junk[:], in_=xt[b][:], func=AF.Square, accum_out=st[b][:, 0:1]),
                  r=[f"fx{b}"], w=["fjunk", f"fst{b}"])
            sc.op("dve", lambda e, b=b: e.tensor_scalar(out=st[b][:, 16:17], in0=st[b][:, 0:1], scalar1=1.0 / D, scalar2=EPS,
                                                         op0=ALU.mult, op1=ALU.add), r=[f"fst{b}"], w=[f"fst{b}"])
            sc.op("act", lambda e, b=b: e.activation(out=st[b][:, 32:33], in_=st[b][:, 16:17], func=AF.Sqrt), r=[f"fst{b}"], w=[f"fst{b}"])
            sc.op("dve", lambda e, b=b: e.reciprocal(out=st[b][:, 48:49], in_=st[b][:, 32:33]), r=[f"fst{b}"], w=[f"fst{b}"])
            sc.op("dve", lambda e, b=b: e.scalar_tensor_tensor(out=yt[b][:], in0=xt[b][:], scalar=st[b][:, 48:49], in1=fw[:],
                                                                op0=ALU.mult, op1=ALU.mult), r=[f"fx{b}", f"fst{b}", "fw"], w=[f"fy{b}"])
            sc.dma("sp", lambda e, b=b, i=i: e.dma_start(out=io["y"][i * 128:(i + 1) * 128, :], in_=yt[b][:]), r=[f"fy{b}"], w=["y"])


def build(S, dbg=(), stop_after=None, layers=DEPTH):
    NB = (2 * S) // 128 + NE
    nc = bass.Bass("TRN2", target_bir_lowering=False)
    g = Ctx()
    g.nc, g.S, g.NB = nc, S, NB
    g.dbgset = set(dbg)
    import os as _os
    g.gdn_stop = _os.environ.get("GDN_STOP")
    g.moe_stop = _os.environ.get("MOE_STOP")
    dbg = [d for d in dbg if d in ("PT", "Vtok", "BD", "YT", "xr", "xs", "ys")]
    g.io = declare_io(nc, S, NB)
    io = g.io
    dbg_out = {}
    for name in dbg:
        src = io[name]
        dbg_out[name] = nc.dram_tensor("dbg_" + name, list(src.shape), src.dtype, kind="ExternalOutput").ap()
    with ExitStack() as es:
        g.es = es
        g.sc = Sched(nc, es)
        sc = g.sc
        phase_setup(g)
        done = False
        for l in range(layers):
            xsrc = io["x"] if l == 0 else io["xr"]
            with ExitStack() as ls:
                hT = T(ls, nc, "hT", [128, KC, S], BF16)
                phase_norm(g, l, xsrc, hT, g.A1[:, l, :], g.mod[:, l, 0:8])
                sc.barrier()
                dbg_sb(g, "hT", hT[:], [f"hT{i}" for i in range(S // 128)])
                phase_proj(g, l, hT)
                sc.barrier()
            if stop_after == "proj":
                break
            phase_attn(g, l)
            sc.barrier()
            if stop_after == "attn":
                break
            phase_gdn(g, l)
            sc.barrier()
            if stop_after == "gdn":
                break
            phase_merge(g, l, xsrc)
            sc.barrier()
            if stop_after == "merge":
                break
            phase_moe(g, l)
            sc.barrier()
            if stop_after == "moe":
                break
        sc.barrier()
        phase_final(g, io["xr"] if stop_after is None else io["x"])
        for name in dbg:
            src = io[name]
            dst = dbg_out[name]
            sc.dma("sp", lambda e, src=src, dst=dst: e.dma_start(out=dst, in_=src), r=[name], w=["dbg_" + name])
        sc.final_wait()
        print("instructions:", sc.ninst, "sems:", len(sc.semobj))
    return nc


def host_shared(inp):
    f = lambda a: np.ascontiguousarray(np.asarray(a, dtype=np.float32))
    sh = {}
    colT = lambda v: f(np.asarray(v).reshape(-1, 128).T)
    sh["ada_w"] = f(inp["ada_w"])
    sh["ada_b_t"] = f(np.stack([colT(inp["ada_b"][l]) for l in range(DEPTH)]))
    sh["n1w_t"] = f(np.stack([colT(inp["norm1_w"][l]) for l in range(DEPTH)]))
    sh["n2w_t"] = f(np.stack([colT(inp["norm2_w"][l]) for l in range(DEPTH)]))
    sh["fnw_bc"] = f(np.broadcast_to(np.asarray(inp["final_norm_w"])[None, :], (128, D)))
    sh["w_in"] = f(inp["w_in"])
    cw = np.asarray(inp["conv_w"])
    sh["conv_t"] = f(cw.reshape(DEPTH, 4, 24, 128).transpose(0, 3, 2, 1))
    lam = np.stack([np.asarray(inp[k]) for k in ("lambda_q1", "lambda_k1", "lambda_q2", "lambda_k2")], axis=1)
    sh["lamv"] = f(np.broadcast_to(lam[:, None], (DEPTH, 128, 4, 64)))
    sh["subln_bc"] = f(np.broadcast_to(np.asarray(inp["subln_w"])[:, None, :], (DEPTH, 128, 128)))
    sh["alog_bc"] = f(np.broadcast_to(np.asarray(inp["a_log"])[:, None, :], (DEPTH, 128, NH)))
    sh["dtb_bc"] = f(np.broadcast_to(np.asarray(inp["dt_bias"])[:, None, :], (DEPTH, 128, NH)))
    sh["gnw_t"] = f(np.asarray(inp["gdn_norm_w"])[:, :, None])
    sh["w_a"] = f(inp["w_branch_a"])
    sh["w_b"] = f(inp["w_branch_b"])
    sh["w_out"] = f(inp["w_out"])
    sh["wr"] = f(np.concatenate([np.asarray(inp["router_group_w"]), np.asarray(inp["router_expert_w"])], axis=2))
    rb = np.concatenate([np.asarray(inp["router_group_b"]), np.asarray(inp["router_expert_b"])], axis=1)
    sh["rb_bc"] = f(np.broadcast_to(rb[:, None, :], (DEPTH, 128, 36)))
    sh["ew1"] = f(np.asarray(inp["expert_w1"]).reshape(DEPTH, NE, KC, 128, DE).transpose(0, 1, 3, 2, 4).reshape(DEPTH * NE * 128, KC * DE))
    sh["ew3"] = f(np.asarray(inp["expert_w3"]).reshape(DEPTH, NE, KC, 128, DE).transpose(0, 1, 3, 2, 4).reshape(DEPTH * NE * 128, KC * DE))
    sh["ew2"] = f(np.asarray(inp["expert_w2"]).reshape(DEPTH, NE, 4, 128, D).transpose(0, 1, 3, 2, 4).reshape(DEPTH * NE * 128, 4 * D))
    sh.update(_consts())
    return sh


def host_core(inp, b):
    S_ = int(np.asarray(inp["x"]).shape[1])
    NB_ = (2 * S_) // 128 + NE
    return {"iota_nb": np.ascontiguousarray(np.broadcast_to(np.arange(NB_, dtype=np.float32)[None, :], (128, NB_))),
            "iota_p": np.arange(128, dtype=np.float32)[:, None].copy(),
            "x": np.ascontiguousarray(np.asarray(inp["x"][b], dtype=np.float32)),
            "c_t": np.ascontiguousarray(np.asarray(inp["c"][b], dtype=np.float32).reshape(KC, 128).T)}


def kernel(**inputs):
    S = int(np.asarray(inputs["x"]).shape[1])
    B = int(np.asarray(inputs["x"]).shape[0])
    sh = host_shared(inputs)
    nc = build(S)
    in_maps = [{**sh, **host_core(inputs, b)} for b in range(B)]
    res = run_bass_kernel_spmd(nc, in_maps, core_ids=list(range(B)))
    return np.stack([np.asarray(r["y"], dtype=np.float32) for r in res.results], axis=0)
```

```python
import math
from contextlib import ExitStack
import numpy as np
import ml_dtypes
import concourse.bass as bass
import concourse.mybir as mybir
from concourse.bass_utils import run_bass_kernel_spmd

F32 = mybir.dt.float32
BF16 = mybir.dt.bfloat16
I32 = mybir.dt.int32
U32 = mybir.dt.uint32
AF = mybir.ActivationFunctionType
ALU = mybir.AluOpType
AX = mybir.AxisListType

D = 1024
KC = 8
DEPTH = 2
NH = 8
NE = 32
DE = 512
PTOT = 9232
EPS = 1e-6
SEM_ROT = 1 << 30
NDSEM = 24
NSWSEM = 64
import os as _os0
SIMSAFE = bool(_os0.environ.get("BASS_SIMSAFE"))


class Sched:
    def __init__(self, nc, es):
        self.nc = nc
        self.es = es
        self.engs = {"pe": nc.tensor, "dve": nc.vector, "act": nc.scalar, "pool": nc.gpsimd, "sp": nc.sync}
        self.semobj = []
        self.cur = {}
        self.cnt = {}
        self.waited = {e: {} for e in self.engs}
        self.lastw = {}
        self.readers = {}
        self.nsem = 0
        for e in self.engs:
            self._newsem(e)
        self.dsem = []
        self.dcnt = []
        self.qsems = {}
        for q, nq in (("sp", 20), ("act", 10), ("pool", 2)):
            self.qsems[q] = []
            for i in range(nq):
                s = es.enter_context(nc.semaphore(f"dma_{q}{i}"))
                self.semobj.append(s)
                self.dsem.append(len(self.semobj) - 1)
                self.dcnt.append(0)
                self.qsems[q].append(len(self.dsem) - 1)
        self.qnext = {q: 0 for q in self.qsems}
        self.dnext = 0
        self.ninst = 0
        self.swsems = []
        for i in range(NSWSEM):
            so = es.enter_context(nc.semaphore(f"swd{i}"))
            self.semobj.append(so)
            self.swsems.append(len(self.semobj) - 1)
        self.sw_used = 0
        self.sw_next = 0
        self.swcnt = [0] * NSWSEM
        self.prog = {e: [] for e in self.engs}

    def _newsem(self, e):
        s = self.es.enter_context(self.nc.semaphore(f"s_{e}_{self.nsem}"))
        self.nsem += 1
        self.semobj.append(s)
        self.cur[e] = len(self.semobj) - 1
        self.cnt[e] = 0

    def _deps(self, e, r, w):
        deps = {}

        def add(ev):
            if ev is None:
                return
            s, v = ev
            if deps.get(s, 0) < v:
                deps[s] = v

        for k in r:
            add(self.lastw.get(k))
        for k in w:
            add(self.lastw.get(k))
            for s, v in self.readers.get(k, {}).items():
                add((s, v))
        eng = self.engs[e]
        wt = self.waited[e]
        for s, v in deps.items():
            if e == "pe" and s == self.cur["pe"]:
                continue
            if wt.get(s, 0) >= v:
                continue
            eng.wait_ge(self.semobj[s], v)
            wt[s] = v

    def _record(self, ev, r, w):
        for k in r:
            d = self.readers.setdefault(k, {})
            if d.get(ev[0], 0) < ev[1]:
                d[ev[0]] = ev[1]
        for k in w:
            self.lastw[k] = ev
            self.readers[k] = {}

    def op(self, e, fn, r=(), w=()):
        pr = [k for k in r if k in PSUM_NAMES]
        if pr:
            r = [k for k in r if k not in PSUM_NAMES]
            w = list(w) + pr
        self._deps(e, r, w)
        if self.cnt[e] >= SEM_ROT:
            self._newsem(e)
        self.cnt[e] += 1
        fn(self.engs[e]).then_inc(self.semobj[self.cur[e]], 1)
        ev = (self.cur[e], self.cnt[e])
        self._record(ev, r, w)
        self.ninst += 1
        return ev

    def dma(self, q, fn, r=(), w=()):
        self._deps(q, r, w)
        i = self.qsems[q][self.qnext[q]]
        self.qnext[q] = (self.qnext[q] + 1) % len(self.qsems[q])
        if self.dcnt[i] > 0 and self.waited[q].get(self.dsem[i], 0) < self.dcnt[i]:
            self.engs[q].wait_ge(self.semobj[self.dsem[i]], self.dcnt[i])
            self.waited[q][self.dsem[i]] = self.dcnt[i]
        self.dcnt[i] += 16
        fn(self.engs[q]).then_inc(self.semobj[self.dsem[i]], 16)
        ev = (self.dsem[i], self.dcnt[i])
        self._record(ev, r, w)
        self.ninst += 1
        return ev

    def sync_only(self, e, r=(), w=()):
        self._deps(e, r, w)

    def swdma(self, fn, r=(), w=()):
        if SIMSAFE:
            if self.sw_used == len(self.swsems):
                self.sw_recycle()
            si = self.swsems[self.sw_used]
            self.sw_used += 1
            self._deps("pool", r, w)
            fn(self.engs["pool"]).then_inc(self.semobj[si], 16)
            ev = (si, 16)
        else:
            k = self.sw_next
            self.sw_next = (self.sw_next + 1) % 16
            si = self.swsems[k]
            self._deps("pool", r, w)
            if self.swcnt[k] > 0 and self.waited["pool"].get(si, 0) < self.swcnt[k]:
                self.engs["pool"].wait_ge(self.semobj[si], self.swcnt[k])
                self.waited["pool"][si] = self.swcnt[k]
            self.swcnt[k] += 16
            fn(self.engs["pool"]).then_inc(self.semobj[si], 16)
            ev = (si, self.swcnt[k])
        self._record(ev, r, w)
        self.ninst += 1
        return ev

    def sw_recycle(self):
        self.barrier()
        self.nc.all_engine_barrier()
        for si in self.swsems[:self.sw_used]:
            self.engs["pool"].sem_clear(self.semobj[si])
        self.nc.all_engine_barrier()
        for e in self.engs:
            for si in self.swsems:
                self.waited[e].pop(si, None)
        self.sw_used = 0

    def barrier(self):
        evs = [(self.cur[e], self.cnt[e]) for e in self.engs if self.cnt[e] > 0]
        evs += [(self.dsem[i], self.dcnt[i]) for i in range(len(self.dsem)) if self.dcnt[i] > 0]
        if SIMSAFE:
            evs += [(si, 16) for si in self.swsems[:self.sw_used]]
        else:
            evs += [(self.swsems[k], self.swcnt[k]) for k in range(16) if self.swcnt[k] > 0]
        for e in self.engs:
            wt = self.waited[e]
            for s, v in evs:
                if e == "pe" and s == self.cur["pe"]:
                    continue
                if wt.get(s, 0) >= v:
                    continue
                self.engs[e].wait_ge(self.semobj[s], v)
                wt[s] = v
        self.lastw = {}
        self.readers = {}

    def final_wait(self):
        self.barrier()

    def emit(self):
        nc = self.nc
        prog = self.prog
        with nc.Block() as block:
            @block.tensor
            def _(en):
                for f in prog["pe"]:
                    f(en)

            @block.vector
            def _(en):
                for f in prog["dve"]:
                    f(en)

            @block.scalar
            def _(en):
                for f in prog["act"]:
                    f(en)

            @block.gpsimd
            def _(en):
                for f in prog["pool"]:
                    f(en)

            @block.sync
            def _(en):
                for f in prog["sp"]:
                    f(en)


def _consts():
    c = {}
    c["ident_f"] = np.eye(128, dtype=np.float32)
    c["ident_b"] = np.eye(128, dtype=np.float32).astype(ml_dtypes.bfloat16)
    k = np.arange(128)[:, None]
    q = np.arange(128)[None, :]
    c["triu_b"] = (q >= k).astype(np.float32).astype(ml_dtypes.bfloat16)
    slopes = 2.0 ** (-8.0 * np.arange(1, NH + 1) / NH)
    tab = np.zeros((128, NH * 64), np.float32)
    for h in range(NH):
        for n in range(1, 65):
            tab[:, h * 64 + n - 1] = slopes[h] * (np.arange(128) + 1 - 128 * n)
    c["alibi"] = tab
    same = (k // 64) == (q // 64)
    c["negmaskU"] = np.where(same & (q >= k), 0.0, -30000.0).astype(np.float32)
    c["strictU"] = (same & (q > k)).astype(np.float32)
    c["Lcum"] = (same & (k <= q)).astype(np.float32)
    c["Lall"] = same.astype(np.float32)
    sel = np.zeros((16, 16 * 128), np.float32)
    for j in range(16):
        sel[j, j * 128:(j + 1) * 128] = 1.0
    c["sel16"] = sel
    c["sltU"] = (k < q).astype(np.float32)
    rm = np.zeros((128, 2), np.float32)
    rm[:64, 0] = 1.0
    rm[64:, 1] = 1.0
    c["rowmask"] = rm
    return c


CONST_SHAPES = {
    "ident_f": ([128, 128], F32), "ident_b": ([128, 128], BF16), "triu_b": ([128, 128], BF16),
    "alibi": ([128, NH * 64], F32), "negmaskU": ([128, 128], F32), "strictU": ([128, 128], F32),
    "Lcum": ([128, 128], F32), "Lall": ([128, 128], F32), "sel16": ([16, 16 * 128], F32),
    "sltU": ([128, 128], F32), "rowmask": ([128, 2], F32),
}


def declare_io(nc, S, NB):
    io = {}

    def inp(name, shape, dt=F32):
        io[name] = nc.dram_tensor(name, list(shape), dt, kind="ExternalInput").ap()

    def scr(name, shape, dt):
        io[name] = nc.dram_tensor(name, list(shape), dt, kind="Internal").ap()

    inp("x", [S, D])
    inp("c_t", [128, KC])
    inp("ada_w", [DEPTH, D, 6 * D])
    inp("ada_b_t", [DEPTH, 128, 48])
    inp("n1w_t", [DEPTH, 128, KC])
    inp("n2w_t", [DEPTH, 128, KC])
    inp("fnw_bc", [128, D])
    inp("w_in", [DEPTH, D, PTOT])
    inp("conv_t", [DEPTH, 128, 24, 4])
    inp("lamv", [DEPTH, 128, 4, 64])
    inp("subln_bc", [DEPTH, 128, 128])
    inp("alog_bc", [DEPTH, 128, NH])
    inp("dtb_bc", [DEPTH, 128, NH])
    inp("gnw_t", [DEPTH, 128, 1])
    inp("w_a", [DEPTH, D, D])
    inp("w_b", [DEPTH, D, D])
    inp("w_out", [DEPTH, D, D])
    inp("wr", [DEPTH, D, 36])
    inp("rb_bc", [DEPTH, 128, 36])
    inp("ew1", [DEPTH * NE * 128, KC * DE])
    inp("ew3", [DEPTH * NE * 128, KC * DE])
    inp("ew2", [DEPTH * NE * 128, 4 * D])
    for k, (shp, dt) in CONST_SHAPES.items():
        inp(k, shp, dt)
    io["y"] = nc.dram_tensor("y", [S, D], F32, kind="ExternalOutput").ap()
    scr("xr", [S, D], F32)
    scr("PT", [64 * 128, S], BF16)
    scr("Vtok", [S, D], BF16)
    scr("BD", [S, 16], F32)
    scr("YT", [16 * 128, S], BF16)
    inp("iota_nb", [128, NB])
    inp("iota_p", [128, 1])
    scr("h2d", [S, D], F32)
    scr("xs", [NB * 128, D], F32)
    scr("ys", [NB * 128, D], F32)
    scr("e1b", [NE, D, DE], BF16)
    scr("e3b", [NE, D, DE], BF16)
    scr("e2b", [NE, DE, D], BF16)
    scr("vecs", [DEPTH, 4, D], F32)
    return io


class Ctx:
    pass


def dbg_sb(g, name, tile_, keys):
    if name not in g.dbgset:
        return
    dst = g.nc.dram_tensor("dbg_" + name, list(tile_.shape), tile_.dtype, kind="ExternalOutput").ap()
    g.sc.dma("sp", lambda e: e.dma_start(out=dst, in_=tile_), r=keys, w=["dbg_" + name])


_UID = [0]


def _uname(name):
    _UID[0] += 1
    return f"{name}_{_UID[0]}"


def T(es, nc, name, shape, dt):
    return es.enter_context(nc.sbuf_tensor(_uname(name), list(shape), dt))


PSUM_NAMES = set()


def PS(es, nc, name, shape, dt):
    PSUM_NAMES.add(name)
    return es.enter_context(nc.psum_tensor(_uname(name), list(shape), dt))


def phase_setup(g):
    nc, sc, io, es = g.nc, g.sc, g.io, g.es
    g.cst = {}
    for k, (shp, dt) in CONST_SHAPES.items():
        t = T(es, nc, "c_" + k, shp, dt)
        g.cst[k] = t
        sc.dma("sp", lambda e, t=t, k=k: e.dma_start(out=t[:], in_=io[k]), w=["c_" + k])
    g.mod = T(es, nc, "mod", [128, DEPTH, 48], F32)
    g.A1 = T(es, nc, "A1", [128, DEPTH, KC], F32)
    g.A2 = T(es, nc, "A2", [128, DEPTH, KC], F32)
    g.ones_b = T(es, nc, "ones_b", [128, 128], BF16)
    g.ones_f = T(es, nc, "ones_f", [128, 128], F32)
    sc.op("pool", lambda e: e.memset(g.ones_b[:], 1.0), w=["ones_b"])
    sc.op("pool", lambda e: e.memset(g.ones_f[:], 1.0), w=["ones_f"])
    with ExitStack() as ls:
        ct = T(ls, nc, "ct", [128, KC], F32)
        cond = T(ls, nc, "cond", [128, KC], F32)
        adab = T(ls, nc, "adab", [128, DEPTH, 48], F32)
        nw = T(ls, nc, "nw", [128, 2, DEPTH, KC], F32)
        tmp = T(ls, nc, "tmpm", [128, DEPTH, KC], F32)
        wg = [T(ls, nc, f"wg{i}", [128, KC, 1024], F32) for i in range(2)]
        psm = PS(ls, nc, "psm", [128, DEPTH * 48], F32)
        sc.dma("sp", lambda e: e.dma_start(out=ct[:], in_=io["c_t"]), w=["ct"])
        sc.dma("sp", lambda e: e.dma_start(out=adab[:], in_=io["ada_b_t"].rearrange("l p f -> p l f")), w=["adab"])
        sc.dma("sp", lambda e: e.dma_start(out=nw[:, 0], in_=io["n1w_t"].rearrange("l p f -> p l f")), w=["nw"])
        sc.dma("sp", lambda e: e.dma_start(out=nw[:, 1], in_=io["n2w_t"].rearrange("l p f -> p l f")), w=["nw"])
        sc.op("act", lambda e: e.activation(out=cond[:], in_=ct[:], func=AF.Silu), r=["ct"], w=["cond"])
        i = 0
        for l in range(DEPTH):
            for gi in range(6):
                b = i % 2
                i += 1
                src = io["ada_w"][l].rearrange("(kc p) f -> p kc f", p=128)[:, :, gi * 1024:(gi + 1) * 1024]
                sc.dma("sp" if b == 0 else "act", lambda e, b=b, src=src: e.dma_start(out=wg[b][:], in_=src), w=[f"wg{b}"])
                for f in range(8):
                    col = l * 48 + gi * 8 + f
                    for kc in range(KC):
                        sc.op("pe", lambda e, b=b, f=f, kc=kc, col=col: e.matmul(
                            psm[:, col:col + 1], lhsT=wg[b][:, kc, f * 128:(f + 1) * 128], rhs=cond[:, kc:kc + 1],
                            start=(kc == 0), stop=(kc == KC - 1)), r=[f"wg{b}", "cond"], w=["psm"])
        sc.op("dve", lambda e: e.tensor_tensor(out=g.mod[:].rearrange("p l f -> p (l f)"), in0=psm[:],
                                               in1=adab[:].rearrange("p l f -> p (l f)"), op=ALU.add),
              r=["psm", "adab"], w=["mod"])
        sc.op("dve", lambda e: e.tensor_scalar(out=tmp[:], in0=g.mod[:, :, 8:16], scalar1=1.0, scalar2=None, op0=ALU.add),
              r=["mod"], w=["tmpm"])
        sc.op("dve", lambda e: e.tensor_tensor(out=g.A1[:], in0=tmp[:], in1=nw[:, 0], op=ALU.mult), r=["tmpm", "nw"], w=["A1"])
        sc.op("dve", lambda e: e.tensor_scalar(out=tmp[:], in0=g.mod[:, :, 32:40], scalar1=1.0, scalar2=None, op0=ALU.add),
              r=["mod", "A1"], w=["tmpm"])
        sc.op("dve", lambda e: e.tensor_tensor(out=g.A2[:], in0=tmp[:], in1=nw[:, 1], op=ALU.mult), r=["tmpm", "nw"], w=["A2"])
        with nc.allow_non_contiguous_dma(reason="tiny per-feature vectors"):
            for l in range(DEPTH):
                srcs = [g.mod[:, l, 16:24], g.A2[:, l, :], g.mod[:, l, 24:32], g.mod[:, l, 40:48]]
                for j, s_ in enumerate(srcs):
                    sc.dma("sp", lambda e, l=l, j=j, s_=s_: e.dma_start(
                        out=io["vecs"][l, j].rearrange("(kc p) -> p kc", p=128), in_=s_, allow_slow_non_contiguous=True),
                        r=["mod", "A2"], w=["vecs"])
        dbg_sb(g, "mod", g.mod[:], ["mod"])
        dbg_sb(g, "A1", g.A1[:], ["A1"])
        sc.barrier()


def load_bc(g, tile_, l, j, key):
    g.sc.dma("sp", lambda e: e.dma_start(out=tile_[:], in_=g.io["vecs"][l, j].partition_broadcast(128)), r=["vecs"], w=[key])


def phase_norm(g, l, xsrc, hT, Acol, Bcol, out_dt_tag="hT"):
    nc, sc, io = g.nc, g.sc, g.io
    NT = g.S // 128
    with ExitStack() as ls:
        if g.S < 2048:
            pad_ = T(ls, nc, "npad", [128, 16384], F32)
        xt = [T(ls, nc, f"nx{i}", [128, D], F32) for i in range(2)]
        xn = [T(ls, nc, f"nxn{i}", [128, D], F32) for i in range(2)]
        junk = T(ls, nc, "njunk", [128, D], BF16)
        st = [T(ls, nc, f"nst{i}", [128, 64], F32) for i in range(2)]
        pst = [PS(ls, nc, f"npst{i}", [128, 512], F32) for i in range(4)]
        for i in range(NT):
            b = i % 2
            sc.dma("sp", lambda e, b=b, i=i: e.dma_start(out=xt[b][:], in_=xsrc[i * 128:(i + 1) * 128, :]), w=[f"nx{b}"])
            sc.op("act", lambda e, b=b: e.activation(out=junk[:], in_=xt[b][:], func=AF.Square, accum_out=st[b][:, 0:1]),
                  r=[f"nx{b}"], w=["njunk", f"nst{b}"])
            sc.op("dve", lambda e, b=b: e.tensor_scalar(out=st[b][:, 16:17], in0=st[b][:, 0:1], scalar1=1.0 / D, scalar2=EPS,
                                                         op0=ALU.mult, op1=ALU.add), r=[f"nst{b}"], w=[f"nst{b}"])
            sc.op("act", lambda e, b=b: e.activation(out=st[b][:, 32:33], in_=st[b][:, 16:17], func=AF.Sqrt), r=[f"nst{b}"], w=[f"nst{b}"])
            sc.op("dve", lambda e, b=b: e.reciprocal(out=st[b][:, 48:49], in_=st[b][:, 32:33]), r=[f"nst{b}"], w=[f"nst{b}"])
            sc.op("dve", lambda e, b=b: e.tensor_scalar(out=xn[b][:], in0=xt[b][:], scalar1=st[b][:, 48:49], scalar2=None, op0=ALU.mult),
                  r=[f"nx{b}", f"nst{b}"], w=[f"nxn{b}"])
            for kc in range(KC):
                pb = (i % 2) * 2 + kc // 4
                sc.op("pe", lambda e, b=b, kc=kc, pb=pb: e.transpose(pst[pb][:, (kc % 4) * 128:(kc % 4 + 1) * 128],
                                                                     xn[b][:, kc * 128:(kc + 1) * 128], g.cst["ident_f"][:]),
                      r=[f"nxn{b}", "c_ident_f"], w=[f"npst{pb}"])
            for kc in range(KC):
                pb = (i % 2) * 2 + kc // 4
                sc.op("act", lambda e, kc=kc, pb=pb, i=i: e.activation(
                    out=hT[:, kc, i * 128:(i + 1) * 128], in_=pst[pb][:, (kc % 4) * 128:(kc % 4 + 1) * 128],
                    func=AF.Identity, bias=Bcol[:, kc:kc + 1], scale=Acol[:, kc:kc + 1]),
                    r=[f"npst{pb}", "mod", "A1", "A2"], w=[f"{out_dt_tag}{i}"])


FM_BLOCKS = []
for _c in range(0, 1024, 512):
    FM_BLOCKS.append((_c, _c // 128, False))
for _c in range(0, 1024, 512):
    FM_BLOCKS.append((1024 + _c, 8 + _c // 128, False))
for _c in range(0, 3072, 512):
    FM_BLOCKS.append((3072 + _c, 16 + _c // 128, False))
for _c in range(0, 1024, 512):
    FM_BLOCKS.append((6144 + _c, 40 + _c // 128, False))
for _c in range(0, 2048, 512):
    FM_BLOCKS.append((7184 + _c, 48 + _c // 128, True))


def phase_proj(g, l, hT):
    nc, sc, io = g.nc, g.sc, g.io
    S = g.S
    NTC = S // 512
    NT = S // 128
    winl = io["w_in"][l].rearrange("(kc p) f -> p kc f", p=128)
    PTv = io["PT"].rearrange("(c p) t -> p c t", p=128)
    with ExitStack() as ls:
        wblk = [T(ls, nc, f"wblk{i}", [128, KC, 512], BF16) for i in range(2)]
        wst = [T(ls, nc, f"wst{i}", [128, KC, 512], F32) for i in range(2)]
        stg = [T(ls, nc, f"pstg{i}", [128, 4, 512], BF16) for i in range(2)]
        pp = [PS(ls, nc, f"pp{i}", [128, 512], F32) for i in range(8)]
        hkeys = lambda tc: [f"hT{i}" for i in range(tc * 4, tc * 4 + 4)]
        n = 0
        for bi, (c0, ch0, sig) in enumerate(FM_BLOCKS):
            wb = bi % 2
            sc.dma("sp", lambda e, wb=wb, c0=c0: e.dma_start(out=wst[wb][:], in_=winl[:, :, c0:c0 + 512]), w=[f"wst{wb}"])
            sc.op("pool", lambda e, wb=wb: e.tensor_copy(out=wblk[wb][:], in_=wst[wb][:]), r=[f"wst{wb}"], w=[f"wblk{wb}"])
            for tc in range(NTC):
                sb = n % 2
                for fc in range(4):
                    pb = (n % 2) * 4 + fc
                    for kc in range(KC):
                        sc.op("pe", lambda e, wb=wb, fc=fc, kc=kc, pb=pb, tc=tc: e.matmul(
                            pp[pb][:], lhsT=wblk[wb][:, kc, fc * 128:(fc + 1) * 128], rhs=hT[:, kc, tc * 512:(tc + 1) * 512],
                            start=(kc == 0), stop=(kc == KC - 1)), r=[f"wblk{wb}"] + hkeys(tc), w=[f"pp{pb}"])
                    if sig:
                        sc.op("act", lambda e, sb=sb, fc=fc, pb=pb: e.activation(out=stg[sb][:, fc, :], in_=pp[pb][:], func=AF.Sigmoid),
                              r=[f"pp{pb}"], w=[f"pstg{sb}"])
                    elif fc % 2 == 0:
                        sc.op("act", lambda e, sb=sb, fc=fc, pb=pb: e.activation(out=stg[sb][:, fc, :], in_=pp[pb][:], func=AF.Copy),
                              r=[f"pp{pb}"], w=[f"pstg{sb}"])
                    else:
                        sc.op("dve", lambda e, sb=sb, fc=fc, pb=pb: e.tensor_copy(out=stg[sb][:, fc, :], in_=pp[pb][:]),
                              r=[f"pp{pb}"], w=[f"pstg{sb}"])
                sc.dma("sp", lambda e, sb=sb, ch0=ch0, tc=tc: e.dma_start(
                    out=PTv[:, ch0:ch0 + 4, tc * 512:(tc + 1) * 512], in_=stg[sb][:]), r=[f"pstg{sb}"], w=["PT"])
                n += 1
    sc.barrier()
    with ExitStack() as ls:
        wv = T(ls, nc, "wv", [128, KC, 1024], BF16)
        wbd = T(ls, nc, "wbd", [128, KC, 16], BF16)
        vst = [T(ls, nc, f"vst{i}", [128, 1024], BF16) for i in range(2)]
        bst = [T(ls, nc, f"bst{i}", [128, 16], F32) for i in range(2)]
        pv = [PS(ls, nc, f"pv{i}", [128, 512], F32) for i in range(6)]
        wst2 = [T(ls, nc, f"wst2{i}", [128, KC, 512], F32) for i in range(2)]
        wbdf = T(ls, nc, "wbdf", [128, KC, 16], F32)
        for hf in range(2):
            sc.dma("sp", lambda e, hf=hf: e.dma_start(out=wst2[hf][:], in_=winl[:, :, 2048 + hf * 512:2048 + (hf + 1) * 512]), w=[f"wst2{hf}"])
            sc.op("pool", lambda e, hf=hf: e.tensor_copy(out=wv[:, :, hf * 512:(hf + 1) * 512], in_=wst2[hf][:]), r=[f"wst2{hf}"], w=["wv"])
        sc.dma("sp", lambda e: e.dma_start(out=wbdf[:], in_=winl[:, :, 7168:7184], allow_slow_non_contiguous=True), w=["wbdf"])
        sc.op("pool", lambda e: e.tensor_copy(out=wbd[:], in_=wbdf[:]), r=["wbdf"], w=["wbd"])
        for i in range(NT):
            b = i % 2
            for hf in range(2):
                pb = b * 3 + hf
                for kc in range(KC):
                    sc.op("pe", lambda e, kc=kc, hf=hf, pb=pb, i=i: e.matmul(
                        pv[pb][:], lhsT=hT[:, kc, i * 128:(i + 1) * 128], rhs=wv[:, kc, hf * 512:(hf + 1) * 512],
                        start=(kc == 0), stop=(kc == KC - 1)), r=["wv", f"hT{i}"], w=[f"pv{pb}"])
            pb2 = b * 3 + 2
            for kc in range(KC):
                sc.op("pe", lambda e, kc=kc, pb2=pb2, i=i: e.matmul(
                    pv[pb2][:, 0:16], lhsT=hT[:, kc, i * 128:(i + 1) * 128], rhs=wbd[:, kc, :],
                    start=(kc == 0), stop=(kc == KC - 1)), r=["wbd", f"hT{i}"], w=[f"pv{pb2}"])
            sc.op("act", lambda e, b=b: e.activation(out=vst[b][:, 0:512], in_=pv[b * 3][:], func=AF.Copy), r=[f"pv{b*3}"], w=[f"vst{b}"])
            sc.op("dve", lambda e, b=b: e.tensor_copy(out=vst[b][:, 512:1024], in_=pv[b * 3 + 1][:]), r=[f"pv{b*3+1}"], w=[f"vst{b}"])
            sc.op("dve", lambda e, b=b, pb2=pb2: e.tensor_copy(out=bst[b][:], in_=pv[pb2][:, 0:16]), r=[f"pv{pb2}"], w=[f"bst{b}"])
            sc.dma("sp", lambda e, b=b, i=i: e.dma_start(out=io["Vtok"][i * 128:(i + 1) * 128, :], in_=vst[b][:]), r=[f"vst{b}"], w=["Vtok"])
            sc.dma("sp", lambda e, b=b, i=i: e.dma_start(out=io["BD"][i * 128:(i + 1) * 128, :], in_=bst[b][:]), r=[f"bst{b}"], w=["BD"])


GRP = [128, 256, 512, 512, 512, 512, 512, 512]


def phase_attn(g, l):
    nc, sc, io = g.nc, g.sc, g.io
    S = g.S
    NT = S // 128
    NQC = S // 512
    lam_init = 0.8 - 0.6 * math.exp(-0.3 * l)
    Vv = io["Vtok"].rearrange("(i p) f -> p i f", p=128)
    with ExitStack() as ls:
        QT = [T(ls, nc, f"aQT{i}", [128, S], BF16) for i in range(2)]
        KT = [T(ls, nc, f"aKT{i}", [128, S], BF16) for i in range(2)]
        VT = [T(ls, nc, f"aVT{i}", [128, NT, 129], BF16) for i in range(2)]
        pT = [T(ls, nc, f"apT{i}", [128, 512], BF16) for i in range(3)]
        O1 = [T(ls, nc, f"aO1{j}", [128, 128], F32) for j in range(4)]
        Ot = [T(ls, nc, f"aO{j}", [128, 128], F32) for j in range(4)]
        yb = [T(ls, nc, f"ayb{j}", [128, 128], BF16) for j in range(4)]
        sq = T(ls, nc, "asq", [128, 128], BF16)
        stt = [T(ls, nc, f"ast{j}", [128, 128], F32) for j in range(4)]
        yst = [T(ls, nc, f"ayst{i}", [128, 512], BF16) for i in range(2)]
        lamt = T(ls, nc, "alam", [128, 4, 64], F32)
        lamp = T(ls, nc, "alamp", [128, 2, 64], F32)
        lams = T(ls, nc, "alams", [128, 128], F32)
        subw = T(ls, nc, "asubw", [128, 128], F32)
        ps_s = [PS(ls, nc, f"aps{i}", [128, 512], F32) for i in range(2)]
        ps_o = [PS(ls, nc, f"apo{j}", [128, 512], F32) for j in range(4)]
        ps_t = PS(ls, nc, "apt", [128, 1024], BF16)
        sc.dma("sp", lambda e: e.dma_start(out=lamt[:], in_=io["lamv"][l]), w=["alam"])
        sc.dma("sp", lambda e: e.dma_start(out=subw[:], in_=io["subln_bc"][l]), w=["asubw"])
        sc.op("dve", lambda e: e.tensor_tensor(out=lamp[:, 0, :], in0=lamt[:, 0, :], in1=lamt[:, 1, :], op=ALU.mult), r=["alam"], w=["alamp"])
        sc.op("dve", lambda e: e.tensor_tensor(out=lamp[:, 1, :], in0=lamt[:, 2, :], in1=lamt[:, 3, :], op=ALU.mult), r=["alam"], w=["alamp"])
        sc.op("dve", lambda e: e.tensor_reduce(out=lams[:, 0:1], in_=lamp[:, 0, :], axis=AX.X, op=ALU.add), r=["alamp"], w=["alams"])
        sc.op("dve", lambda e: e.tensor_reduce(out=lams[:, 16:17], in_=lamp[:, 1, :], axis=AX.X, op=ALU.add), r=["alamp"], w=["alams"])
        sc.op("act", lambda e: e.activation(out=lams[:, 32:33], in_=lams[:, 0:1], func=AF.Exp), r=["alams"], w=["alams"])
        sc.op("act", lambda e: e.activation(out=lams[:, 48:49], in_=lams[:, 16:17], func=AF.Exp), r=["alams"], w=["alams"])
        sc.op("dve", lambda e: e.tensor_tensor(out=lams[:, 64:65], in0=lams[:, 48:49], in1=lams[:, 32:33], op=ALU.subtract), r=["alams"], w=["alams"])
        sc.op("dve", lambda e: e.tensor_scalar(out=lams[:, 80:81], in0=lams[:, 64:65], scalar1=-lam_init, scalar2=None, op0=ALU.add),
              r=["alams"], w=["alams"])
        neglam = lams[:, 80:81]
        sc.op("dve", lambda e: e.tensor_scalar(out=subw[:], in0=subw[:], scalar1=(1.0 - lam_init), scalar2=None, op0=ALU.mult),
              r=["asubw"], w=["asubw"])
        for hb in range(2):
            sc.op("pool", lambda e, hb=hb: e.memset(VT[hb][:, :, 128:129], 1.0), w=[f"aVT{hb}"])
        n_u = 0
        n_y = 0
        for h in range(NH):
            hb = h % 2
            sc.dma("sp", lambda e, hb=hb, h=h: e.dma_start(out=QT[hb][:], in_=io["PT"][h * 128:(h + 1) * 128, :]), r=["PT"], w=[f"aQT{hb}"])
            sc.dma("sp", lambda e, hb=hb, h=h: e.dma_start(out=KT[hb][:], in_=io["PT"][(8 + h) * 128:(9 + h) * 128, :]), r=["PT"], w=[f"aKT{hb}"])
            sc.dma("sp", lambda e, hb=hb, h=h: e.dma_start(out=VT[hb][:, :, 0:128], in_=Vv[:, :, h * 128:(h + 1) * 128]), r=["Vtok"], w=[f"aVT{hb}"])
            G = GRP[h]
            for qc in range(NQC):
                for m in range(2):
                    nkb = 4 * qc + 4
                    for kb in range(nkb):
                        kl = kb - 4 * qc
                        j0 = max(0, kl)
                        sb = n_u % 2
                        pb = n_u % 3
                        n_u += 1
                        c0 = j0 * 128
                        sc.op("pe", lambda e, hb=hb, m=m, kb=kb, qc=qc, sb=sb, c0=c0: e.matmul(
                            ps_s[sb][:, c0:512], lhsT=KT[hb][m * 64:(m + 1) * 64, kb * 128:(kb + 1) * 128],
                            rhs=QT[hb][m * 64:(m + 1) * 64, qc * 512 + c0:qc * 512 + 512], start=True, stop=True),
                            r=[f"aKT{hb}", f"aQT{hb}"], w=[f"aps{sb}"])
                        cs = c0
                        while cs < 512:
                            ce = min(512, (cs // G + 1) * G)
                            nn = (512 * qc + ce - 128 * kb) // 128
                            col = h * 64 + nn - 1
                            sc.op("act", lambda e, sb=sb, pb=pb, cs=cs, ce=ce, col=col: e.activation(
                                out=pT[pb][:, cs:ce], in_=ps_s[sb][:, cs:ce], func=AF.Exp,
                                bias=g.cst["alibi"][:, col:col + 1], scale=0.125), r=[f"aps{sb}", "c_alibi"], w=[f"apT{pb}"])
                            cs = ce
                        if kl >= 0:
                            sc.op("pool", lambda e, pb=pb, kl=kl: e.tensor_tensor(
                                out=pT[pb][:, kl * 128:(kl + 1) * 128], in0=pT[pb][:, kl * 128:(kl + 1) * 128],
                                in1=g.cst["triu_b"][:], op=ALU.mult), r=[f"apT{pb}", "c_triu_b"], w=[f"apT{pb}"])
                        for j in range(j0, 4):
                            sc.op("pe", lambda e, pb=pb, j=j, hb=hb, kb=kb, qc=qc: e.matmul(
                                ps_o[j][:, 0:129], lhsT=pT[pb][:, j * 128:(j + 1) * 128], rhs=VT[hb][:, kb, :],
                                start=(kb == 0), stop=(kb == 4 * qc + j)), r=[f"apT{pb}", f"aVT{hb}"], w=[f"apo{j}"])
                    for j in range(4):
                        st_ = stt[j]
                        sc.op("dve", lambda e, j=j, st_=st_: e.reciprocal(out=st_[:, 0:1], in_=ps_o[j][:, 128:129]), r=[f"apo{j}"], w=[f"ast{j}"])
                        if m == 0:
                            sc.op("dve", lambda e, j=j, st_=st_: e.tensor_scalar(out=O1[j][:], in0=ps_o[j][:, 0:128], scalar1=st_[:, 0:1],
                                                                                scalar2=None, op0=ALU.mult), r=[f"apo{j}", f"ast{j}"], w=[f"aO1{j}"])
                        else:
                            sc.op("dve", lambda e, st_=st_: e.tensor_tensor(out=st_[:, 16:17], in0=st_[:, 0:1], in1=neglam, op=ALU.mult),
                                  r=[f"ast{j}", "alams"], w=[f"ast{j}"])
                            sc.op("dve", lambda e, j=j, st_=st_: e.scalar_tensor_tensor(
                                out=Ot[j][:], in0=ps_o[j][:, 0:128], scalar=st_[:, 16:17], in1=O1[j][:], op0=ALU.mult, op1=ALU.add),
                                r=[f"apo{j}", f"ast{j}", f"aO1{j}"], w=[f"aO{j}"])
                            sc.op("act", lambda e, j=j, st_=st_: e.activation(out=sq[:], in_=Ot[j][:], func=AF.Square, accum_out=st_[:, 32:33]),
                                  r=[f"aO{j}"], w=["asq", f"ast{j}"])
                            sc.op("dve", lambda e, st_=st_: e.tensor_scalar(out=st_[:, 48:49], in0=st_[:, 32:33], scalar1=1.0 / 128, scalar2=EPS,
                                                                         op0=ALU.mult, op1=ALU.add), r=[f"ast{j}"], w=[f"ast{j}"])
                            sc.op("act", lambda e, st_=st_: e.activation(out=st_[:, 64:65], in_=st_[:, 48:49], func=AF.Ln), r=[f"ast{j}"], w=[f"ast{j}"])
                            sc.op("act", lambda e, st_=st_: e.activation(out=st_[:, 80:81], in_=st_[:, 64:65], func=AF.Exp, scale=-0.5),
                                  r=[f"ast{j}"], w=[f"ast{j}"])
                            sc.op("dve", lambda e, j=j, st_=st_: e.scalar_tensor_tensor(
                                out=yb[j][:], in0=Ot[j][:], scalar=st_[:, 80:81], in1=subw[:], op0=ALU.mult, op1=ALU.mult),
                                r=[f"aO{j}", f"ast{j}", "asubw"], w=[f"ayb{j}"])
                            sc.op("pe", lambda e, j=j: e.transpose(ps_t[:, j * 128:(j + 1) * 128], yb[j][:], g.cst["ident_b"][:]),
                                  r=[f"ayb{j}", "c_ident_b"], w=["apt"])
                    if m == 1:
                        ysb = n_y % 2
                        n_y += 1
                        sc.op("dve", lambda e, ysb=ysb: e.tensor_copy(out=yst[ysb][:], in_=ps_t[:, 0:512]), r=["apt"], w=[f"ayst{ysb}"])
                        sc.dma("sp", lambda e, ysb=ysb, h=h, qc=qc: e.dma_start(
                            out=io["YT"][h * 128:(h + 1) * 128, qc * 512:(qc + 1) * 512], in_=yst[ysb][:]), r=[f"ayst{ysb}"], w=["YT"])


def phase_gdn(g, l):
    nc, sc, io = g.nc, g.sc, g.io
    S = g.S
    NT = S // 128
    CW = min(2048, S)
    NCW = S // CW
    cst = g.cst
    with ExitStack() as ls:
        SC = T(ls, nc, "gSC", [128, NT, 5, 8], F32)
        nega = T(ls, nc, "gnega", [128, 8], F32)
        dtb = T(ls, nc, "gdtb", [128, 8], F32)
        convw = T(ls, nc, "gconvw", [128, 24, 4], F32)
        gnw = T(ls, nc, "ggnw", [128, 1], F32)
        sc.dma("sp", lambda e: e.dma_start(out=nega[:], in_=io["alog_bc"][l]), w=["gnega"])
        sc.dma("sp", lambda e: e.dma_start(out=dtb[:], in_=io["dtb_bc"][l]), w=["gdtb"])
        sc.dma("sp", lambda e: e.dma_start(out=convw[:], in_=io["conv_t"][l]), w=["gconvw"])
        sc.dma("sp", lambda e: e.dma_start(out=gnw[:], in_=io["gnw_t"][l]), w=["ggnw"])
        sc.op("act", lambda e: e.activation(out=nega[:], in_=nega[:], func=AF.Exp), r=["gnega"], w=["gnega"])
        sc.op("dve", lambda e: e.tensor_scalar(out=nega[:], in0=nega[:], scalar1=-1.0, scalar2=None, op0=ALU.mult), r=["gnega"], w=["gnega"])
        with ExitStack() as la:
            bd = [T(la, nc, f"gbd{i}", [128, 16], F32) for i in range(2)]
            tg = [T(la, nc, f"gtg{i}", [128, 4, 8], F32) for i in range(2)]
            psa = [PS(la, nc, f"gpsa{i}", [128, 512], F32) for i in range(2)]
            for i in range(NT):
                b = i % 2
                sc.dma("sp", lambda e, b=b, i=i: e.dma_start(out=bd[b][:], in_=io["BD"][i * 128:(i + 1) * 128, :]), r=["BD"], w=[f"gbd{b}"])
                sc.op("act", lambda e, b=b, i=i: e.activation(out=SC[:, i, 2, :], in_=bd[b][:, 0:8], func=AF.Sigmoid), r=[f"gbd{b}"], w=[f"gSC{i}"])
                sc.op("dve", lambda e, b=b: e.tensor_tensor(out=tg[b][:, 0, :], in0=bd[b][:, 8:16], in1=dtb[:], op=ALU.add), r=[f"gbd{b}", "gdtb"], w=[f"gtg{b}"])
                sc.op("act", lambda e, b=b: e.activation(out=tg[b][:, 1, :], in_=tg[b][:, 0, :], func=AF.Exp), r=[f"gtg{b}"], w=[f"gtg{b}"])
                sc.op("dve", lambda e, b=b: e.tensor_scalar(out=tg[b][:, 1, :], in0=tg[b][:, 1, :], scalar1=1.0, scalar2=None, op0=ALU.add), r=[f"gtg{b}"], w=[f"gtg{b}"])
                sc.op("act", lambda e, b=b: e.activation(out=tg[b][:, 2, :], in_=tg[b][:, 1, :], func=AF.Ln), r=[f"gtg{b}"], w=[f"gtg{b}"])
                sc.op("dve", lambda e, b=b: e.tensor_tensor(out=tg[b][:, 3, :], in0=tg[b][:, 2, :], in1=nega[:], op=ALU.mult), r=[f"gtg{b}", "gnega"], w=[f"gtg{b}"])
                sc.op("pe", lambda e, b=b: e.matmul(psa[b][:, 0:8], lhsT=cst["Lcum"][:], rhs=tg[b][:, 3, :], start=True, stop=True),
                      r=[f"gtg{b}", "c_Lcum"], w=[f"gpsa{b}"])
                sc.op("pe", lambda e, b=b: e.matmul(psa[b][:, 8:16], lhsT=cst["Lall"][:], rhs=tg[b][:, 3, :], start=True, stop=True),
                      r=[f"gtg{b}", "c_Lall"], w=[f"gpsa{b}"])
                sc.op("dve", lambda e, b=b, i=i: e.tensor_copy(out=SC[:, i, 0, :], in_=psa[b][:, 0:8]), r=[f"gpsa{b}"], w=[f"gSC{i}"])
                sc.op("dve", lambda e, b=b, i=i: e.tensor_scalar(out=SC[:, i, 1, :], in0=psa[b][:, 0:8], scalar1=-1.0, scalar2=None, op0=ALU.mult),
                      r=[f"gpsa{b}"], w=[f"gSC{i}"])
                sc.op("act", lambda e, b=b, i=i: e.activation(out=SC[:, i, 3, :], in_=psa[b][:, 0:8], func=AF.Exp), r=[f"gpsa{b}"], w=[f"gSC{i}"])
                sc.op("dve", lambda e, i=i: e.tensor_tensor(out=SC[:, i, 3, :], in0=SC[:, i, 3, :], in1=SC[:, i, 2, :], op=ALU.mult), r=[f"gSC{i}"], w=[f"gSC{i}"])
                sc.op("dve", lambda e, b=b, i=i: e.tensor_tensor(out=SC[:, i, 4, :], in0=psa[b][:, 8:16], in1=SC[:, i, 0, :], op=ALU.subtract),
                      r=[f"gpsa{b}", f"gSC{i}"], w=[f"gSC{i}"])
                sc.op("act", lambda e, i=i: e.activation(out=SC[:, i, 4, :], in_=SC[:, i, 4, :], func=AF.Exp), r=[f"gSC{i}"], w=[f"gSC{i}"])
        sc.barrier()
        if getattr(g, "gdn_stop", None) == "A":
            return
        raw = T(ls, nc, "graw", [128, S + 3], BF16)
        QTn = T(ls, nc, "gQTn", [128, S], BF16)
        KTn = T(ls, nc, "gKTn", [128, S], BF16)
        Ktok = T(ls, nc, "gKtok", [128, NT, 128], BF16)
        Vtk = T(ls, nc, "gVtk", [128, NT, 128], BF16)
        zsT = T(ls, nc, "gzsT", [128, S], BF16)
        acc = T(ls, nc, "gacc", [128, CW], F32)
        yv = T(ls, nc, "gyv", [128, CW], F32)
        ybf = T(ls, nc, "gybf", [128, CW], BF16)
        sqb = T(ls, nc, "gsqb", [128, 512], BF16)
        rnt = T(ls, nc, "grnt", [128, 512], F32)
        Sf = T(ls, nc, "gSf", [128, 128], F32)
        Sb = T(ls, nc, "gSb", [128, 128], BF16)
        names = ["dg", "db", "E", "decT", "eG", "Bst", "t1", "u"]
        f32t = {n: T(ls, nc, "g_" + n, [128, 128], F32) for n in names}
        bnames = ["AT", "A", "ATn", "An", "TT", "intraT", "vb", "kbg", "kdec", "qgT", "wT", "vn", "sq2", "yo"]
        bft = {n: T(ls, nc, "g_" + n, [128, 128], BF16) for n in bnames}
        Fu = [f32t["u"], T(ls, nc, "g_u1", [128, 128], F32)]
        FeG = [f32t["eG"], T(ls, nc, "g_eG1", [128, 128], F32)]
        BwT = [bft["wT"], T(ls, nc, "g_wT1", [128, 128], BF16)]
        Bkd = [bft["kdec"], T(ls, nc, "g_kdec1", [128, 128], BF16)]
        Bqg = [bft["qgT"], T(ls, nc, "g_qgT1", [128, 128], BF16)]
        Bin = [bft["intraT"], T(ls, nc, "g_intraT1", [128, 128], BF16)]
        rn2 = T(ls, nc, "g_rn2", [128, 128], F32)
        t2 = T(ls, nc, "g_t2", [128, 128], F32)
        ystg = [T(ls, nc, f"gystg{i}", [128, 512], BF16) for i in range(2)]
        pG = PS(ls, nc, "gpG", [128, 512], F32)
        pK = PS(ls, nc, "gpK", [128, 512], F32)
        pP = PS(ls, nc, "gpP", [128, 512], F32)
        pTu = PS(ls, nc, "gpTu", [128, 512], F32)
        pU = PS(ls, nc, "gpU", [128, 512], F32)
        pV = PS(ls, nc, "gpV", [128, 512], F32)
        pO = PS(ls, nc, "gpO", [128, 512], F32)
        pB = PS(ls, nc, "gpB", [128, 1024], BF16)
        sc.op("pool", lambda e: e.memset(raw[:, 0:3], 0.0), w=["graw"])
        vnc = [T(ls, nc, f"g_vnc{i}", [128, 128], BF16) for i in range(2)]
        for i_ in range(2):
            sc.op("pool", lambda e, i_=i_: e.memset(vnc[i_][:], 0.0), w=[f"g_vn{i_}"])
        n_st = 0
        for h in range(NH):
            for which in range(3):
                chunk = 16 + which * 8 + h
                sc.dma("sp", lambda e, chunk=chunk: e.dma_start(out=raw[:, 3:3 + S], in_=io["PT"][chunk * 128:(chunk + 1) * 128, :]), r=["PT"], w=["graw"])
                cc = which * 8 + h
                for cw in range(NCW):
                    c0 = cw * CW
                    sc.op("dve", lambda e, c0=c0, cc=cc: e.tensor_scalar(out=acc[:], in0=raw[:, c0:c0 + CW], scalar1=convw[:, cc, 0:1], scalar2=None,
                                                                      op0=ALU.mult), r=["graw", "gconvw"], w=["gacc"])
                    for k in range(1, 4):
                        sc.op("dve", lambda e, c0=c0, cc=cc, k=k: e.scalar_tensor_tensor(
                            out=acc[:], in0=raw[:, c0 + k:c0 + k + CW], scalar=convw[:, cc, k:k + 1], in1=acc[:], op0=ALU.mult, op1=ALU.add),
                            r=["graw", "gconvw", "gacc"], w=["gacc"])
                    if which == 2:
                        sc.op("act", lambda e: e.activation(out=ybf[:], in_=acc[:], func=AF.Silu), r=["gacc"], w=["gybf"])
                        for tt in range(CW // 128):
                            ti = c0 // 128 + tt
                            sc.op("pe", lambda e, tt=tt: e.transpose(pB[:, (tt % 4) * 128:(tt % 4 + 1) * 128], ybf[:, tt * 128:(tt + 1) * 128], cst["ident_b"][:]),
                                  r=["gybf", "c_ident_b"], w=["gpB"])
                            sc.op("act", lambda e, tt=tt, ti=ti: e.activation(out=Vtk[:, ti, :], in_=pB[:, (tt % 4) * 128:(tt % 4 + 1) * 128], func=AF.Copy),
                                  r=["gpB"], w=["gVtk"])
                    else:
                        dst = QTn if which == 0 else KTn
                        dkey = "gQTn" if which == 0 else "gKTn"
                        sc.op("act", lambda e: e.activation(out=yv[:], in_=acc[:], func=AF.Silu), r=["gacc"], w=["gyv"])
                        for sbk in range(CW // 512):
                            cs = sbk * 512
                            sc.op("pool", lambda e, cs=cs: e.tensor_tensor(out=sqb[:], in0=yv[:, cs:cs + 512], in1=yv[:, cs:cs + 512], op=ALU.mult),
                                  r=["gyv"], w=["gsqb"])
                            sc.op("pe", lambda e: e.matmul(pTu[:, 0:512], lhsT=g.ones_b[:], rhs=sqb[:], start=True, stop=True), r=["gsqb", "ones_b"], w=["gpTu"])
                            sc.op("dve", lambda e: e.tensor_scalar(out=rnt[:], in0=pTu[:, 0:512], scalar1=EPS, scalar2=None, op0=ALU.add), r=["gpTu"], w=["grnt"])
                            sc.op("act", lambda e: e.activation(out=rnt[:], in_=rnt[:], func=AF.Ln), r=["grnt"], w=["grnt"])
                            sc.op("act", lambda e: e.activation(out=rnt[:], in_=rnt[:], func=AF.Exp, scale=-0.5), r=["grnt"], w=["grnt"])
                            qs = (128.0 ** -0.5) if which == 0 else 1.0
                            sc.op("dve", lambda e, cs=cs, c0=c0, dst=dst, qs=qs: e.scalar_tensor_tensor(
                                out=dst[:, c0 + cs:c0 + cs + 512], in0=yv[:, cs:cs + 512], scalar=qs, in1=rnt[:], op0=ALU.mult, op1=ALU.mult),
                                r=["gyv", "grnt"], w=[dkey])
                        if which == 1:
                            for tt in range(CW // 128):
                                ti = c0 // 128 + tt
                                sc.op("pe", lambda e, tt=tt, ti=ti: e.transpose(pB[:, (tt % 4) * 128:(tt % 4 + 1) * 128], KTn[:, ti * 128:(ti + 1) * 128], cst["ident_b"][:]),
                                      r=["gKTn", "c_ident_b"], w=["gpB"])
                                sc.op("act", lambda e, tt=tt, ti=ti: e.activation(out=Ktok[:, ti, :], in_=pB[:, (tt % 4) * 128:(tt % 4 + 1) * 128], func=AF.Copy),
                                      r=["gpB"], w=["gKtok"])
            zc = 40 + h
            sc.dma("sp", lambda e, zc=zc: e.dma_start(out=raw[:, 3:3 + S], in_=io["PT"][zc * 128:(zc + 1) * 128, :]), r=["PT"], w=["graw"])
            sc.op("act", lambda e: e.activation(out=zsT[:], in_=raw[:, 3:3 + S], func=AF.Silu), r=["graw"], w=["gzsT"])
            sc.op("pool", lambda e: e.memset(Sf[:], 0.0), w=["gSf"])
            sc.op("pool", lambda e: e.memset(Sb[:], 0.0), w=["gSb"])
            if getattr(g, "gdn_stop", None) == "B":
                return
            F = f32t
            Bt = bft
            def tpart(i):
                par = i % 2
                tsl = slice(i * 128, (i + 1) * 128)
                sck = [f"gSC{i}"]
                gc_col = SC[:, i, 0, h:h + 1]
                ngc_col = SC[:, i, 1, h:h + 1]
                be_col = SC[:, i, 2, h:h + 1]
                bege_col = SC[:, i, 3, h:h + 1]
                kd_col = SC[:, i, 4, h:h + 1]
                sc.op("dve", lambda e: e.tensor_scalar(out=F["dg"][:], in0=cst["ident_f"][:], scalar1=gc_col, scalar2=None, op0=ALU.mult), r=sck + ["c_ident_f"], w=["g_dg"])
                yield
                sc.op("dve", lambda e: e.tensor_scalar(out=F["db"][:], in0=cst["ident_f"][:], scalar1=be_col, scalar2=None, op0=ALU.mult), r=sck + ["c_ident_f"], w=["g_db"])
                yield
                sc.op("pe", lambda e: e.matmul(pG[:, 0:128], lhsT=g.ones_f[:], rhs=F["dg"][:], start=True, stop=True), r=["g_dg", "ones_f"], w=["gpG"])
                yield
                sc.op("pe", lambda e: e.matmul(pG[:, 128:256], lhsT=g.ones_f[:], rhs=F["db"][:], start=True, stop=True), r=["g_db", "ones_f"], w=["gpG"])
                yield
                sc.op("dve", lambda e: e.tensor_tensor(out=F["E"][:], in0=pG[:, 0:128], in1=cst["negmaskU"][:], op=ALU.add), r=["gpG", "c_negmaskU"], w=["g_E"])
                yield
                sc.op("act", lambda e: e.activation(out=F["decT"][:], in_=F["E"][:], func=AF.Exp, bias=ngc_col, scale=1.0), r=["g_E"] + sck, w=["g_decT"])
                yield
                sc.op("act", lambda e: e.activation(out=FeG[par][:], in_=pG[:, 0:128], func=AF.Exp), r=["gpG"], w=[f"g_eG{par}"])
                yield
                sc.op("dve", lambda e: e.tensor_tensor(out=F["Bst"][:], in0=pG[:, 128:256], in1=cst["strictU"][:], op=ALU.mult), r=["gpG", "c_strictU"], w=["g_Bst"])
                yield
                if g.gdn_stop == "C1":
                    return
                sc.op("pe", lambda e, tsl=tsl: e.matmul(pK[:, 0:128], lhsT=KTn[:, tsl], rhs=KTn[:, tsl], start=True, stop=True), r=["gKTn"], w=["gpK"])
                yield
                sc.op("pe", lambda e, tsl=tsl: e.matmul(pK[:, 128:256], lhsT=KTn[:, tsl], rhs=QTn[:, tsl], start=True, stop=True), r=["gKTn", "gQTn"], w=["gpK"])
                yield
                sc.op("dve", lambda e: e.tensor_tensor(out=F["t1"][:], in0=pK[:, 0:128], in1=F["decT"][:], op=ALU.mult), r=["gpK", "g_decT"], w=["g_t1"])
                yield
                sc.op("dve", lambda e: e.scalar_tensor_tensor(out=Bt["ATn"][:], in0=F["t1"][:], scalar=-1.0, in1=F["Bst"][:], op0=ALU.mult, op1=ALU.mult),
                      r=["g_t1", "g_Bst"], w=["g_ATn"])
                yield
                sc.op("dve", lambda e: e.tensor_tensor(out=Bin[par][:], in0=pK[:, 128:256], in1=F["decT"][:], op=ALU.mult), r=["gpK", "g_decT"], w=[f"g_intraT{par}"])
                yield
                sc.op("pe", lambda e: e.transpose(pB[:, 512:640], Bt["ATn"][:], cst["ident_b"][:]), r=["g_ATn", "c_ident_b"], w=["gpB"])
                yield
                sc.op("act", lambda e: e.activation(out=Bt["An"][:], in_=pB[:, 512:640], func=AF.Copy), r=["gpB"], w=["g_An"])
                yield
                sc.op("dve", lambda e: e.tensor_tensor(out=Bt["TT"][:], in0=Bt["ATn"][:], in1=cst["ident_b"][:], op=ALU.add), r=["g_ATn", "c_ident_b"], w=["g_TT"])
                yield
                if g.gdn_stop == "C2":
                    return
                Pk, PTk = "An", "ATn"
                Pn, PTn = "A", "AT"
                for k in range(1, 6):
                    sc.op("pe", lambda e, Pk=Pk, PTk=PTk: e.matmul(pP[:, 0:128], lhsT=Bt[PTk][:], rhs=Bt[Pk][:], start=True, stop=True),
                          r=["g_" + Pk, "g_" + PTk], w=["gpP"])
                    yield
                    if k < 5:
                        sc.op("pe", lambda e, Pk=Pk, PTk=PTk: e.matmul(pU[:, 256:384], lhsT=Bt[Pk][:], rhs=Bt[PTk][:], start=True, stop=True),
                              r=["g_" + Pk, "g_" + PTk], w=["gpU"])
                        yield
                    sc.op("act", lambda e, Pn=Pn: e.activation(out=Bt[Pn][:], in_=pP[:, 0:128], func=AF.Copy), r=["gpP"], w=["g_" + Pn])
                    yield
                    if k < 5:
                        sc.op("dve", lambda e, PTn=PTn: e.tensor_copy(out=Bt[PTn][:], in_=pU[:, 256:384]), r=["gpU"], w=["g_" + PTn])
                        yield
                    sc.op("pe", lambda e, Pn=Pn: e.matmul(pTu[:, 0:128], lhsT=Bt[Pn][:], rhs=Bt["TT"][:], start=True, stop=True), r=["g_" + Pn, "g_TT"], w=["gpTu"])
                    yield
                    sc.op("dve", lambda e: e.tensor_tensor(out=Bt["TT"][:], in0=pTu[:, 0:128], in1=Bt["TT"][:], op=ALU.add), r=["gpTu", "g_TT"], w=["g_TT"])
                    yield
                    Pk, PTk, Pn, PTn = Pn, PTn, Pk, PTk
                if g.gdn_stop == "C3":
                    return
                sc.op("dve", lambda e, i=i: e.tensor_scalar(out=Bt["vb"][:], in0=Vtk[:, i, :], scalar1=be_col, scalar2=None, op0=ALU.mult), r=["gVtk"] + sck, w=["g_vb"])
                yield
                sc.op("dve", lambda e, i=i: e.tensor_scalar(out=Bt["kbg"][:], in0=Ktok[:, i, :], scalar1=bege_col, scalar2=None, op0=ALU.mult), r=["gKtok"] + sck, w=["g_kbg"])
                yield
                sc.op("dve", lambda e, i=i: e.tensor_scalar(out=Bkd[par][:], in0=Ktok[:, i, :], scalar1=kd_col, scalar2=None, op0=ALU.mult), r=["gKtok"] + sck, w=[f"g_kdec{par}"])
                yield
                sc.op("dve", lambda e, tsl=tsl: e.tensor_tensor(out=Bqg[par][:], in0=QTn[:, tsl], in1=FeG[par][:], op=ALU.mult), r=["gQTn", f"g_eG{par}"], w=[f"g_qgT{par}"])
                yield
                sc.op("pe", lambda e: e.matmul(pU[:, 0:128], lhsT=Bt["TT"][:], rhs=Bt["vb"][:], start=True, stop=True), r=["g_TT", "g_vb"], w=["gpU"])
                yield
                sc.op("pe", lambda e: e.matmul(pU[:, 128:256], lhsT=Bt["kbg"][:], rhs=Bt["TT"][:], start=True, stop=True), r=["g_TT", "g_kbg"], w=["gpU"])
                yield
                sc.op("act", lambda e: e.activation(out=Fu[par][:], in_=pU[:, 0:128], func=AF.Copy), r=["gpU"], w=[f"g_u{par}"])
                yield
                sc.op("dve", lambda e: e.tensor_copy(out=BwT[par][:], in_=pU[:, 128:256]), r=["gpU"], w=[f"g_wT{par}"])
                yield
            def rpart(i):
                nonlocal n_st
                par = i % 2
                tsl = slice(i * 128, (i + 1) * 128)
                if g.gdn_stop == "C4":
                    return
                for cj in range(2):
                    r0 = cj * 64
                    rs = slice(r0, r0 + 64)
                    vn = vnc[cj]
                    vk = f"g_vn{cj}"
                    sc.op("pe", lambda e: e.matmul(pV[:, 0:128], lhsT=BwT[par][:], rhs=Sb[:], start=True, stop=True), r=[f"g_wT{par}", "gSb"], w=["gpV"])
                    yield
                    sc.op("dve", lambda e: e.scalar_tensor_tensor(out=t2[:], in0=pV[:, 0:128], scalar=-1.0, in1=Fu[par][:], op0=ALU.mult, op1=ALU.add), r=[f"g_u{par}", "gpV"], w=["g_t2"])
                    yield
                    sc.op("dve", lambda e, cj=cj, vn=vn: e.tensor_scalar(out=vn[:], in0=t2[:], scalar1=cst["rowmask"][:, cj:cj + 1], scalar2=None, op0=ALU.mult), r=["g_t2", "c_rowmask"], w=[vk])
                    yield
                    sc.op("pe", lambda e, rs=rs: e.matmul(pO[:, rs], lhsT=Sb[:], rhs=Bqg[par][:, rs], start=True, stop=False), r=["gSb", f"g_qgT{par}"], w=["gpO"])
                    yield
                    sc.op("pe", lambda e, rs=rs, vn=vn: e.matmul(pO[:, rs], lhsT=vn[:], rhs=Bin[par][:, rs], start=False, stop=True), r=[vk, f"g_intraT{par}"], w=["gpO"])
                    yield
                    sc.op("pe", lambda e, vn=vn: e.matmul(pV[:, 128:256], lhsT=Bkd[par][:], rhs=vn[:], start=True, stop=True), r=[f"g_kdec{par}", vk], w=["gpV"])
                    yield
                    sc.op("dve", lambda e, r0=r0: e.scalar_tensor_tensor(out=Sf[:], in0=Sf[:], scalar=FeG[par][:, r0 + 63:r0 + 64], in1=pV[:, 128:256],
                                                                       op0=ALU.mult, op1=ALU.add), r=["gSf", f"g_eG{par}", "gpV"], w=["gSf"])
                    yield
                    sc.op("act", lambda e: e.activation(out=Sb[:], in_=Sf[:], func=AF.Copy), r=["gSf"], w=["gSb"])
                    yield
                if g.gdn_stop == "C5":
                    return
                sc.op("act", lambda e: e.activation(out=Bt["sq2"][:], in_=pO[:, 0:128], func=AF.Square), r=["gpO"], w=["g_sq2"])
                yield
                sc.op("pe", lambda e: e.matmul(pO[:, 128:256], lhsT=g.ones_b[:], rhs=Bt["sq2"][:], start=True, stop=True), r=["g_sq2", "ones_b"], w=["gpO"])
                yield
                sc.op("dve", lambda e: e.tensor_scalar(out=rn2[:], in0=pO[:, 128:256], scalar1=1.0 / 128, scalar2=EPS, op0=ALU.mult, op1=ALU.add), r=["gpO"], w=["g_rn2"])
                yield
                sc.op("act", lambda e: e.activation(out=rn2[:], in_=rn2[:], func=AF.Ln), r=["g_rn2"], w=["g_rn2"])
                yield
                sc.op("act", lambda e: e.activation(out=rn2[:], in_=rn2[:], func=AF.Exp, scale=-0.5), r=["g_rn2"], w=["g_rn2"])
                yield
                sc.op("dve", lambda e: e.scalar_tensor_tensor(out=t2[:], in0=pO[:, 0:128], scalar=gnw[:, 0:1], in1=rn2[:], op0=ALU.mult, op1=ALU.mult),
                      r=["gpO", "ggnw", "g_rn2"], w=["g_t2"])
                yield
                yb_ = n_st % 2
                sc.op("dve", lambda e, yb_=yb_, i=i, tsl=tsl: e.tensor_tensor(out=ystg[yb_][:, (i % 4) * 128:(i % 4 + 1) * 128], in0=t2[:], in1=zsT[:, tsl], op=ALU.mult),
                      r=["g_t2", "gzsT"], w=[f"gystg{yb_}"])
                yield
                if i % 4 == 3:
                    q0 = (i // 4) * 512
                    sc.dma("sp", lambda e, yb_=yb_, h=h, q0=q0: e.dma_start(out=io["YT"][(8 + h) * 128:(9 + h) * 128, q0:q0 + 512], in_=ystg[yb_][:]),
                           r=[f"gystg{yb_}"], w=["YT"])
                    n_st += 1
            for _ in tpart(0):
                pass
            for i in range(NT):
                ga = tpart(i + 1) if i + 1 < NT else None
                gb = rpart(i)
                while ga is not None or gb is not None:
                    if ga is not None:
                        try:
                            next(ga)
                        except StopIteration:
                            ga = None
                    if gb is not None:
                        try:
                            next(gb)
                        except StopIteration:
                            gb = None


def load_w_bf16(g, dst, src_l, stg, key):
    sc = g.sc
    v = src_l.rearrange("(kc p) f -> p kc f", p=128)
    for hf in range(2):
        sc.dma("sp", lambda e, hf=hf: e.dma_start(out=stg[hf][:], in_=v[:, :, hf * 512:(hf + 1) * 512]), w=[f"mstg{hf}"])
        sc.op("pool", lambda e, hf=hf: e.tensor_copy(out=dst[:, :, hf * 512:(hf + 1) * 512], in_=stg[hf][:]), r=[f"mstg{hf}"], w=[key])


def phase_merge(g, l, xsrc):
    nc, sc, io = g.nc, g.sc, g.io
    S = g.S
    NTC = S // 512
    YTv = io["YT"].rearrange("(c p) t -> p c t", p=128)
    PTv = io["PT"].rearrange("(c p) t -> p c t", p=128)
    with ExitStack() as ls:
        wa = T(ls, nc, "mwa", [128, KC, D], BF16)
        wb = T(ls, nc, "mwb", [128, KC, D], BF16)
        wo = T(ls, nc, "mwo", [128, KC, D], BF16)
        g1bc = T(ls, nc, "mg1", [128, D], F32)
        with ExitStack() as l2:
            stg = [T(l2, nc, f"mstg{i}", [128, KC, 512], F32) for i in range(2)]
            load_w_bf16(g, wa, io["w_a"][l], stg, "mwa")
            load_w_bf16(g, wb, io["w_b"][l], stg, "mwb")
            load_w_bf16(g, wo, io["w_out"][l], stg, "mwo")
            sc.barrier()
        load_bc(g, g1bc, l, 0, "mg1")
        yaT = T(ls, nc, "myaT", [128, KC, 512], BF16)
        ybT = T(ls, nc, "mybT", [128, KC, 512], BF16)
        gaT = T(ls, nc, "mgaT", [128, KC, 512], BF16)
        gbT = T(ls, nc, "mgbT", [128, KC, 512], BF16)
        mixT = T(ls, nc, "mmixT", [128, KC, 512], BF16)
        m1 = [T(ls, nc, f"mm1{i}", [128, 512], F32) for i in range(2)]
        m2 = [T(ls, nc, f"mm2{i}", [128, 512], F32) for i in range(2)]
        xt = [T(ls, nc, f"mxt{i}", [128, D], F32) for i in range(2)]
        xo = [T(ls, nc, f"mxo{i}", [128, D], F32) for i in range(2)]
        pA = [PS(ls, nc, f"mpA{i}", [128, 512], F32) for i in range(2)]
        pBm = [PS(ls, nc, f"mpB{i}", [128, 512], F32) for i in range(2)]
        pO = [PS(ls, nc, f"mpO{i}", [128, 512], F32) for i in range(2)]
        n = 0
        for tc in range(NTC):
            cs = slice(tc * 512, (tc + 1) * 512)
            sc.dma("sp", lambda e, cs=cs: e.dma_start(out=yaT[:], in_=YTv[:, 0:8, cs]), r=["YT"], w=["myaT"])
            sc.dma("sp", lambda e, cs=cs: e.dma_start(out=ybT[:], in_=YTv[:, 8:16, cs]), r=["YT"], w=["mybT"])
            sc.dma("act", lambda e, cs=cs: e.dma_start(out=gaT[:], in_=PTv[:, 48:56, cs]), r=["PT"], w=["mgaT"])
            sc.dma("act", lambda e, cs=cs: e.dma_start(out=gbT[:], in_=PTv[:, 56:64, cs]), r=["PT"], w=["mgbT"])
            for dc in range(KC):
                b = dc % 2
                for kc in range(KC):
                    sc.op("pe", lambda e, b=b, kc=kc, dc=dc: e.matmul(pA[b][:], lhsT=wa[:, kc, dc * 128:(dc + 1) * 128], rhs=yaT[:, kc, :],
                                                                      start=(kc == 0), stop=(kc == KC - 1)), r=["mwa", "myaT"], w=[f"mpA{b}"])
                for kc in range(KC):
                    sc.op("pe", lambda e, b=b, kc=kc, dc=dc: e.matmul(pBm[b][:], lhsT=wb[:, kc, dc * 128:(dc + 1) * 128], rhs=ybT[:, kc, :],
                                                                      start=(kc == 0), stop=(kc == KC - 1)), r=["mwb", "mybT"], w=[f"mpB{b}"])
                sc.op("dve", lambda e, b=b, dc=dc: e.tensor_tensor(out=m1[b][:], in0=pA[b][:], in1=gaT[:, dc, :], op=ALU.mult), r=[f"mpA{b}", "mgaT"], w=[f"mm1{b}"])
                sc.op("dve", lambda e, b=b, dc=dc: e.tensor_tensor(out=m2[b][:], in0=pBm[b][:], in1=gbT[:, dc, :], op=ALU.mult), r=[f"mpB{b}", "mgbT"], w=[f"mm2{b}"])
                sc.op("pool", lambda e, b=b, dc=dc: e.tensor_tensor(out=mixT[:, dc, :], in0=m1[b][:], in1=m2[b][:], op=ALU.add), r=[f"mm1{b}", f"mm2{b}"], w=["mmixT"])
            for tt in range(4):
                ti = tc * 4 + tt
                xb = n % 2
                n += 1
                sc.dma("sp", lambda e, xb=xb, ti=ti: e.dma_start(out=xt[xb][:], in_=xsrc[ti * 128:(ti + 1) * 128, :]), r=[f"xr{ti}"], w=[f"mxt{xb}"])
                for hf in range(2):
                    for kc in range(KC):
                        sc.op("pe", lambda e, hf=hf, kc=kc, tt=tt: e.matmul(pO[hf][:], lhsT=mixT[:, kc, tt * 128:(tt + 1) * 128], rhs=wo[:, kc, hf * 512:(hf + 1) * 512],
                                                                            start=(kc == 0), stop=(kc == KC - 1)), r=["mmixT", "mwo"], w=[f"mpO{hf}"])
                    hs = slice(hf * 512, (hf + 1) * 512)
                    sc.op("dve", lambda e, hf=hf, hs=hs, xb=xb: e.tensor_tensor(out=xo[xb][:, hs], in0=pO[hf][:], in1=g1bc[:, hs], op=ALU.mult), r=[f"mpO{hf}", "mg1"], w=[f"mxo{xb}"])
                    sc.op("pool", lambda e, hs=hs, xb=xb: e.tensor_tensor(out=xo[xb][:, hs], in0=xo[xb][:, hs], in1=xt[xb][:, hs], op=ALU.add), r=[f"mxo{xb}", f"mxt{xb}"], w=[f"mxo{xb}"])
                sc.dma("sp", lambda e, xb=xb, ti=ti: e.dma_start(out=io["xr"][ti * 128:(ti + 1) * 128, :], in_=xo[xb][:]), r=[f"mxo{xb}"], w=[f"xr{ti}"])


def phase_moe(g, l):
    nc, sc, io = g.nc, g.sc, g.io
    S, NB = g.S, g.NB
    NT = S // 128
    cst = g.cst
    SP = mybir.EngineType.SP
    with ExitStack() as ls:
        E1 = T(ls, nc, "oE1", [128, NT, 32], F32)
        E2 = T(ls, nc, "oE2", [128, NT, 32], F32)
        POS = T(ls, nc, "oPOS", [128, NT, 32], F32)
        Wt = T(ls, nc, "oW", [128, NT, 2], F32)
        IDXf = T(ls, nc, "oIDXf", [128, NT, 2], F32)
        IDX = T(ls, nc, "oIDX", [128, NT, 2], I32)
        basebc = T(ls, nc, "obase", [128, 32], F32)
        a2bc = T(ls, nc, "oa2bc", [128, D], F32)
        b2bc = T(ls, nc, "ob2bc", [128, D], F32)
        g2bc = T(ls, nc, "og2bc", [128, D], F32)
        wr = T(ls, nc, "owr", [128, KC, 36], F32)
        rb = T(ls, nc, "orb", [128, 36], F32)
        blk = T(ls, nc, "oblk", [128, NB], F32)
        blki = T(ls, nc, "oblki", [128, NB], I32)
        load_bc(g, a2bc, l, 1, "oa2bc")
        load_bc(g, b2bc, l, 2, "ob2bc")
        load_bc(g, g2bc, l, 3, "og2bc")
        sc.dma("sp", lambda e: e.dma_start(out=wr[:], in_=io["wr"][l].rearrange("(kc p) f -> p kc f", p=128)), w=["owr"])
        sc.dma("sp", lambda e: e.dma_start(out=rb[:], in_=io["rb_bc"][l]), w=["orb"])
        sc.op("pool", lambda e: e.memset(basebc[:], 0.0), w=["obase"])
        with ExitStack() as l1:
            xt = [T(l1, nc, f"ox{i}", [128, D], F32) for i in range(2)]
            xn = [T(l1, nc, f"oxn{i}", [128, D], F32) for i in range(2)]
            h2 = [T(l1, nc, f"oh2{i}", [128, D], F32) for i in range(2)]
            h2T = [T(l1, nc, f"oh2T{i}", [128, KC, 128], F32) for i in range(2)]
            junk = T(l1, nc, "ojunk", [128, D], BF16)
            st = [T(l1, nc, f"ost{i}", [128, 16, 16], F32) for i in range(2)]
            lg = [T(l1, nc, f"olg{i}", [128, 36], F32) for i in range(2)]
            es_ = [T(l1, nc, f"oes{i}", [128, 8], F32) for i in range(2)]
            em_ = [T(l1, nc, f"oem{i}", [128, 8], F32) for i in range(2)]
            oh = [T(l1, nc, f"ooh{i}", [128, 3, 8], F32) for i in range(2)]
            esum = [T(l1, nc, f"oesum{i}", [128, 32], F32) for i in range(2)]
            pst = [PS(l1, nc, f"opst{i}", [128, 512], F32) for i in range(4)]
            plg = [PS(l1, nc, f"oplg{i}", [128, 512], F32) for i in range(2)]
            ppos = [PS(l1, nc, f"oppos{i}", [128, 512], F32) for i in range(2)]
            for i in range(NT):
                b = i % 2
                S_ = lambda j, b=b: st[b][:, j, 0:1]
                kst = f"ost{b}"
                sc.dma("sp", lambda e, b=b, i=i: e.dma_start(out=xt[b][:], in_=io["xr"][i * 128:(i + 1) * 128, :]), r=[f"xr{i}"], w=[f"ox{b}"])
                sc.op("act", lambda e, b=b: e.activation(out=junk[:], in_=xt[b][:], func=AF.Square, accum_out=st[b][:, 0, 0:1]), r=[f"ox{b}"], w=["ojunk", kst])
                sc.op("dve", lambda e, b=b: e.tensor_scalar(out=st[b][:, 1, 0:1], in0=st[b][:, 0, 0:1], scalar1=1.0 / D, scalar2=EPS, op0=ALU.mult, op1=ALU.add), r=[kst], w=[kst])
                sc.op("act", lambda e, b=b: e.activation(out=st[b][:, 2, 0:1], in_=st[b][:, 1, 0:1], func=AF.Sqrt), r=[kst], w=[kst])
                sc.op("dve", lambda e, b=b: e.reciprocal(out=st[b][:, 3, 0:1], in_=st[b][:, 2, 0:1]), r=[kst], w=[kst])
                sc.op("dve", lambda e, b=b: e.tensor_scalar(out=xn[b][:], in0=xt[b][:], scalar1=st[b][:, 3, 0:1], scalar2=None, op0=ALU.mult), r=[f"ox{b}", kst], w=[f"oxn{b}"])
                sc.op("pool", lambda e, b=b: e.tensor_tensor(out=h2[b][:], in0=xn[b][:], in1=a2bc[:], op=ALU.mult), r=[f"oxn{b}", "oa2bc"], w=[f"oh2{b}"])
                sc.op("pool", lambda e, b=b: e.tensor_tensor(out=h2[b][:], in0=h2[b][:], in1=b2bc[:], op=ALU.add), r=[f"oh2{b}", "ob2bc"], w=[f"oh2{b}"])
                sc.dma("sp", lambda e, b=b, i=i: e.dma_start(out=io["h2d"][i * 128:(i + 1) * 128, :], in_=h2[b][:]), r=[f"oh2{b}"], w=[f"h2d{i}"])
                for kc in range(KC):
                    pb = b * 2 + kc // 4
                    sc.op("pe", lambda e, b=b, kc=kc, pb=pb: e.transpose(pst[pb][:, (kc % 4) * 128:(kc % 4 + 1) * 128], xn[b][:, kc * 128:(kc + 1) * 128], cst["ident_f"][:]),
                          r=[f"oxn{b}", "c_ident_f"], w=[f"opst{pb}"])
                for kc in range(KC):
                    pb = b * 2 + kc // 4
                    sc.op("act", lambda e, b=b, kc=kc, pb=pb: e.activation(out=h2T[b][:, kc, :], in_=pst[pb][:, (kc % 4) * 128:(kc % 4 + 1) * 128], func=AF.Identity,
                                                                          bias=g.mod[:, l, 24 + kc:25 + kc], scale=g.A2[:, l, kc:kc + 1]), r=[f"opst{pb}", "mod", "A2"], w=[f"oh2T{b}"])
                for kc in range(KC):
                    sc.op("pe", lambda e, b=b, kc=kc: e.matmul(plg[b][:, 0:36], lhsT=h2T[b][:, kc, :], rhs=wr[:, kc, :], start=(kc == 0), stop=(kc == KC - 1)),
                          r=[f"oh2T{b}", "owr"], w=[f"oplg{b}"])
                L = lg[b]
                kl = f"olg{b}"
                sc.op("dve", lambda e, b=b, L=L: e.tensor_tensor(out=L[:], in0=plg[b][:, 0:36], in1=rb[:], op=ALU.add), r=[f"oplg{b}", "orb"], w=[kl])
                sc.op("dve", lambda e, b=b, L=L: e.tensor_reduce(out=st[b][:, 4, 0:1], in_=L[:, 0:4], axis=AX.X, op=ALU.max), r=[kl], w=[kst])
                sc.op("dve", lambda e, b=b: e.tensor_scalar(out=st[b][:, 5, 0:1], in0=st[b][:, 4, 0:1], scalar1=-1.0, scalar2=None, op0=ALU.mult), r=[kst], w=[kst])
                sc.op("act", lambda e, b=b, L=L: e.activation(out=es_[b][:, 0:4], in_=L[:, 0:4], func=AF.Exp, bias=st[b][:, 5, 0:1], scale=1.0, accum_out=st[b][:, 6, 0:1]),
                      r=[kl, kst], w=[f"oes{b}", kst])
                sc.op("dve", lambda e, b=b: e.reciprocal(out=st[b][:, 7, 0:1], in_=st[b][:, 6, 0:1]), r=[kst], w=[kst])
                sc.op("dve", lambda e, b=b, L=L: e.tensor_scalar(out=oh[b][:, 0, 0:4], in0=L[:, 0:4], scalar1=st[b][:, 4, 0:1], scalar2=None, op0=ALU.is_equal),
                      r=[kl, kst], w=[f"ooh{b}"])
                sc.op("dve", lambda e, b=b, L=L: e.tensor_scalar(out=es_[b][:], in0=L[:, 4:12], scalar1=oh[b][:, 0, 0:1], scalar2=None, op0=ALU.mult), r=[kl, f"ooh{b}"], w=[f"oes{b}"])
                for gq in range(1, 4):
                    sc.op("dve", lambda e, b=b, L=L, gq=gq: e.scalar_tensor_tensor(out=es_[b][:], in0=L[:, 4 + gq * 8:12 + gq * 8], scalar=oh[b][:, 0, gq:gq + 1], in1=es_[b][:],
                                                                                 op0=ALU.mult, op1=ALU.add), r=[kl, f"ooh{b}", f"oes{b}"], w=[f"oes{b}"])
                sc.op("dve", lambda e, b=b: e.tensor_reduce(out=st[b][:, 8, 0:1], in_=es_[b][:], axis=AX.X, op=ALU.max), r=[f"oes{b}"], w=[kst])
                sc.op("dve", lambda e, b=b: e.tensor_scalar(out=oh[b][:, 1, :], in0=es_[b][:], scalar1=st[b][:, 8, 0:1], scalar2=None, op0=ALU.is_equal), r=[f"oes{b}", kst], w=[f"ooh{b}"])
                sc.op("dve", lambda e, b=b: e.scalar_tensor_tensor(out=em_[b][:], in0=oh[b][:, 1, :], scalar=-1e30, in1=es_[b][:], op0=ALU.mult, op1=ALU.add),
                      r=[f"ooh{b}", f"oes{b}"], w=[f"oem{b}"])
                sc.op("dve", lambda e, b=b: e.tensor_reduce(out=st[b][:, 9, 0:1], in_=em_[b][:], axis=AX.X, op=ALU.max), r=[f"oem{b}"], w=[kst])
                sc.op("dve", lambda e, b=b: e.tensor_scalar(out=oh[b][:, 2, :], in0=em_[b][:], scalar1=st[b][:, 9, 0:1], scalar2=None, op0=ALU.is_equal), r=[f"oem{b}", kst], w=[f"ooh{b}"])
                sc.op("dve", lambda e, b=b: e.tensor_scalar(out=st[b][:, 10, 0:1], in0=st[b][:, 8, 0:1], scalar1=-1.0, scalar2=None, op0=ALU.mult), r=[kst], w=[kst])
                sc.op("act", lambda e, b=b: e.activation(out=st[b][:, 11, 0:1], in_=st[b][:, 9, 0:1], func=AF.Exp, bias=st[b][:, 10, 0:1], scale=1.0), r=[kst], w=[kst])
                sc.op("dve", lambda e, b=b: e.tensor_scalar(out=st[b][:, 12, 0:1], in0=st[b][:, 11, 0:1], scalar1=1.0, scalar2=None, op0=ALU.add), r=[kst], w=[kst])
                sc.op("dve", lambda e, b=b: e.reciprocal(out=st[b][:, 13, 0:1], in_=st[b][:, 12, 0:1]), r=[kst], w=[kst])
                sc.op("dve", lambda e, b=b, i=i: e.tensor_tensor(out=Wt[:, i, 0:1], in0=st[b][:, 13, 0:1], in1=st[b][:, 7, 0:1], op=ALU.mult), r=[kst], w=[f"oW{i}"])
                sc.op("dve", lambda e, b=b, i=i: e.tensor_tensor(out=Wt[:, i, 1:2], in0=Wt[:, i, 0:1], in1=st[b][:, 11, 0:1], op=ALU.mult), r=[kst, f"oW{i}"], w=[f"oW{i}"])
                for gq in range(4):
                    sc.op("dve", lambda e, b=b, i=i, gq=gq: e.tensor_scalar(out=E1[:, i, gq * 8:(gq + 1) * 8], in0=oh[b][:, 1, :], scalar1=oh[b][:, 0, gq:gq + 1], scalar2=None, op0=ALU.mult),
                          r=[f"ooh{b}"], w=[f"oE{i}"])
                    sc.op("dve", lambda e, b=b, i=i, gq=gq: e.tensor_scalar(out=E2[:, i, gq * 8:(gq + 1) * 8], in0=oh[b][:, 2, :], scalar1=oh[b][:, 0, gq:gq + 1], scalar2=None, op0=ALU.mult),
                          r=[f"ooh{b}"], w=[f"oE{i}"])
                sc.op("dve", lambda e, b=b, i=i: e.tensor_tensor(out=esum[b][:], in0=E1[:, i, :], in1=E2[:, i, :], op=ALU.add), r=[f"oE{i}"], w=[f"oesum{b}"])
                sc.op("pe", lambda e, b=b: e.matmul(ppos[b][:, 0:32], lhsT=cst["sltU"][:], rhs=esum[b][:], start=True, stop=True), r=[f"oesum{b}", "c_sltU"], w=[f"oppos{b}"])
                sc.op("pe", lambda e, b=b: e.matmul(ppos[b][:, 32:64], lhsT=g.ones_f[:], rhs=esum[b][:], start=True, stop=True), r=[f"oesum{b}", "ones_f"], w=[f"oppos{b}"])
                sc.op("dve", lambda e, b=b, i=i: e.tensor_tensor(out=POS[:, i, :], in0=ppos[b][:, 0:32], in1=basebc[:], op=ALU.add), r=[f"oppos{b}", "obase"], w=[f"oPOS{i}"])
                sc.op("dve", lambda e, b=b: e.tensor_tensor(out=basebc[:], in0=ppos[b][:, 32:64], in1=basebc[:], op=ALU.add), r=[f"oppos{b}", "obase"], w=["obase"])
            sc.barrier()
        if g.moe_stop == "1":
            return
        with ExitStack() as l2:
            v = T(l2, nc, "ov", [128, 8, 32], F32)
            vi = T(l2, nc, "ovi", [128, 32], I32)
            ones32 = T(l2, nc, "oones32", [128, 32], F32)
            tmp32 = T(l2, nc, "otmp32", [128, 32], F32)
            acc = T(l2, nc, "oacc", [128, NB], F32)
            cmp_ = T(l2, nc, "ocmp", [128, NB], F32)
            iot = T(l2, nc, "oiot", [128, NB], F32)
            h2r = [T(l2, nc, f"oh2r{i}", [128, D], F32) for i in range(2)]
            sc.dma("sp", lambda e: e.dma_start(out=iot[:], in_=io["iota_nb"]), w=["oiot"])
            sc.op("pool", lambda e: e.memset(ones32[:], 1.0), w=["oones32"])
            sc.op("dve", lambda e: e.tensor_scalar(out=v[:, 0, :], in0=basebc[:], scalar1=127.0, scalar2=1.0 / 128, op0=ALU.add, op1=ALU.mult), r=["obase"], w=["ov"])
            sc.op("dve", lambda e: e.tensor_copy(out=vi[:], in_=v[:, 0, :]), r=["ov"], w=["ovi"])
            sc.op("dve", lambda e: e.tensor_copy(out=v[:, 2, :], in_=vi[:]), r=["ovi"], w=["ov"])
            sc.op("dve", lambda e: e.tensor_tensor(out=v[:, 3, :], in0=v[:, 2, :], in1=v[:, 0, :], op=ALU.is_gt), r=["ov"], w=["ov"])
            sc.op("dve", lambda e: e.tensor_tensor(out=v[:, 2, :], in0=v[:, 2, :], in1=v[:, 3, :], op=ALU.subtract), r=["ov"], w=["ov"])
            sc.op("dve", lambda e: e.tensor_tensor(out=v[:, 3, :], in0=v[:, 0, :], in1=v[:, 2, :], op=ALU.subtract), r=["ov"], w=["ov"])
            sc.op("dve", lambda e: e.tensor_scalar(out=v[:, 3, :], in0=v[:, 3, :], scalar1=1.0, scalar2=None, op0=ALU.is_ge), r=["ov"], w=["ov"])
            sc.op("dve", lambda e: e.tensor_tensor(out=v[:, 2, :], in0=v[:, 2, :], in1=v[:, 3, :], op=ALU.add), r=["ov"], w=["ov"])
            sc.op("dve", lambda e: e.tensor_tensor_scan(out=v[:, 4, :], data0=ones32[:], data1=v[:, 2, :], initial=0.0, op0=ALU.mult, op1=ALU.add), r=["ov", "oones32"], w=["ov"])
            sc.op("dve", lambda e: e.tensor_tensor(out=v[:, 5, :], in0=v[:, 4, :], in1=v[:, 2, :], op=ALU.subtract), r=["ov"], w=["ov"])
            sc.op("dve", lambda e: e.tensor_scalar(out=v[:, 5, :], in0=v[:, 5, :], scalar1=128.0, scalar2=None, op0=ALU.mult), r=["ov"], w=["ov"])
            sc.op("pool", lambda e: e.memset(acc[:], 0.0), w=["oacc"])
            for e_ in range(32):
                sc.op("dve", lambda e, e_=e_: e.tensor_scalar(out=cmp_[:], in0=iot[:], scalar1=v[:, 4, e_:e_ + 1], scalar2=None, op0=ALU.is_ge), r=["oiot", "ov"], w=["ocmp"])
                sc.op("dve", lambda e: e.tensor_tensor(out=acc[:], in0=acc[:], in1=cmp_[:], op=ALU.add), r=["ocmp", "oacc"], w=["oacc"])
            sc.op("dve", lambda e: e.tensor_scalar(out=blk[:], in0=acc[:], scalar1=31.0, scalar2=None, op0=ALU.min), r=["oacc"], w=["oblk"])
            sc.op("dve", lambda e: e.tensor_copy(out=blki[:], in_=blk[:]), r=["oblk"], w=["oblki"])
            zt = T(l2, nc, "ozt", [128, D], F32)
            sc.op("pool", lambda e: e.memset(zt[:], 0.0), w=["ozt"])
            for bk in range(NB):
                sc.dma("act" if bk % 2 else "sp", lambda e, bk=bk: e.dma_start(out=io["xs"][bk * 128:(bk + 1) * 128, :], in_=zt[:]), r=["ozt"], w=["xs"])
            for i in range(NT):
                b = i % 2
                for k, Ek in enumerate((E1, E2)):
                    sc.op("dve", lambda e, i=i: e.tensor_tensor(out=tmp32[:], in0=POS[:, i, :], in1=v[:, 5, :], op=ALU.add), r=[f"oPOS{i}", "ov"], w=["otmp32"])
                    sc.op("dve", lambda e, i=i, Ek=Ek: e.tensor_tensor(out=tmp32[:], in0=tmp32[:], in1=Ek[:, i, :], op=ALU.mult), r=["otmp32", f"oE{i}"], w=["otmp32"])
                    sc.op("dve", lambda e, i=i, k=k: e.tensor_reduce(out=IDXf[:, i, k:k + 1], in_=tmp32[:], axis=AX.X, op=ALU.add), r=["otmp32"], w=[f"oIDX{i}"])
                sc.op("dve", lambda e, i=i: e.tensor_copy(out=IDX[:, i, :], in_=IDXf[:, i, :]), r=[f"oIDX{i}"], w=[f"oIDX{i}"])
                sc.dma("sp", lambda e, b=b, i=i: e.dma_start(out=h2r[b][:], in_=io["h2d"][i * 128:(i + 1) * 128, :]), r=[f"h2d{i}"], w=[f"oh2r{b}"])
                for k in range(2):
                    sc.swdma(lambda e, b=b, i=i, k=k: e.indirect_dma_start(
                        out=io["xs"], out_offset=bass.IndirectOffsetOnAxis(ap=IDX[:, i, k:k + 1], axis=0), in_=h2r[b][:], in_offset=None),
                        r=[f"oh2r{b}", f"oIDX{i}"], w=["xs"])
            sc.barrier()
        if g.moe_stop == "2":
            return
        with ExitStack() as l3:
            w1 = [T(l3, nc, f"ow1{i}", [128, KC, DE], F32) for i in range(2)]
            w3 = [T(l3, nc, f"ow3{i}", [128, KC, DE], F32) for i in range(2)]
            w2 = [T(l3, nc, f"ow2{i}", [128, 4, D], F32) for i in range(2)]
            xb_ = [T(l3, nc, f"oxb{i}", [128, D], F32) for i in range(2)]
            xT = [T(l3, nc, f"oxT{i}", [128, KC, 128], F32) for i in range(2)]
            sl = T(l3, nc, "osl", [128, 128], F32)
            gT = T(l3, nc, "ogT", [128, 4, 128], F32)
            yb_ = [T(l3, nc, f"oyb{i}", [128, D], F32) for i in range(2)]
            ptr = [PS(l3, nc, f"optr{i}", [128, 512], F32) for i in range(2)]
            ph = [PS(l3, nc, f"oph{i}", [128, 512], F32) for i in range(2)]
            py = [PS(l3, nc, f"opy{i}", [128, 512], F32) for i in range(2)]
            widf = T(l3, nc, "owidf", [128, 16], F32)
            widx = [T(l3, nc, f"owidx{i}", [128, 16], I32) for i in range(2)]
            iop = T(l3, nc, "oiop", [128, 1], F32)
            sc.dma("sp", lambda e: e.dma_start(out=iop[:], in_=io["iota_p"]), w=["oiop"])
            for bk in range(NB):
                b = bk % 2
                sc.op("dve", lambda e, bk=bk: e.scalar_tensor_tensor(out=widf[:, 0:1], in0=blk[:, bk:bk + 1], scalar=128.0, in1=iop[:], op0=ALU.mult, op1=ALU.add),
                      r=["oblk", "oiop"], w=["owidf"])
                sc.op("dve", lambda e: e.tensor_scalar(out=widf[:, 0:1], in0=widf[:, 0:1], scalar1=float(l * NE * 128), scalar2=None, op0=ALU.add), r=["owidf"], w=["owidf"])
                sc.op("dve", lambda e, b=b: e.tensor_copy(out=widx[b][:, 0:1], in_=widf[:, 0:1]), r=["owidf"], w=[f"owidx{b}"])
                sc.swdma(lambda e, b=b: e.indirect_dma_start(out=w1[b][:].rearrange("p k f -> p (k f)"), out_offset=None, in_=io["ew1"],
                                                             in_offset=bass.IndirectOffsetOnAxis(ap=widx[b][:, 0:1], axis=0)), r=[f"owidx{b}"], w=[f"ow1{b}"])
                sc.swdma(lambda e, b=b: e.indirect_dma_start(out=w3[b][:].rearrange("p k f -> p (k f)"), out_offset=None, in_=io["ew3"],
                                                             in_offset=bass.IndirectOffsetOnAxis(ap=widx[b][:, 0:1], axis=0)), r=[f"owidx{b}"], w=[f"ow3{b}"])
                sc.swdma(lambda e, b=b: e.indirect_dma_start(out=w2[b][:].rearrange("p k f -> p (k f)"), out_offset=None, in_=io["ew2"],
                                                             in_offset=bass.IndirectOffsetOnAxis(ap=widx[b][:, 0:1], axis=0)), r=[f"owidx{b}"], w=[f"ow2{b}"])
                sc.dma("act", lambda e, b=b, bk=bk: e.dma_start(out=xb_[b][:], in_=io["xs"][bk * 128:(bk + 1) * 128, :]), r=["xs"], w=[f"oxb{b}"])
                for kc in range(KC):
                    pb = kc // 4
                    sc.op("pe", lambda e, b=b, kc=kc, pb=pb: e.transpose(ptr[pb][:, (kc % 4) * 128:(kc % 4 + 1) * 128], xb_[b][:, kc * 128:(kc + 1) * 128], cst["ident_f"][:]),
                          r=[f"oxb{b}", "c_ident_f"], w=[f"optr{pb}"])
                for kc in range(KC):
                    pb = kc // 4
                    if pb == 0:
                        sc.op("act", lambda e, b=b, kc=kc, pb=pb: e.activation(out=xT[b][:, kc, :], in_=ptr[pb][:, (kc % 4) * 128:(kc % 4 + 1) * 128], func=AF.Copy),
                              r=[f"optr{pb}"], w=[f"oxT{b}"])
                    else:
                        sc.op("dve", lambda e, b=b, kc=kc, pb=pb: e.tensor_copy(out=xT[b][:, kc, :], in_=ptr[pb][:, (kc % 4) * 128:(kc % 4 + 1) * 128]),
                              r=[f"optr{pb}"], w=[f"oxT{b}"])
                for fc in range(4):
                    pb = fc % 2
                    for kc in range(KC):
                        sc.op("pe", lambda e, b=b, fc=fc, kc=kc, pb=pb: e.matmul(ph[pb][:, 0:128], lhsT=w1[b][:, kc, fc * 128:(fc + 1) * 128], rhs=xT[b][:, kc, :],
                                                                              start=(kc == 0), stop=(kc == KC - 1)), r=[f"ow1{b}", f"oxT{b}"], w=[f"oph{pb}"])
                    for kc in range(KC):
                        sc.op("pe", lambda e, b=b, fc=fc, kc=kc, pb=pb: e.matmul(py[pb][:, 0:128], lhsT=w3[b][:, kc, fc * 128:(fc + 1) * 128], rhs=xT[b][:, kc, :],
                                                                              start=(kc == 0), stop=(kc == KC - 1)), r=[f"ow3{b}", f"oxT{b}"], w=[f"opy{pb}"])
                    sc.op("act", lambda e, pb=pb: e.activation(out=sl[:], in_=ph[pb][:, 0:128], func=AF.Silu), r=[f"oph{pb}"], w=["osl"])
                    sc.op("dve", lambda e, pb=pb, fc=fc: e.tensor_tensor(out=gT[:, fc, :], in0=py[pb][:, 0:128], in1=sl[:], op=ALU.mult), r=[f"opy{pb}", "osl"], w=["ogT"])
                for hf in range(2):
                    for fc in range(4):
                        sc.op("pe", lambda e, b=b, hf=hf, fc=fc: e.matmul(ptr[hf][:], lhsT=gT[:, fc, :], rhs=w2[b][:, fc, hf * 512:(hf + 1) * 512],
                                                                          start=(fc == 0), stop=(fc == 3)), r=["ogT", f"ow2{b}"], w=[f"optr{hf}"])
                    if hf == 0:
                        sc.op("act", lambda e, b=b: e.activation(out=yb_[b][:, 0:512], in_=ptr[0][:], func=AF.Copy), r=["optr0"], w=[f"oyb{b}"])
                    else:
                        sc.op("dve", lambda e, b=b: e.tensor_copy(out=yb_[b][:, 512:1024], in_=ptr[1][:]), r=["optr1"], w=[f"oyb{b}"])
                sc.dma("act", lambda e, b=b, bk=bk: e.dma_start(out=io["ys"][bk * 128:(bk + 1) * 128, :], in_=yb_[b][:]), r=[f"oyb{b}"], w=["ys"])
            sc.barrier()
        if g.moe_stop == "3":
            return
        with ExitStack() as l4:
            y0 = [T(l4, nc, f"oy0{i}", [128, D], F32) for i in range(2)]
            y1 = [T(l4, nc, f"oy1{i}", [128, D], F32) for i in range(2)]
            xt = [T(l4, nc, f"oxc{i}", [128, D], F32) for i in range(2)]
            for i in range(NT):
                b = i % 2
                sc.swdma(lambda e, b=b, i=i: e.indirect_dma_start(out=y0[b][:], out_offset=None, in_=io["ys"],
                                                                  in_offset=bass.IndirectOffsetOnAxis(ap=IDX[:, i, 0:1], axis=0)), r=["ys", f"oIDX{i}"], w=[f"oy0{b}"])
                sc.swdma(lambda e, b=b, i=i: e.indirect_dma_start(out=y1[b][:], out_offset=None, in_=io["ys"],
                                                                  in_offset=bass.IndirectOffsetOnAxis(ap=IDX[:, i, 1:2], axis=0)), r=["ys", f"oIDX{i}"], w=[f"oy1{b}"])
                if g.moe_stop == "4a":
                    continue
                sc.dma("sp", lambda e, b=b, i=i: e.dma_start(out=xt[b][:], in_=io["xr"][i * 128:(i + 1) * 128, :]), r=[f"xr{i}"], w=[f"oxc{b}"])
                sc.op("dve", lambda e, b=b, i=i: e.tensor_scalar(out=y0[b][:], in0=y0[b][:], scalar1=Wt[:, i, 0:1], scalar2=None, op0=ALU.mult), r=[f"oy0{b}", f"oW{i}"], w=[f"oy0{b}"])
                sc.op("dve", lambda e, b=b, i=i: e.scalar_tensor_tensor(out=y0[b][:], in0=y1[b][:], scalar=Wt[:, i, 1:2], in1=y0[b][:], op0=ALU.mult, op1=ALU.add),
                      r=[f"oy0{b}", f"oy1{b}", f"oW{i}"], w=[f"oy0{b}"])
                sc.op("pool", lambda e, b=b: e.tensor_tensor(out=y0[b][:], in0=y0[b][:], in1=g2bc[:], op=ALU.mult), r=[f"oy0{b}", "og2bc"], w=[f"oy0{b}"])
                sc.op("pool", lambda e, b=b: e.tensor_tensor(out=xt[b][:], in0=xt[b][:], in1=y0[b][:], op=ALU.add), r=[f"oy0{b}", f"oxc{b}"], w=[f"oxc{b}"])
                sc.dma("sp", lambda e, b=b, i=i: e.dma_start(out=io["xr"][i * 128:(i + 1) * 128, :], in_=xt[b][:]), r=[f"oxc{b}"], w=[f"xr{i}"])


def phase_final(g, xsrc):
    nc, sc, io = g.nc, g.sc, g.io
    NT = g.S // 128
    with ExitStack() as ls:
        fw = T(ls, nc, "fw", [128, D], F32)
        xt = [T(ls, nc, f"fx{i}", [128, D], F32) for i in range(2)]
        yt = [T(ls, nc, f"fy{i}", [128, D], F32) for i in range(2)]
        junk = T(ls, nc, "fjunk", [128, D], BF16)
        st = [T(ls, nc, f"fst{i}", [128, 64], F32) for i in range(2)]
        sc.dma("sp", lambda e: e.dma_start(out=fw[:], in_=io["fnw_bc"]), w=["fw"])
        for i in range(NT):
            b = i % 2
            sc.dma("sp", lambda e, b=b, i=i: e.dma_start(out=xt[b][:], in_=xsrc[i * 128:(i + 1) * 128, :]), w=[f"fx{b}"])
            sc.op("act", lambda e, b=b: e.activation(out=junk[:], in_=xt[b][:], func=AF.Square, accum_out=st[b][:, 0:1]),
                  r=[f"fx{b}"], w=["fjunk", f"fst{b}"])
            sc.op("dve", lambda e, b=b: e.tensor_scalar(out=st[b][:, 16:17], in0=st[b][:, 0:1], scalar1=1.0 / D, scalar2=EPS,
                                                         op0=ALU.mult, op1=ALU.add), r=[f"fst{b}"], w=[f"fst{b}"])
            sc.op("act", lambda e, b=b: e.activation(out=st[b][:, 32:33], in_=st[b][:, 16:17], func=AF.Sqrt), r=[f"fst{b}"], w=[f"fst{b}"])
            sc.op("dve", lambda e, b=b: e.reciprocal(out=st[b][:, 48:49], in_=st[b][:, 32:33]), r=[f"fst{b}"], w=[f"fst{b}"])
            sc.op("dve", lambda e, b=b: e.scalar_tensor_tensor(out=yt[b][:], in0=xt[b][:], scalar=st[b][:, 48:49], in1=fw[:],
                                                                op0=ALU.mult, op1=ALU.mult), r=[f"fx{b}", f"fst{b}", "fw"], w=[f"fy{b}"])
            sc.dma("sp", lambda e, b=b, i=i: e.dma_start(out=io["y"][i * 128:(i + 1) * 128, :], in_=yt[b][:]), r=[f"fy{b}"], w=["y"])


def build(S, dbg=(), stop_after=None, layers=DEPTH):
    NB = (2 * S) // 128 + NE
    nc = bass.Bass("TRN2", target_bir_lowering=False)
    g = Ctx()
    g.nc, g.S, g.NB = nc, S, NB
    g.dbgset = set(dbg)
    import os as _os
    g.gdn_stop = _os.environ.get("GDN_STOP")
    g.moe_stop = _os.environ.get("MOE_STOP")
    dbg = [d for d in dbg if d in ("PT", "Vtok", "BD", "YT", "xr", "xs", "ys")]
    g.io = declare_io(nc, S, NB)
    io = g.io
    dbg_out = {}
    for name in dbg:
        src = io[name]
        dbg_out[name] = nc.dram_tensor("dbg_" + name, list(src.shape), src.dtype, kind="ExternalOutput").ap()
    with ExitStack() as es:
        g.es = es
        g.sc = Sched(nc, es)
        sc = g.sc
        phase_setup(g)
        done = False
        for l in range(layers):
            xsrc = io["x"] if l == 0 else io["xr"]
            with ExitStack() as ls:
                hT = T(ls, nc, "hT", [128, KC, S], BF16)
                phase_norm(g, l, xsrc, hT, g.A1[:, l, :], g.mod[:, l, 0:8])
                sc.barrier()
                dbg_sb(g, "hT", hT[:], [f"hT{i}" for i in range(S // 128)])
                phase_proj(g, l, hT)
                sc.barrier()
            if stop_after == "proj":
                break
            phase_attn(g, l)
            sc.barrier()
            if stop_after == "attn":
                break
            phase_gdn(g, l)
            sc.barrier()
            if stop_after == "gdn":
                break
            phase_merge(g, l, xsrc)
            sc.barrier()
            if stop_after == "merge":
                break
            phase_moe(g, l)
            sc.barrier()
            if stop_after == "moe":
                break
        sc.barrier()
        phase_final(g, io["xr"] if stop_after is None else io["x"])
        for name in dbg:
            src = io[name]
            dst = dbg_out[name]
            sc.dma("sp", lambda e, src=src, dst=dst: e.dma_start(out=dst, in_=src), r=[name], w=["dbg_" + name])
        sc.final_wait()
        print("instructions:", sc.ninst, "sems:", len(sc.semobj))
    return nc


def host_shared(inp):
    f = lambda a: np.ascontiguousarray(np.asarray(a, dtype=np.float32))
    sh = {}
    colT = lambda v: f(np.asarray(v).reshape(-1, 128).T)
    sh["ada_w"] = f(inp["ada_w"])
    sh["ada_b_t"] = f(np.stack([colT(inp["ada_b"][l]) for l in range(DEPTH)]))
    sh["n1w_t"] = f(np.stack([colT(inp["norm1_w"][l]) for l in range(DEPTH)]))
    sh["n2w_t"] = f(np.stack([colT(inp["norm2_w"][l]) for l in range(DEPTH)]))
    sh["fnw_bc"] = f(np.broadcast_to(np.asarray(inp["final_norm_w"])[None, :], (128, D)))
    sh["w_in"] = f(inp["w_in"])
    cw = np.asarray(inp["conv_w"])
    sh["conv_t"] = f(cw.reshape(DEPTH, 4, 24, 128).transpose(0, 3, 2, 1))
    lam = np.stack([np.asarray(inp[k]) for k in ("lambda_q1", "lambda_k1", "lambda_q2", "lambda_k2")], axis=1)
    sh["lamv"] = f(np.broadcast_to(lam[:, None], (DEPTH, 128, 4, 64)))
    sh["subln_bc"] = f(np.broadcast_to(np.asarray(inp["subln_w"])[:, None, :], (DEPTH, 128, 128)))
    sh["alog_bc"] = f(np.broadcast_to(np.asarray(inp["a_log"])[:, None, :], (DEPTH, 128, NH)))
    sh["dtb_bc"] = f(np.broadcast_to(np.asarray(inp["dt_bias"])[:, None, :], (DEPTH, 128, NH)))
    sh["gnw_t"] = f(np.asarray(inp["gdn_norm_w"])[:, :, None])
    sh["w_a"] = f(inp["w_branch_a"])
    sh["w_b"] = f(inp["w_branch_b"])
    sh["w_out"] = f(inp["w_out"])
    sh["wr"] = f(np.concatenate([np.asarray(inp["router_group_w"]), np.asarray(inp["router_expert_w"])], axis=2))
    rb = np.concatenate([np.asarray(inp["router_group_b"]), np.asarray(inp["router_expert_b"])], axis=1)
    sh["rb_bc"] = f(np.broadcast_to(rb[:, None, :], (DEPTH, 128, 36)))
    sh["ew1"] = f(np.asarray(inp["expert_w1"]).reshape(DEPTH, NE, KC, 128, DE).transpose(0, 1, 3, 2, 4).reshape(DEPTH * NE * 128, KC * DE))
    sh["ew3"] = f(np.asarray(inp["expert_w3"]).reshape(DEPTH, NE, KC, 128, DE).transpose(0, 1, 3, 2, 4).reshape(DEPTH * NE * 128, KC * DE))
    sh["ew2"] = f(np.asarray(inp["expert_w2"]).reshape(DEPTH, NE, 4, 128, D).transpose(0, 1, 3, 2, 4).reshape(DEPTH * NE * 128, 4 * D))
    sh.update(_consts())
    return sh


def host_core(inp, b):
    S_ = int(np.asarray(inp["x"]).shape[1])
    NB_ = (2 * S_) // 128 + NE
    return {"iota_nb": np.ascontiguousarray(np.broadcast_to(np.arange(NB_, dtype=np.float32)[None, :], (128, NB_))),
            "iota_p": np.arange(128, dtype=np.float32)[:, None].copy(),
            "x": np.ascontiguousarray(np.asarray(inp["x"][b], dtype=np.float32)),
            "c_t": np.ascontiguousarray(np.asarray(inp["c"][b], dtype=np.float32).reshape(KC, 128).T)}


def kernel(**inputs):
    S = int(np.asarray(inputs["x"]).shape[1])
    B = int(np.asarray(inputs["x"]).shape[0])
    sh = host_shared(inputs)
    nc = build(S)
    in_maps = [{**sh, **host_core(inputs, b)} for b in range(B)]
    res = run_bass_kernel_spmd(nc, in_maps, core_ids=list(range(B)))
    return np.stack([np.asarray(r["y"], dtype=np.float32) for r in res.results], axis=0)
```
